# Optimizing a Trainium2 kernel written in Bass

```python
import math
import jax
import jax.numpy as jnp
from jax import lax
import numpy as np


D_MODEL = 1024
BATCH = 16
SEQ = 2048
DEPTH = 1

MIX_WIDTH = D_MODEL
HEAD_DIM = 64
ATT_WIDTH = MIX_WIDTH // 2
ATT_HEADS = ATT_WIDTH // HEAD_DIM
IDX_HEADS = 4
IDX_DIM = HEAD_DIM
TOPK_MAX = 256
Q_BLOCK = 128
ROPE_THETA = 10000.0
INDEXER_SCALE = (IDX_HEADS ** -0.5) * (IDX_DIM ** -0.5)
SSD_WIDTH = MIX_WIDTH - ATT_WIDTH
SSD_HEAD_DIM = 64
SSD_HEADS = SSD_WIDTH // SSD_HEAD_DIM
SSD_GROUPS = 2
D_STATE = 64
CONV_WIDTH = 4
CONV_CH = SSD_WIDTH + 2 * SSD_GROUPS * D_STATE
CHUNK = 128
N_GROUPS_MOE = 4
EXPERTS_PER_GROUP = 4
N_EXPERTS = N_GROUPS_MOE * EXPERTS_PER_GROUP
TOP_K_IN_GROUP = 2
EXPERT_FF = 256
ALPHA = (2 * DEPTH) ** 0.25
BETA = (8 * DEPTH) ** -0.25
LN_EPS = 1e-5
IN_SPLIT_SIZES = (ATT_WIDTH, HEAD_DIM, HEAD_DIM, IDX_HEADS * IDX_DIM, IDX_DIM, IDX_HEADS, SSD_WIDTH, CONV_CH, SSD_HEADS)
IN_WIDTH = sum(IN_SPLIT_SIZES)
IN_SPLIT_POINTS = tuple(int(v) for v in np.cumsum(IN_SPLIT_SIZES)[:-1])

kernel_name = 'hybrid_dsa_ssd_hmoe_deepnorm'


def layer_norm(x, g, b):
    xf = x.astype(jnp.float32)
    mu = jnp.mean(xf, -1, keepdims=True)
    var = jnp.mean(jnp.square(xf - mu), -1, keepdims=True)
    return ((xf - mu) * lax.rsqrt(var + LN_EPS)).astype(x.dtype) * g + b


def rope_tables(seq_len):
    inv = ROPE_THETA ** (-jnp.arange(0, HEAD_DIM, 2, dtype=jnp.float32) / HEAD_DIM)
    ang = jnp.arange(seq_len, dtype=jnp.float32)[:, None] * inv[None, :]
    return jnp.cos(ang), jnp.sin(ang)


def apply_rope(x, cos, sin):
    half = x.shape[-1] // 2
    x1, x2 = x[..., :half], x[..., half:]
    cos = cos.astype(x.dtype)
    sin = sin.astype(x.dtype)
    return jnp.concatenate([x1 * cos - x2 * sin, x1 * sin + x2 * cos], -1)


def dsa_attention(q, k, v, iq, ik, iw, topk):
    bsz, s, h, dh = q.shape
    n_blocks = s // Q_BLOCK
    key_pos = jnp.arange(s)

    def block(i):
        t0 = i * Q_BLOCK
        qb = lax.dynamic_slice_in_dim(q, t0, Q_BLOCK, 1)
        iqb = lax.dynamic_slice_in_dim(iq, t0, Q_BLOCK, 1)
        iwb = lax.dynamic_slice_in_dim(iw, t0, Q_BLOCK, 1)
        qpos = t0 + jnp.arange(Q_BLOCK)
        causal = key_pos[None, :] <= qpos[:, None]
        sc = jax.nn.relu(jnp.einsum('bthd,bsd->bths', iqb, ik))
        sc = jnp.einsum('bths,bth->bts', sc, iwb).astype(jnp.float32)
        sc = jnp.where(causal[None], sc, -jnp.inf)
        _, sel = lax.top_k(sc, topk)
        valid = sel <= qpos[None, :, None]
        k_sel = jax.vmap(lambda a, idx: a[idx])(k, sel)
        v_sel = jax.vmap(lambda a, idx: a[idx])(v, sel)
        logits = jnp.einsum('bthd,btkd->bthk', qb, k_sel).astype(jnp.float32) * (dh ** -0.5)
        logits = jnp.where(valid[:, :, None, :], logits, -jnp.inf)
        p = jax.nn.softmax(logits, -1).astype(v.dtype)
        return jnp.einsum('bthk,btkd->bthd', p, v_sel)

    out = lax.map(block, jnp.arange(n_blocks))
    return jnp.moveaxis(out, 0, 1).reshape(bsz, s, h * dh)


def causal_depthwise_conv(u, w, b):
    out = lax.conv_general_dilated(
        u, w[:, None, :].astype(u.dtype), window_strides=(1,),
        padding=[(CONV_WIDTH - 1, 0)], dimension_numbers=('NWC', 'WIO', 'NWC'),
        feature_group_count=u.shape[-1])
    return out + b


def segsum(a):
    t = a.shape[-1]
    ar = jnp.broadcast_to(a[..., :, None], a.shape + (t,))
    strict = jnp.tril(jnp.ones((t, t), dtype=bool), -1)
    cs = jnp.cumsum(jnp.where(strict, ar, 0.0), axis=-2)
    return jnp.where(jnp.tril(jnp.ones((t, t), dtype=bool)), cs, -jnp.inf)


def ssd_chunked(xdt, adt, bm, cm):
    b, s, h, p = xdt.shape
    n = bm.shape[-1]
    c = s // CHUNK
    X = xdt.reshape(b, c, CHUNK, h, p)
    Bc = bm.reshape(b, c, CHUNK, h, n)
    Cc = cm.reshape(b, c, CHUNK, h, n)
    A = adt.reshape(b, c, CHUNK, h).transpose(0, 3, 1, 2)
    a_cum = jnp.cumsum(A, -1)
    Lmat = jnp.exp(segsum(A))
    y_diag = jnp.einsum('bclhn,bcshn,bhcls,bcshp->bclhp', Cc, Bc, Lmat, X)
    decay_states = jnp.exp(a_cum[..., -1:] - a_cum)
    chunk_states = jnp.einsum('bclhn,bhcl,bclhp->bchpn', Bc, decay_states, X)
    chunk_decay = jnp.exp(a_cum[..., -1])

    def step(carry, inp):
        st, dec = inp
        return carry * dec[..., None, None] + st, carry

    init = jnp.zeros((b, h, p, n), X.dtype)
    _, prev = lax.scan(step, init, (jnp.moveaxis(chunk_states, 1, 0), jnp.moveaxis(chunk_decay, 2, 0)))
    prev = jnp.moveaxis(prev, 0, 1)
    y_off = jnp.einsum('bclhn,bchpn,bhcl->bclhp', Cc, prev, jnp.exp(a_cum))
    return (y_diag + y_off).reshape(b, s, h, p)


def ssd_mixer(z, xbc, dt_raw, conv_w, conv_b, dt_bias, a_log, d_skip, norm_w):
    bsz, s, _ = xbc.shape
    f32 = jnp.float32
    xbc = jax.nn.silu(causal_depthwise_conv(xbc, conv_w, conv_b))
    gn = SSD_GROUPS * D_STATE
    xs = xbc[..., :SSD_WIDTH].reshape(bsz, s, SSD_HEADS, SSD_HEAD_DIM).astype(f32)
    bm = xbc[..., SSD_WIDTH:SSD_WIDTH + gn].reshape(bsz, s, SSD_GROUPS, D_STATE)
    cm = xbc[..., SSD_WIDTH + gn:].reshape(bsz, s, SSD_GROUPS, D_STATE)
    rep = SSD_HEADS // SSD_GROUPS
    bm = jnp.repeat(bm, rep, axis=2).astype(f32)
    cm = jnp.repeat(cm, rep, axis=2).astype(f32)
    dt = jax.nn.softplus(dt_raw.astype(f32) + dt_bias.astype(f32))
    a = -jnp.exp(a_log.astype(f32))
    y = ssd_chunked(xs * dt[..., None], dt * a, bm, cm)
    y = y + d_skip.astype(f32)[:, None] * xs
    y = y.reshape(bsz, s, SSD_WIDTH) * jax.nn.silu(z.astype(f32))
    y = y * lax.rsqrt(jnp.mean(jnp.square(y), -1, keepdims=True) + LN_EPS)
    return y.astype(z.dtype) * norm_w


def hier_moe(h, w_rg, b_rg, w_re, b_re, w_gate, w_up, w_down):
    bsz, s, _ = h.shape
    f32 = jnp.float32
    p_group = jax.nn.softmax((h @ w_rg + b_rg).astype(f32), -1)
    g_idx = jnp.argmax(p_group, -1)
    g_prob = jnp.max(p_group, -1)
    e_logits = (h @ w_re + b_re).astype(f32).reshape(bsz, s, N_GROUPS_MOE, EXPERTS_PER_GROUP)
    e_in_group = jnp.take_along_axis(e_logits, g_idx[..., None, None], axis=2)[..., 0, :]
    top_logits, top_idx = lax.top_k(e_in_group, TOP_K_IN_GROUP)
    top_w = jax.nn.softmax(top_logits, -1) * g_prob[..., None]
    expert_id = g_idx[..., None] * EXPERTS_PER_GROUP + top_idx
    gates = jnp.sum(jax.nn.one_hot(expert_id, N_EXPERTS, dtype=f32) * top_w[..., None], axis=-2).astype(h.dtype)
    y = jnp.zeros_like(h)
    for e in range(N_EXPERTS):
        hid = jax.nn.silu(h @ w_gate[e]) * (h @ w_up[e])
        y = y + gates[..., e:e + 1] * (hid @ w_down[e])
    return y


def setup_inputs(seed: int = 0) -> dict:
    key = jax.random.key(seed)
    ks = jax.random.split(key, 21)
    f32 = jnp.float32

    def nrm(k, shape, scale):
        return jax.random.normal(k, shape, f32) * scale

    x = nrm(ks[0], (BATCH, SEQ, D_MODEL), 1.0)
    w_in = nrm(ks[1], (DEPTH, D_MODEL, IN_WIDTH), D_MODEL ** -0.5)
    conv_w = nrm(ks[2], (DEPTH, CONV_WIDTH, CONV_CH), CONV_WIDTH ** -0.5)
    conv_b = nrm(ks[3], (DEPTH, CONV_CH), 0.02)
    dt0 = jnp.exp(jax.random.uniform(ks[4], (DEPTH, SSD_HEADS), f32, math.log(1e-3), math.log(1e-1)))
    dt_bias = dt0 + jnp.log(-jnp.expm1(-dt0))
    a_log = jnp.log(jax.random.uniform(ks[5], (DEPTH, SSD_HEADS), f32, 1.0, 16.0))
    d_skip = 1.0 + nrm(ks[6], (DEPTH, SSD_HEADS), 0.1)
    ssd_norm_w = 1.0 + nrm(ks[7], (DEPTH, SSD_WIDTH), 0.05)
    w_out = nrm(ks[8], (DEPTH, MIX_WIDTH, D_MODEL), (MIX_WIDTH ** -0.5) * BETA)
    ln1_g = 1.0 + nrm(ks[9], (DEPTH, D_MODEL), 0.05)
    ln1_b = nrm(ks[10], (DEPTH, D_MODEL), 0.02)
    w_route_group = nrm(ks[11], (DEPTH, D_MODEL, N_GROUPS_MOE), D_MODEL ** -0.5)
    b_route_group = nrm(ks[12], (DEPTH, N_GROUPS_MOE), 0.01)
    w_route_expert = nrm(ks[13], (DEPTH, D_MODEL, N_EXPERTS), D_MODEL ** -0.5)
    b_route_expert = nrm(ks[14], (DEPTH, N_EXPERTS), 0.01)
    w_gate = nrm(ks[15], (DEPTH, N_EXPERTS, D_MODEL, EXPERT_FF), D_MODEL ** -0.5)
    w_up = nrm(ks[16], (DEPTH, N_EXPERTS, D_MODEL, EXPERT_FF), D_MODEL ** -0.5)
    w_down = nrm(ks[17], (DEPTH, N_EXPERTS, EXPERT_FF, D_MODEL), (EXPERT_FF ** -0.5) * BETA)
    ln2_g = 1.0 + nrm(ks[18], (DEPTH, D_MODEL), 0.05)
    ln2_b = nrm(ks[19], (DEPTH, D_MODEL), 0.02)
    return {'x': x, 'w_in': w_in, 'conv_w': conv_w, 'conv_b': conv_b, 'dt_bias': dt_bias,
            'a_log': a_log, 'd_skip': d_skip, 'ssd_norm_w': ssd_norm_w, 'w_out': w_out,
            'ln1_g': ln1_g, 'ln1_b': ln1_b, 'w_route_group': w_route_group,
            'b_route_group': b_route_group, 'w_route_expert': w_route_expert,
            'b_route_expert': b_route_expert, 'w_gate': w_gate, 'w_up': w_up,
            'w_down': w_down, 'ln2_g': ln2_g, 'ln2_b': ln2_b}


def reference(x, w_in, conv_w, conv_b, dt_bias, a_log, d_skip, ssd_norm_w, w_out, ln1_g, ln1_b,
              w_route_group, b_route_group, w_route_expert, b_route_expert, w_gate, w_up,
              w_down, ln2_g, ln2_b):
    bsz, s, _ = x.shape
    topk = min(TOPK_MAX, s // 4)
    cos, sin = rope_tables(s)
    cos_h, sin_h = cos[:, None, :], sin[:, None, :]
    for l in range(DEPTH):
        proj = jnp.einsum('bsd,de->bse', x, w_in[l])
        q, k, v, iq, ik, iw, z, xbc, dt_raw = jnp.split(proj, IN_SPLIT_POINTS, axis=-1)
        q = apply_rope(q.reshape(bsz, s, ATT_HEADS, HEAD_DIM), cos_h, sin_h)
        k = apply_rope(k, cos, sin)
        iq = apply_rope(iq.reshape(bsz, s, IDX_HEADS, IDX_DIM), cos_h, sin_h)
        ik = apply_rope(ik, cos, sin)
        att = dsa_attention(q, k, v, iq, ik, iw * INDEXER_SCALE, topk)
        ssd = ssd_mixer(z, xbc, dt_raw, conv_w[l], conv_b[l], dt_bias[l], a_log[l],
                        d_skip[l], ssd_norm_w[l])
        mixed = jnp.einsum('bse,ed->bsd', jnp.concatenate([att, ssd], -1), w_out[l])
        x = layer_norm(ALPHA * x + mixed, ln1_g[l], ln1_b[l])
        ffn = hier_moe(x, w_route_group[l], b_route_group[l], w_route_expert[l],
                       b_route_expert[l], w_gate[l], w_up[l], w_down[l])
        x = layer_norm(ALPHA * x + ffn, ln2_g[l], ln2_b[l])
    return x
```

```python
import numpy as np
import concourse.bass as bass
import concourse.mybir as mybir
from concourse.bass_utils import run_bass_kernel_spmd

F32 = mybir.dt.float32
BF16 = mybir.dt.bfloat16
ALU = mybir.AluOpType
AF = mybir.ActivationFunctionType
AX = mybir.AxisListType

NCORES = 8
S = 2048
D = 1024
NT = S // 128
SEQ_PER_CORE = 2
ALPHA = 2.0 ** 0.25
LN_EPS = 1e-5
IDX_SCALE = (4 ** -0.5) * (64 ** -0.5)
NF = 22
TMW = 588
NBIS = 14
NEG = -1.0e30
MASKB = -262144.0


class Prog:
    CE = ("pe", "act", "dve", "pool")

    def __init__(self, nc, kdma=12):
        self.nc = nc
        self.ops = {e: [] for e in ("pe", "act", "dve", "pool", "sp")}
        self.cnt = {e: 0 for e in self.CE}
        self.last_w = {}
        self.readers = {}
        self.seen = {e: {} for e in self.ops}
        self.kdma = kdma
        self.ring = {"sp": [0] * kdma, "pool": [0] * kdma}
        self.ring_next = {"sp": 0, "pool": 0}
        self.nops = 0
        self.floor = {e: {} for e in self.ops}
        self.ps_last = {}

    def barrier(self):
        cur = {e: self.cnt[e] for e in self.CE if self.cnt[e] > 0}
        for q in ("sp", "pool"):
            for k in range(self.kdma):
                if self.ring[q][k] > 0:
                    cur["d%s%d" % (q, k)] = 16 * self.ring[q][k]
        for e in self.floor:
            self.floor[e] = dict(cur)

    def _deps(self, reads, writes):
        deps = {}

        def add(tok):
            if tok is None:
                return
            k, v = tok
            if deps.get(k, 0) < v:
                deps[k] = v
        for r in reads:
            add(self.last_w.get(r))
        for w in writes:
            add(self.last_w.get(w))
            for k, v in self.readers.get(w, {}).items():
                add((k, v))
        return deps

    def _commit(self, tok, reads, writes):
        for r in reads:
            d = self.readers.setdefault(r, {})
            if d.get(tok[0], 0) < tok[1]:
                d[tok[0]] = tok[1]
        for w in writes:
            self.last_w[w] = tok
            self.readers[w] = {}

    def _waits(self, eng, deps):
        waits = []
        fl = self.floor[eng]
        if fl:
            for k, v in fl.items():
                if deps.get(k, 0) < v:
                    deps[k] = v
            self.floor[eng] = {}
        for k, v in deps.items():
            if k == "pe" and eng == "pe":
                continue
            if self.seen[eng].get(k, 0) >= v:
                continue
            self.seen[eng][k] = v
            waits.append((k, v))
        return waits

    def op(self, eng, fn, reads=(), writes=(), inc=True):
        assert eng in self.CE
        if eng != "pe":
            assert inc
        deps = self._deps(reads, writes)
        idx = self.cnt[eng] + 1
        if inc:
            self.cnt[eng] = idx
        tok = (eng, idx)
        for r in reads:
            if isinstance(r, tuple) and r[0] == "ps":
                prev = self.ps_last.get(r[1])
                if prev is not None and prev[0] != eng and deps.get(prev[0], 0) < prev[1]:
                    deps[prev[0]] = prev[1]
                self.ps_last[r[1]] = tok
        waits = self._waits(eng, deps)
        self.ops[eng].append((waits, fn, (eng, 1) if inc else None))
        self._commit(tok, reads, writes)
        self.nops += 1
        return tok

    def dma(self, q, fn, reads=(), writes=()):
        deps = self._deps(reads, writes)
        k = self.ring_next[q] % self.kdma
        self.ring_next[q] += 1
        key = "d%s%d" % (q, k)
        if self.ring[q][k] > 0:
            v = 16 * self.ring[q][k]
            if deps.get(key, 0) < v:
                deps[key] = v
        waits = self._waits(q, deps)
        self.ring[q][k] += 1
        tok = (key, 16 * self.ring[q][k])
        self.ops[q].append((waits, fn, (key, 16)))
        self._commit(tok, reads, writes)
        self.nops += 1
        return tok

    def emit(self):
        nc = self.nc
        names = list(self.CE) + ["d%s%d" % (q, k) for q in ("sp", "pool") for k in range(self.kdma)]
        fin = []
        for q in ("sp", "pool"):
            for k in range(self.kdma):
                if self.ring[q][k] > 0:
                    fin.append(("d%s%d" % (q, k), 16 * self.ring[q][k]))
        for e in self.CE:
            if self.cnt[e] > 0:
                fin.append((e, self.cnt[e]))
        ops = self.ops
        import contextlib
        with contextlib.ExitStack() as st:
            sems = {n: st.enter_context(nc.semaphore("s_" + n)) for n in names}
            block = st.enter_context(nc.Block())

            def replay(eng_name):
                def run(e):
                    for waits, fn, inc in ops[eng_name]:
                        for k, v in waits:
                            e.wait_ge(sems[k], v)
                        ins = fn(e)
                        if inc is not None:
                            ins.then_inc(sems[inc[0]], inc[1])
                    if eng_name == "sp":
                        for k, v in fin:
                            e.wait_ge(sems[k], v)
                return run
            block.sync(replay("sp"))
            block.tensor(replay("pe"))
            block.scalar(replay("act"))
            block.vector(replay("dve"))
            block.gpsimd(replay("pool"))


def _rot(cols):
    return np.concatenate([cols[32:], cols[:32]])


def _layout_w_in(w_in):
    w = w_in[0]
    oq, ok, ov, oiq, oik, oiw, oz, oxbc, odt = 0, 512, 576, 640, 896, 960, 964, 1476, 2244
    tiles = []
    qh = [np.arange(oq + 64 * h, oq + 64 * (h + 1)) for h in range(8)]
    for p in range(4):
        tiles.append(np.concatenate([qh[2 * p], qh[2 * p + 1]]))
    for p in range(4):
        tiles.append(np.concatenate([_rot(qh[2 * p]), _rot(qh[2 * p + 1])]))
    kc = np.arange(ok, ok + 64)
    tiles.append(np.concatenate([kc, kc]))
    tiles.append(np.concatenate([_rot(kc), _rot(kc)]))
    ih = [np.arange(oiq + 64 * h, oiq + 64 * (h + 1)) for h in range(4)]
    for p in range(2):
        tiles.append(np.concatenate([ih[2 * p], ih[2 * p + 1]]))
    for p in range(2):
        tiles.append(np.concatenate([_rot(ih[2 * p]), _rot(ih[2 * p + 1])]))
    ikc = np.arange(oik, oik + 64)
    tiles.append(np.concatenate([ikc, ikc]))
    tiles.append(np.concatenate([_rot(ikc), _rot(ikc)]))
    for t in range(6):
        tiles.append(np.arange(oxbc + 128 * t, oxbc + 128 * (t + 1)))
    fm_cols = np.concatenate(tiles)
    assert fm_cols.shape[0] == NF * 128
    tm_cols = np.concatenate([np.arange(oz, oz + 512), np.arange(ov, ov + 64), np.arange(oiw, oiw + 4),
                              np.arange(odt, odt + 8)])
    assert tm_cols.shape[0] == TMW
    return np.ascontiguousarray(w[:, fm_cols]), np.ascontiguousarray(w[:, tm_cols])


def _consts():
    c = {}
    c["ident"] = np.eye(128, dtype=np.float32)
    s_ = np.arange(128)
    c["tri"] = (s_[:, None] <= s_[None, :]).astype(np.float32)
    c["cneg"] = np.where(s_[None, :] <= s_[:, None], 0.0, NEG).astype(np.float32)
    inv = 10000.0 ** (-np.arange(0, 64, 2, dtype=np.float32) / 64.0)
    ang = np.arange(S, dtype=np.float32)[:, None] * inv[None, :]
    cos, sin = np.cos(ang).astype(np.float32), np.sin(ang).astype(np.float32)
    cosF = np.concatenate([cos, cos], 1).T
    sinS = np.concatenate([-sin, sin], 1).T
    c["cosF"] = np.ascontiguousarray(np.concatenate([cosF, cosF], 0))
    c["sinS"] = np.ascontiguousarray(np.concatenate([sinS, sinS], 0))
    sel = np.zeros((16, 16, 128), np.float32)
    for e in range(16):
        sel[e, e, :] = 1.0
    c["sel"] = sel.reshape(16, 16 * 128)
    c["halfpow"] = np.broadcast_to((0.5 ** np.arange(1, NBIS + 3, dtype=np.float32))[None, :], (128, NBIS + 2)).copy()
    return c


def _rep(v, n=128):
    return np.ascontiguousarray(np.broadcast_to(np.asarray(v, np.float32).reshape(1, -1), (n, np.asarray(v).size)))


def MM(P, out, lhsT, rhs, start, stop, r, w, inc=True):
    return P.op("pe", lambda e: e.matmul(out, lhsT=lhsT, rhs=rhs, start=start, stop=stop), r, w, inc)


def TR(P, out, in_, ident, r, w, inc=True):
    return P.op("pe", lambda e: e.transpose(out, in_, ident), r, w, inc)


def ACT(P, out, in_, func, r, w, bias=0.0, scale=1.0, accum=None):
    if accum is None:
        return P.op("act", lambda e: e.activation(out=out, in_=in_, func=func, bias=bias, scale=scale), r, w)
    return P.op("act", lambda e: e.activation(out=out, in_=in_, func=func, bias=bias, scale=scale, accum_out=accum), r, w)


def TT(P, eng, out, a, b, op, r, w):
    return P.op(eng, lambda e: e.tensor_tensor(out, a, b, op), r, w)


def TS(P, eng, out, a, s1, s2, op0, op1, r, w, accum=None):
    if op1 is None:
        return P.op(eng, lambda e: e.tensor_scalar(out, a, s1, None, op0=op0), r, w)
    if accum is None:
        return P.op(eng, lambda e: e.tensor_scalar(out, a, s1, s2, op0=op0, op1=op1), r, w)
    return P.op(eng, lambda e: e.tensor_scalar(out, a, s1, s2, op0=op0, op1=op1, accum_out=accum), r, w)


def STT(P, eng, out, a, scalar, b, op0, op1, r, w):
    return P.op(eng, lambda e: e.scalar_tensor_tensor(out, a, scalar, b, op0=op0, op1=op1), r, w)


def CP(P, eng, out, in_, r, w):
    if eng == "act":
        return P.op("act", lambda e: e.copy(out, in_), r, w)
    return P.op(eng, lambda e: e.tensor_copy(out, in_), r, w)


def MSET(P, eng, ap, val, r, w):
    return P.op(eng, lambda e: e.memset(ap, val), r, w)


def DMA(P, q, out, in_, r, w):
    return P.dma(q, lambda e: e.dma_start(out=out, in_=in_), r, w)


class Rot:
    def __init__(self, items):
        self.items = list(items)
        self.i = 0

    def next(self):
        v = self.items[self.i % len(self.items)]
        self.i += 1
        return v


class Arena:
    def __init__(self, nc, lo=16512, hi=229344):
        self.nc = nc
        self.free_list = [(lo, hi)]
        self.live = {}
        self.uid = 0
        self.cache = {}

    def alloc(self, name, shape, dt):
        n = 1
        for d in shape[1:]:
            n *= d
        size = n * (4 if dt == F32 else 2)
        size = (size + 31) // 32 * 32
        for i, (a, b) in enumerate(self.free_list):
            if b - a >= size:
                self.free_list[i] = (a + size, b)
                if a + size == b:
                    self.free_list.pop(i)
                key = (name, tuple(shape), str(dt), a)
                h = self.cache.get(key)
                if h is None:
                    self.uid += 1
                    h = self.nc.alloc_sbuf_tensor_at("sb%d_%s" % (self.uid, name), list(shape), dt, offset=a)
                    self.cache[key] = h
                self.live[id(h)] = (a, size, h)
                return h
        raise RuntimeError("arena out of SBUF for %s (%d bytes); free=%s" % (name, size, self.free_list))

    def free(self, *hs):
        for h in hs:
            a, size, _ = self.live.pop(id(h))
            self.free_list.append((a, a + size))
        self.free_list.sort()
        merged = []
        for a, b in self.free_list:
            if merged and merged[-1][1] == a:
                merged[-1] = (merged[-1][0], b)
            else:
                merged.append((a, b))
        self.free_list = merged


def build(dbg=(), nseq=SEQ_PER_CORE, stop_after=None):
    nc = bass.Bass("TRN2", target_bir_lowering=False)
    P = Prog(nc)
    A = Arena(nc)
    dbg = set(dbg)
    dumps = {}

    def din(name, shape):
        return nc.dram_tensor(name, list(shape), F32, kind="ExternalInput").ap()

    x_d = din("x", [SEQ_PER_CORE, S, D])
    wfm_d = din("wfm", [D, NF * 128])
    wtm_d = din("wtm", [D, TMW])
    ident_d = din("ident", [128, 128])
    tri_d = din("tri", [128, 128])
    cneg_d = din("cneg", [128, 128])
    cosF_d = din("cosF", [128, S])
    sinS_d = din("sinS", [128, S])
    sel_d = din("sel", [16, 16 * 128])
    halfpow_d = din("halfpow", [128, NBIS + 2])
    convw_d = din("convw", [128, 24])
    convb_d = din("convb", [128, 6])
    dtb_d = din("dtb", [128, 8])
    alog_d = din("alog", [128, 8])
    dskip_d = din("dskip", [128, 512])
    nw_d = din("nw", [128, 512])
    nwc_d = din("nwc", [128, 4])
    wout_d = din("wout", [D, D])
    ln1g_d = din("ln1g", [128, D])
    ln1b_d = din("ln1b", [128, D])
    ln2g_d = din("ln2g", [128, D])
    ln2b_d = din("ln2b", [128, D])
    wr_d = din("wr", [D, 20])
    br_d = din("br", [128, 20])
    wg_d = din("wg", [16, D, 256])
    wu_d = din("wu", [16, D, 256])
    wd_d = din("wd", [16, 256, D])
    out_d = nc.dram_tensor("out", [SEQ_PER_CORE, S, D], F32, kind="ExternalOutput").ap()
    hscr_d = nc.dram_tensor("hscr", [SEQ_PER_CORE, S, D], F32, kind="Internal").ap()

    import contextlib
    with contextlib.ExitStack() as es:
        ps = [es.enter_context(nc.psum_tensor("ps%d" % i, [128, 512], F32)) for i in range(8)]

        def psf(b):
            return ps[b][:]

        def psb(b):
            return ps[b][:].bitcast(BF16)

        def PSR(b):
            return ("ps", b)

        def dump(name, ap, shape, reads, dt=None):
            if name not in dbg:
                return
            t = nc.dram_tensor("dbg_" + name, list(shape), dt or ap.dtype, kind="ExternalOutput").ap()
            dumps[name] = t
            DMA(P, "sp", t, ap, reads, [])

        ident_f = A.alloc("ident_f", [128, 128], F32)
        ident_b = A.alloc("ident_b", [128, 128], BF16)
        tri_f = A.alloc("tri_f", [128, 128], F32)
        tri_b = A.alloc("tri_b", [128, 128], BF16)
        cneg = A.alloc("cneg", [128, 128], F32)
        ones_f = A.alloc("ones_f", [128, 128], F32)
        DMA(P, "sp", ident_f[:], ident_d, [], ["ident_f"])
        DMA(P, "pool", ident_b[:], ident_d, [], ["ident_b"])
        DMA(P, "sp", tri_f[:], tri_d, [], ["tri_f"])
        DMA(P, "pool", tri_b[:], tri_d, [], ["tri_b"])
        DMA(P, "sp", cneg[:], cneg_d, [], ["cneg"])
        MSET(P, "dve", ones_f[:], 1.0, [], ["ones_f"])

        for seq in range(nseq):
            qT = A.alloc("qz", [128, 8, S], BF16)
            kT = A.alloc("kT", [128, S], BF16)
            iqT = A.alloc("iqz", [128, 4, S], BF16)
            ikT = A.alloc("ikT", [128, S], BF16)
            Vext = A.alloc("Vext", [128, NT, 192], BF16)
            iws = A.alloc("iws", [128, NT, 4], F32)
            xbcT = A.alloc("xbcT", [128, 6, S], BF16)
            siluz = A.alloc("siluz", [128, NT, 512], BF16)
            dtr = A.alloc("dtr", [128, NT, 8], F32)

            xT = A.alloc("xT", [128, 8, S], BF16)
            wtm = A.alloc("wtm", [128, 8, TMW], BF16)
            xb = [A.alloc("xb%d" % i, [128, D], BF16) for i in range(4)]
            for c in range(8):
                DMA(P, "pool", wtm[:, c, :], wtm_d[c * 128:(c + 1) * 128, :], [], [("wtm", c)])
            MSET(P, "pool", qT[:], 0.0, [], ["qz0"])
            MSET(P, "pool", iqT[:], 0.0, [], ["iqz0"])
            MSET(P, "pool", Vext[:, :, 64:128], 0.0, [], [("Vext0",)])
            MSET(P, "pool", Vext[:, :, 64:65], 1.0, [("Vext0",)], [("Vext0",)])
            tr_banks = Rot([0, 1])
            fm_banks = Rot([4, 5, 6, 7])
            cp_eng = Rot(["act", "dve"])
            def a_dma(t):
                tok = slice(t * 128, (t + 1) * 128)
                DMA(P, "pool", xb[t % 4][:], x_d[seq, tok, :], [], [("xb", t % 4)])

            def a_tr(t):
                tok = slice(t * 128, (t + 1) * 128)
                xbuf = xb[t % 4]
                b = tr_banks.next()
                pv = psb(b)
                for c in range(8):
                    TR(P, pv[:, c * 128:(c + 1) * 128], xbuf[:, c * 128:(c + 1) * 128], ident_b[:],
                       [("xb", t % 4), "ident_b"], [PSR(b)], inc=(c == 7))
                CP(P, cp_eng.next(), xT[:, :, tok], pv.rearrange("p (c t) -> p c t", c=8),
                   [PSR(b)], [("xT", t)])

            def a_mm(t):
                tok = slice(t * 128, (t + 1) * 128)
                bz, bv = (2, 3) if t % 2 == 0 else (4, 5)
                for c in range(8):
                    MM(P, psf(bz), xT[:, c, tok], wtm[:, c, 0:512], c == 0, c == 7,
                       [("xT", t), ("wtm", c)], [PSR(bz)], inc=(c == 7))
                for c in range(8):
                    MM(P, ps[bv][:, 0:76], xT[:, c, tok], wtm[:, c, 512:588], c == 0, c == 7,
                       [("xT", t), ("wtm", c)], [PSR(bv)], inc=(c == 7))
                ACT(P, siluz[:, t, :], psf(bz), AF.Silu, [PSR(bz)], [("siluz", t)])
                CP(P, "dve", Vext[:, t, 0:64], ps[bv][:, 0:64], [PSR(bv)], [("Vext", t)])
                CP(P, "dve", Vext[:, t, 128:192], ps[bv][:, 0:64], [PSR(bv)], [("Vext", t)])
                TS(P, "dve", iws[:, t, :], ps[bv][:, 64:68], IDX_SCALE, None, ALU.mult, None,
                   [PSR(bv)], [("iws", t)])
                CP(P, "dve", dtr[:, t, :], ps[bv][:, 68:76], [PSR(bv)], [("dtr", t)])

            for t in range(3):
                a_dma(t)
            a_tr(0)
            for t in range(NT):
                if t + 3 < NT:
                    a_dma(t + 3)
                if t + 1 < NT:
                    a_tr(t + 1)
                a_mm(t)

            P.barrier()
            A.free(wtm, *xb)
            cosF = A.alloc("cosF", [128, S], F32)
            sinS = A.alloc("sinS", [128, S], F32)
            convw = A.alloc("convw", [128, 24], F32)
            convb = A.alloc("convb", [128, 6], F32)
            diagw = A.alloc("diagw", [128, 24, 128], BF16)
            uT = A.alloc("uT", [128, 6, S + 3], BF16)
            rt1 = [A.alloc("rt1_%d" % i, [128, 512], F32) for i in range(2)]
            rt2 = [A.alloc("rt2_%d" % i, [128, 512], F32) for i in range(2)]
            wsl = [A.alloc("wsl%d" % i, [128, 8, 128], BF16) for i in range(4)]
            p1_tmp = [xT, cosF, sinS, convw, convb, diagw, uT] + rt1 + rt2 + wsl
            DMA(P, "sp", cosF[:], cosF_d, [], ["cosF"])
            DMA(P, "sp", sinS[:], sinS_d, [], ["sinS"])
            DMA(P, "sp", convw[:], convw_d, [], ["convw"])
            DMA(P, "sp", convb[:], convb_d, [], ["convb"])
            for i in range(24):
                TS(P, "dve", diagw[:, i, :], ident_f[:], convw[:, i:i + 1], None, ALU.mult, None,
                   ["ident_f", "convw"], [("diagw", i)])
            MSET(P, "pool", uT[:, :, 0:3], 0.0, [], [("uT", -1)])
            wsi = [0]

            worder = [0, 4, 1, 5, 2, 6, 3, 7, 8, 9, 10, 12, 11, 13, 14, 15, 16, 17, 18, 19, 20, 21]
            wslot = {}

            def prefetch(n):
                for _ in range(n):
                    if wsi[0] >= len(worder):
                        return
                    f = worder[wsi[0]]
                    k = wsi[0] % 4
                    wsi[0] += 1
                    wslot[f] = k
                    DMA(P, "pool", wsl[k][:], wfm_d[:, f * 128:(f + 1) * 128].rearrange("(c p) f -> p c f", p=128),
                        [], [("wsl", k)])

            def load_w(f):
                return wslot[f]

            prefetch(4)

            def fm_proj(k, tg, b):
                tokg = slice(tg * 512, tg * 512 + 512)
                for c in range(8):
                    MM(P, psf(b), wsl[k][:, c, :], xT[:, c, tokg], c == 0, c == 7,
                       [("xT", tg * 4 + i) for i in range(4)] + [("wsl", k)], [PSR(b)], inc=(c == 7))

            pairs = [(0, 4, (qT, 0), "qT0"), (1, 5, (qT, 1), "qT1"), (2, 6, (qT, 2), "qT2"), (3, 7, (qT, 3), "qT3"),
                     (8, 9, kT, "kT"), (10, 12, (iqT, 0), "iqT0"), (11, 13, (iqT, 1), "iqT1"), (14, 15, ikT, "ikT")]
            rti = 0
            for fa, fr, dstf, dname in pairs:
                ka, kr = load_w(fa), load_w(fr)
                for tg in range(4):
                    tokg = slice(tg * 512, tg * 512 + 512)
                    ba, br = fm_banks.next(), fm_banks.next()
                    fm_proj(ka, tg, ba)
                    fm_proj(kr, tg, br)
                    r1, r2 = rt1[rti % 2], rt2[rti % 2]
                    TT(P, "dve", r1[:], psf(ba), cosF[:, tokg], ALU.mult, [PSR(ba), "cosF"], [("rt1", rti % 2)])
                    TT(P, "dve", r2[:], psf(br), sinS[:, tokg], ALU.mult, [PSR(br), "sinS"], [("rt2", rti % 2)])
                    rr_ = [("rt1", rti % 2), ("rt2", rti % 2), "qz0", "iqz0"]
                    if isinstance(dstf, tuple):
                        dt_, pp = dstf
                        TT(P, "pool", dt_[0:64, 2 * pp, tokg], r1[0:64, :], r2[0:64, :], ALU.add, rr_, [(dname, tg, 0)])
                        TT(P, "pool", dt_[64:128, 2 * pp + 1, tokg], r1[64:128, :], r2[64:128, :], ALU.add, rr_,
                           [(dname, tg, 1)])
                    else:
                        TT(P, "pool", dstf[:, tokg], r1[:], r2[:], ALU.add, rr_, [(dname, tg)])
                    rti += 1
                prefetch(2)
            for ct in range(6):
                k = load_w(16 + ct)
                for tg in range(4):
                    t0 = tg * 512
                    b = fm_banks.next()
                    fm_proj(k, tg, b)
                    CP(P, "act", uT[:, ct, 3 + t0:3 + t0 + 512], psf(b), [PSR(b)], [("uT", ct, tg)])
                prefetch(1)
                for tg in range(4):
                    t0 = tg * 512
                    b = fm_banks.next()
                    rr = [("uT", ct, tg), ("uT", -1)] + ([("uT", ct, tg - 1)] if tg > 0 else [])
                    for j in range(4):
                        MM(P, psf(b), diagw[:, ct * 4 + j, :], uT[:, ct, t0 + j:t0 + j + 512], j == 0, j == 3,
                           rr + [("diagw", ct * 4 + j)], [PSR(b)], inc=(j == 3))
                    ACT(P, xbcT[:, ct, t0:t0 + 512], psf(b), AF.Silu, [PSR(b), "convb"], [("xbcT", ct, tg)],
                        bias=convb[:, ct:ct + 1])
            g4 = range(4)
            P.barrier()
            dump("qz", qT[:], [128, 8, S], [])
            dump("iqz", iqT[:], [128, 4, S], [])
            A.free(*p1_tmp)
            if stop_after == 1:
                A.free(qT, kT, iqT, ikT, Vext, iws, xbcT, siluz, dtr)
                continue

            mixT = A.alloc("mixT", [128, 8, S], BF16)
            dtb = A.alloc("dtb", [128, 8], F32)
            arep = A.alloc("arep", [128, 8], F32)
            dskip = A.alloc("dskip", [128, 512], F32)
            nwr = A.alloc("nwr", [128, 512], F32)
            dt_all = A.alloc("dt_all", [128, NT, 8], F32)
            A_all = A.alloc("A_all", [128, NT, 8], F32)
            acum = A.alloc("acum", [128, NT, 8], F32)
            tot = A.alloc("tot", [128, NT, 8], F32)
            ds_all = A.alloc("ds_all", [128, NT, 8], F32)
            cdec = A.alloc("cdec", [128, NT, 8], F32)
            state = A.alloc("state", [128, 8, 64], F32)
            NB = 2
            xdt = [A.alloc("xdt%d" % i, [128, 8, 64], BF16) for i in range(NB)]
            xdtd = [A.alloc("xdtd%d" % i, [128, 8, 64], BF16) for i in range(NB)]
            dsk = [A.alloc("dsk%d" % i, [128, 512], F32) for i in range(NB)]
            Btok = [A.alloc("Btok%d" % i, [128, 128], BF16) for i in range(NB)]
            Gm = [A.alloc("Gm%d" % i, [128, 2, 128], BF16) for i in range(NB)]
            Ab = [A.alloc("Ab%d" % i, [128, 2, 8, 128], BF16) for i in range(NB)]
            A_hl = A.alloc("A_hl", [128, 2, NT, 8], BF16)
            A_res = A.alloc("A_res", [128, NT, 8], F32)
            Dm = [A.alloc("Dm%d" % i, [128, 8, 128], F32) for i in range(NB)]
            Lm = [A.alloc("Lm%d" % i, [128, 8, 128], BF16) for i in range(NB)]
            Eb = [A.alloc("Eb%d" % i, [128, 8, 128], BF16) for i in range(NB)]
            MT = [A.alloc("MT%d" % i, [128, 8, 128], BF16) for i in range(NB)]
            Cp = [A.alloc("Cp%d" % i, [128, 8, 128], BF16) for i in range(NB)]
            CTz = [A.alloc("CTz%d" % i, [128, 2, 128], BF16) for i in range(NB)]
            prevb = [A.alloc("prevb%d" % i, [128, 8, 64], BF16) for i in range(NB)]
            y1 = [A.alloc("y1_%d" % i, [128, 512], F32) for i in range(NB)]
            y2 = [A.alloc("y2_%d" % i, [128, 512], F32) for i in range(NB)]
            yo = [A.alloc("yo%d" % i, [128, 512], BF16) for i in range(NB)]
            junk = [A.alloc("junk%d" % i, [128, 512], BF16) for i in range(NB)]
            ms = [A.alloc("ms%d" % i, [128, 2], F32) for i in range(NB)]
            p3_tmp = ([A_hl, A_res, dtb, arep, dskip, nwr, dt_all, A_all, acum, tot, ds_all, cdec, state] + xdt + xdtd + dsk + Btok + Gm
                      + Ab + Dm + Lm + Eb + MT + Cp + CTz + prevb + y1 + y2 + yo + junk + ms)

            for i in range(NB):
                MSET(P, "pool", Cp[i][:], 0.0, [], [("Cp", i, 0), ("Cp", i, 1)])
                MSET(P, "pool", CTz[i][:], 0.0, [], [("CTz", i)])
            DMA(P, "sp", dtb[:], dtb_d, [], ["dtb"])
            DMA(P, "sp", arep[:], alog_d, [], ["arep"])
            DMA(P, "sp", dskip[:], dskip_d, [], ["dskip"])
            DMA(P, "sp", nwr[:], nw_d, [], ["nwr"])
            ACT(P, arep[:], arep[:], AF.Exp, ["arep"], ["arep"])
            TS(P, "dve", arep[:], arep[:], -1.0, None, ALU.mult, None, ["arep"], ["arep"])
            alldtr = [("dtr", t) for t in range(NT)]
            TT(P, "dve", dt_all[:], dtr[:], dtb[:].unsqueeze(1).to_broadcast([128, NT, 8]), ALU.add,
               alldtr + ["dtb"], ["dt_all"])
            ACT(P, dt_all[:], dt_all[:], AF.Exp, ["dt_all"], ["dt_all"])
            ACT(P, dt_all[:], dt_all[:], AF.Ln, ["dt_all"], ["dt_all"], bias=1.0)
            TT(P, "dve", A_all[:], dt_all[:], arep[:].unsqueeze(1).to_broadcast([128, NT, 8]), ALU.mult,
               ["dt_all", "arep"], ["A_all"])
            CP(P, "dve", A_hl[:, 0, :, :], A_all[:], ["A_all"], ["A_hl0"])
            TT(P, "dve", A_res[:], A_all[:], A_hl[:, 0, :, :], ALU.subtract, ["A_all", "A_hl0"], ["A_res"])
            CP(P, "dve", A_hl[:, 1, :, :], A_res[:], ["A_res"], ["A_hl1"])
            A2 = A_all[:].rearrange("p c h -> p (c h)")
            MM(P, ps[0][:, 0:128], tri_f[:], A2, True, True, ["tri_f", "A_all"], [PSR(0)])
            MM(P, ps[1][:, 0:128], ones_f[:], A2, True, True, ["ones_f", "A_all"], [PSR(1)])
            CP(P, "dve", acum[:].rearrange("p c h -> p (c h)"), ps[0][:, 0:128], [PSR(0)], ["acum"])
            CP(P, "dve", tot[:].rearrange("p c h -> p (c h)"), ps[1][:, 0:128], [PSR(1)], ["tot"])
            TT(P, "dve", ds_all[:], tot[:], acum[:], ALU.subtract, ["tot", "acum"], ["ds_all"])
            ACT(P, ds_all[:], ds_all[:], AF.Exp, ["ds_all"], ["ds_all"])
            ACT(P, cdec[:], tot[:], AF.Exp, ["tot"], ["cdec"])
            trb = Rot([0, 1])
            def ssd_s1(c):
                    i = c % NB
                    tok = slice(c * 128, (c + 1) * 128)
                    g4 = c // 4
                    xres = [("xbcT", ct, g4) for ct in range(6)]
                    CP(P, "dve", Ab[i][:], A_hl[:, :, c, :].unsqueeze(3).to_broadcast([128, 2, 8, 128]), ["A_hl0", "A_hl1"],
                       [("Ab", i)])
                    for h in range(8):
                        bb = 3 + h // 4
                        MM(P, ps[bb][:, (h % 4) * 128:(h % 4 + 1) * 128], Ab[i][:, 0, h, :], tri_b[:], True, False,
                           [("Ab", i), "tri_b"], [PSR(bb)], inc=False)
                        MM(P, ps[bb][:, (h % 4) * 128:(h % 4 + 1) * 128], Ab[i][:, 1, h, :], tri_b[:], False, True,
                           [("Ab", i), "tri_b"], [PSR(bb)], inc=(h % 4 == 3))
                    b = trb.next()
                    pv = psb(b)
                    for ct in range(5):
                        TR(P, pv[:, ct * 128:(ct + 1) * 128], xbcT[:, ct, tok], ident_b[:], xres + ["ident_b"], [PSR(b)],
                           inc=(ct == 4))
                    xs_ps = pv[:, 0:512].rearrange("p (h d) -> p h d", h=8)
                    TT(P, "dve", xdt[i][:], xs_ps, dt_all[:, c, :].unsqueeze(2).to_broadcast([128, 8, 64]), ALU.mult,
                       [PSR(b), "dt_all"], [("xdt", i)])
                    TT(P, "dve", dsk[i][:], pv[:, 0:512], dskip[:], ALU.mult, [PSR(b), "dskip"], [("dsk", i)])
                    CP(P, "act", Btok[i][:], pv[:, 512:640], [PSR(b)], [("Btok", i)])
                    TT(P, "dve", xdtd[i][:], xdt[i][:], ds_all[:, c, :].unsqueeze(2).to_broadcast([128, 8, 64]), ALU.mult,
                       [("xdt", i), "ds_all"], [("xdtd", i)])
                    for g in range(2):
                        CP(P, "pool", CTz[i][64 * g:64 * g + 64, g, :], xbcT[64 * g:64 * g + 64, 5, tok], xres, [("CTz", i)])
                    for g in range(2):
                        MM(P, ps[2][:, g * 128:(g + 1) * 128], xbcT[:, 4, tok], CTz[i][:, g, :],
                           True, True, xres + [("CTz", i)], [PSR(2)], inc=(g == 1))
                    TT(P, "dve", Gm[i][:], ps[2][:, 0:256].rearrange("p (g l) -> p g l", g=2),
                       tri_f[:].unsqueeze(1).to_broadcast([128, 2, 128]), ALU.mult, [PSR(2), "tri_f"], [("Gm", i)])
                    for g in range(2):
                        TT(P, "dve", Dm[i][:, 4 * g:4 * g + 4, :], ps[3 + g][:].rearrange("p (h l) -> p h l", h=4),
                           acum[:, c, 4 * g:4 * g + 4].unsqueeze(2).to_broadcast([128, 4, 128]), ALU.subtract,
                           [PSR(3 + g), "acum"], [("Dm", i)])
                    ACT(P, Dm[i][:], Dm[i][:], AF.Relu, [("Dm", i)], [("Dm", i)], scale=-1.0)
                    ACT(P, Lm[i][:], Dm[i][:], AF.Exp, [("Dm", i)], [("Lm", i)], scale=-1.0)
                    for g in range(2):
                        ACT(P, Eb[i][:, g * 4:(g + 1) * 4, :], ps[3 + g][:].rearrange("p (h l) -> p h l", h=4), AF.Exp,
                            [PSR(3 + g)], [("Eb", i, g)])
                    for g in range(2):
                        TT(P, "dve", MT[i][:, g * 4:(g + 1) * 4, :], Lm[i][:, g * 4:(g + 1) * 4, :],
                           Gm[i][:, g, :].unsqueeze(1).to_broadcast([128, 4, 128]), ALU.mult,
                           [("Lm", i), ("Gm", i)], [("MT", i, g)])
                        TT(P, "dve", Cp[i][64 * g:64 * g + 64, g * 4:(g + 1) * 4, :], Eb[i][64 * g:64 * g + 64, g * 4:(g + 1) * 4, :],
                           xbcT[64 * g:64 * g + 64, 5, tok].unsqueeze(1).to_broadcast([64, 4, 128]), ALU.mult,
                           [("Eb", i, g)] + xres, [("Cp", i, g)])

            def ssd_s2(c):
                    i = c % NB
                    tok = slice(c * 128, (c + 1) * 128)
                    g4 = c // 4
                    xres = [("xbcT", ct, g4) for ct in range(6)]
                    if c > 0:
                        CP(P, "pool", prevb[i][:], state[:], ["state"], [("prevb", i)])
                    for h in range(8):
                        g = h // 4
                        MM(P, ps[5][:, h * 64:(h + 1) * 64], MT[i][:, h, :], xdt[i][:, h, :], True, c == 0,
                           [("MT", i, g), ("xdt", i)], [PSR(5)], inc=(c == 0 and h == 7))
                        if c > 0:
                            MM(P, ps[5][:, h * 64:(h + 1) * 64], Cp[i][:, h, :],
                               prevb[i][:, h, :], False, True, [("Cp", i, g), ("prevb", i)], [PSR(5)],
                               inc=(h == 7))
                    if c < NT - 1:
                        for h in range(8):
                            MM(P, ps[6][:, h * 64:(h + 1) * 64], Btok[i][:], xdtd[i][:, h, :], True, True,
                               [("Btok", i), ("xdtd", i)], [PSR(6)], inc=(h == 7))
                        st2 = state[:].rearrange("p h d -> p (h d)")
                        if c == 0:
                            CP(P, "dve", st2, psf(6), [PSR(6)], ["state"])
                        else:
                            TT(P, "dve", state[:], state[:], cdec[:, c, :].unsqueeze(2).to_broadcast([128, 8, 64]), ALU.mult,
                               ["state", "cdec", ("prevb", i)], ["state"])
                            TT(P, "dve", st2, st2, psf(6), ALU.add, ["state", PSR(6)], ["state"])
                    TT(P, "dve", y1[i][:], psf(5), dsk[i][:], ALU.add, [PSR(5), ("dsk", i)], [("y1", i)])
                    TT(P, "pool", y2[i][:], y1[i][:], siluz[:, c, :], ALU.mult, [("y1", i), ("siluz", c)], [("y2", i)])
                    ACT(P, junk[i][:], y2[i][:], AF.Square, [("y2", i)], [("junk", i), ("ms", i)], scale=float(512 ** -0.5),
                        accum=ms[i][:, 0:1])
                    ACT(P, ms[i][:, 1:2], ms[i][:, 0:1], AF.Ln, [("ms", i)], [("ms2", i)], bias=LN_EPS)
                    ACT(P, ms[i][:, 1:2], ms[i][:, 1:2], AF.Exp, [("ms2", i)], [("ms2", i)], scale=-0.5)

            def ssd_s3(c):
                    i = c % NB
                    tok = slice(c * 128, (c + 1) * 128)
                    ACT(P, yo[i][:], y2[i][:], AF.Copy, [("y2", i), ("ms2", i)], [("yo", i)], scale=ms[i][:, 1:2])
                    pv7 = psb(7)
                    for k in range(4):
                        TR(P, pv7[:, k * 128:(k + 1) * 128], yo[i][:, k * 128:(k + 1) * 128], ident_b[:], [("yo", i), "ident_b"],
                           [PSR(7)], inc=(k == 3))
                    CP(P, "act", mixT[:, 4:8, tok], pv7[:, 0:512].rearrange("p (k t) -> p k t", k=4), [PSR(7)], [("mixT", "s", c)])


            ssd_s1(0)
            for c in range(NT):
                if c > 0:
                    ssd_s3(c - 1)
                if c + 1 < NT:
                    ssd_s1(c + 1)
                ssd_s2(c)
            ssd_s3(NT - 1)
            dump("ssdT", mixT[:, 4:8, :], [128, 4, S], [("mixT", "s", c) for c in range(NT)])
            P.barrier()
            A.free(*p3_tmp)
            A.free(xbcT, siluz, dtr)
            if stop_after == 3:
                A.free(mixT, qT, kT, iqT, ikT, Vext, iws)
                continue

            I4 = A.alloc("I4", [128, 4, S], F32)
            junkb = A.alloc("junkb", [128, S], BF16)
            selb = [A.alloc("selb%d" % i, [128, S], BF16) for i in range(2)]
            Rt = [A.alloc("Rt%d" % i, [128, 512], F32) for i in range(4)]
            maskT = [A.alloc("maskT%d" % i, [128, NT, 512], BF16) for i in range(2)]
            Pexp = [A.alloc("Pexp%d" % i, [128, 512], BF16) for i in range(3)]
            Pm = []
            Osb = [A.alloc("Osb%d" % i, [128, 512], F32) for i in range(3)]
            rec = []
            Sden = A.alloc("Sden", [128, 2, 128], F32)
            halfpow = A.alloc("halfpow", [128, NBIS + 2], F32)
            aw = A.alloc("aw", [128, 4, 4], F32)
            sg = A.alloc("sg", [128, 4, 4], F32)
            lo0 = A.alloc("lo0", [128, 4], F32)
            hi0 = A.alloc("hi0", [128, 4], F32)
            wd = A.alloc("wd", [128, 4, NBIS + 2], F32)
            mid = A.alloc("mid", [128, 4], F32)
            cnt = A.alloc("cnt", [128, 4], F32)
            sacc = A.alloc("sacc", [128, 2], F32)
            lhalf = A.alloc("lhalf", [128, 2], F32)
            junka = A.alloc("junka", [128, S], BF16)
            u2 = A.alloc("u2", [128, 4], F32)
            tq = A.alloc("tq", [128, 4], F32)
            thr = A.alloc("thr", [128, 4], F32)
            p2_tmp = maskT + [sacc, lhalf, junka, I4, junkb, Sden, halfpow, aw, sg, lo0, hi0, wd, mid, cnt, u2, tq, thr] + selb + Rt + Pexp + Pm + Osb + rec
            DMA(P, "sp", halfpow[:], halfpow_d, [], ["halfpow"])
            MSET(P, "pool", Sden[:], 0.0, [], ["Sden"])
            MSET(P, "pool", Sden[64:65, 0, :], 1.0, ["Sden"], ["Sden"])
            MSET(P, "pool", Sden[0:1, 1, :], 1.0, ["Sden"], ["Sden"])
            ibank = Rot([0, 1, 2])
            oi_box = [0]

            def gen_IB(g):
                mT = maskT[g % 2]
                nkb = g + 1
                Ls = []
                for b in range(4):
                    qi = 4 * g + b
                    L = (qi + 1) * 128
                    Ls.append(L)
                    qtok = slice(qi * 128, (qi + 1) * 128)
                    STT(P, "dve", aw[:, b, :], iws[:, qi, :], -1.0, iws[:, qi, :], ALU.mult, ALU.max, [], [("aw", b)])
                    TS(P, "dve", sg[:, b, :], iws[:, qi, :], 0.0, 2.0, ALU.is_ge, ALU.mult, [], [("sg", b)])
                    TS(P, "dve", sg[:, b, :], sg[:, b, :], -1.0, None, ALU.add, None, [("sg", b)], [("sg", b)])
                    for kb in range(nkb):
                        w = min(512, L - kb * 512)
                        ks = slice(kb * 512, kb * 512 + w)
                        for h in range(4):
                            bk = ibank.next()
                            MM(P, ps[bk][:, 0:w], iqT[:, h, qtok], ikT[:, ks], True, True, [], [PSR(bk)])
                            ACT(P, Rt[h][:, 0:w], ps[bk][:, 0:w], AF.Relu, [PSR(bk), ("aw", b)], [("Rt", h)],
                                scale=aw[:, b, h:h + 1])
                        TS(P, "dve", I4[:, b, ks], Rt[0][:, 0:w], sg[:, b, 0:1], None, ALU.mult, None,
                           [("Rt", 0), ("sg", b)], [("I4", b)])
                        for h in range(1, 4):
                            STT(P, "dve", I4[:, b, ks], Rt[h][:, 0:w], sg[:, b, h:h + 1], I4[:, b, ks], ALU.mult, ALU.add,
                                [("Rt", h), ("sg", b), ("I4", b)], [("I4", b)])
                        yield 3.0 * w / 512 + 0.5
                    P.op("dve", lambda e, o=lo0[:, b:b + 1], i_=I4[:, b, 0:L]: e.tensor_reduce(o, i_, axis=AX.X, op=ALU.min),
                         [("I4", b)], [("lo0", b)])
                    TT(P, "dve", I4[:, b, qi * 128:(qi + 1) * 128], I4[:, b, qi * 128:(qi + 1) * 128], cneg[:], ALU.add,
                       [("I4", b), ("lo0", b), "cneg"], [("I4", b)])
                    P.op("dve", lambda e, o=hi0[:, b:b + 1], i_=I4[:, b, 0:L]: e.tensor_reduce(o, i_, axis=AX.X, op=ALU.max),
                         [("I4", b)], [("hi0", b)])
                    yield 2.2 * L / 1024 + 0.6
                allb = [("lo0", b) for b in range(4)] + [("hi0", b) for b in range(4)]
                TT(P, "dve", tq[:], hi0[:], lo0[:], ALU.subtract, allb, ["tq"])
                TT(P, "dve", wd[:], tq[:].unsqueeze(2).to_broadcast([128, 4, NBIS + 2]),
                   halfpow[:].unsqueeze(1).to_broadcast([128, 4, NBIS + 2]), ALU.mult, ["tq", "halfpow"], ["wd"])
                TT(P, "dve", mid[:], lo0[:], wd[:, :, 0], ALU.add, allb + ["wd"], ["mid"])
                yield 1.0
                MSET(P, "pool", lhalf[:, 0:1], Ls[2] / 2.0, [], ["lhalf"])
                MSET(P, "pool", lhalf[:, 1:2], Ls[3] / 2.0, [], ["lhalf"])
                for k in range(NBIS):
                    for b in (2, 3):
                        ACT(P, junka[:, 0:Ls[b]], I4[:, b, 0:Ls[b]], AF.Sign, [("I4", b), "mid"], ["junka", ("sacc", b)],
                            bias=mid[:, b:b + 1], scale=-1.0, accum=sacc[:, b - 2:b - 1])
                    for b in (0, 1):
                        TS(P, "dve", junkb[:, 0:Ls[b]], I4[:, b, 0:Ls[b]], mid[:, b:b + 1], 0.0, ALU.is_ge, ALU.add,
                           [("I4", b), "mid"], ["junkb", ("cnt", b)], accum=cnt[:, b:b + 1])
                    STT(P, "dve", cnt[:, 2:4], sacc[:, 0:2], -0.5, lhalf[:, 0:2], ALU.mult, ALU.add,
                        [("sacc", 2), ("sacc", 3), "lhalf"], [("cnt", 2)])
                    TS(P, "dve", u2[:], cnt[:], 256.0, 2.0, ALU.is_ge, ALU.mult, [("cnt", 0), ("cnt", 1), ("cnt", 2)], ["u2"])
                    STT(P, "dve", tq[:], u2[:], -1.0, wd[:, :, k + 1], ALU.add, ALU.mult, ["u2", "wd"], ["tq"])
                    TT(P, "dve", mid[:], mid[:], tq[:], ALU.add, ["mid", "tq"], ["mid"])
                    yield (Ls[0] + Ls[1]) / 960.0 + 1.2
                TT(P, "dve", thr[:], mid[:], wd[:, :, NBIS - 2], ALU.subtract, ["mid", "wd"], ["thr"])
                for b in range(4):
                    qi = 4 * g + b
                    sb_ = selb[b % 2]
                    TS(P, "dve", sb_[:, 0:Ls[b]], I4[:, b, 0:Ls[b]], thr[:, b:b + 1], MASKB, ALU.is_lt, ALU.mult,
                       [("I4", b), "thr"], [("selb", b % 2)])
                    for c0 in range(0, qi + 1, 8):
                        n = min(8, qi + 1 - c0)
                        pv = psb(3)
                        for j in range(n):
                            TR(P, pv[:, j * 128:(j + 1) * 128], sb_[:, (c0 + j) * 128:(c0 + j + 1) * 128], ident_b[:],
                               [("selb", b % 2)], [PSR(3)], inc=(j == n - 1))
                        CP(P, "act", mT[:, c0:c0 + n, b * 128:(b + 1) * 128],
                           pv[:, 0:n * 128].rearrange("p (c q) -> p c q", c=n), [PSR(3)], [("maskT", g % 2, b)])
                    yield Ls[b] / 1024.0 + 1.0

            def gen_A(g):
                mT = maskT[g % 2]
                mres = [("maskT", g % 2, b) for b in range(4)]
                nkc = 4 * g + 4
                items = [(h, c) for h in range(8) for c in range(nkc)]

                def emit_S(idx):
                    h, c = items[idx]
                    cs = max(0, c - 4 * g) * 128
                    qs = slice(g * 512 + cs, g * 512 + 512)
                    bk = 4 + idx % 3
                    MM(P, ps[bk][:, cs:512], kT[:, c * 128:(c + 1) * 128], qT[:, h, qs], True, False, [], [PSR(bk)], inc=False)
                    MM(P, ps[bk][:, cs:512], ident_b[:], mT[:, c, cs:512], False, True, mres, [PSR(bk)])
                    ACT(P, Pexp[idx % 3][:, cs:512], ps[bk][:, cs:512], AF.Exp, [PSR(bk)], [("Pexp", idx % 3)], scale=0.125)

                def emit_PV(idx):
                    h, c = items[idx]
                    par = h % 2
                    rows = slice(64 * par, 64 * par + 64)
                    vcols = slice(0, 128) if par == 0 else slice(64, 192)
                    cs = max(0, c - 4 * g) * 128
                    MM(P, ps[7][:, cs:512], Vext[:, c, vcols], Pexp[idx % 3][:, cs:512], c == 0, c == nkc - 1,
                       [("Pexp", idx % 3)], [PSR(7)], inc=True)
                    if c == nkc - 1:
                        o_ = oi_box[0]
                        ob = Osb[o_ % 3]
                        ores = ("Osb", o_ % 3)
                        dr = slice(64, 65) if par == 0 else slice(0, 1)
                        CP(P, "act", ob[:], psf(7), [PSR(7)], [ores])
                        ACT(P, ob[dr, :], ob[dr, :], AF.Ln, [ores], [ores])
                        ACT(P, ob[dr, :], ob[dr, :], AF.Exp, [ores], [ores], scale=-1.0)

                        def fin1(ob=ob, ores=ores, par=par):
                            MM(P, psf(3), Sden[:, par, :], ob[:], True, True, [ores, "Sden"], [PSR(3)])

                        def fin2(ob=ob, ores=ores, rows=rows, h=h):
                            TT(P, "dve", mixT[rows, h // 2, g * 512:(g + 1) * 512], ob[rows, :], ps[3][rows, :], ALU.mult,
                               [ores, PSR(3)], [("mixT", "a", h, g)])
                        pending.append([2, fin1, fin2])
                        oi_box[0] += 1

                pending = []

                def run_pending(force=False):
                    for p_ in list(pending):
                        p_[0] -= 1
                        if p_[0] <= 0 or force:
                            p_[1]()
                            p_[2]()
                            pending.remove(p_)

                emit_S(0)
                emit_S(1)
                yield 1.4
                for idx in range(len(items)):
                    emit_PV(idx)
                    if idx + 2 < len(items):
                        emit_S(idx + 2)
                    run_pending()
                    yield 0.75
                run_pending(force=True)

            def timed_interleave(*gens):
                gens = [[0.0, g_] for g_ in gens]
                while gens:
                    gens.sort(key=lambda x: x[0])
                    cur = gens[0]
                    try:
                        cur[0] += next(cur[1])
                    except StopIteration:
                        gens.remove(cur)

            gorder = [3, 2, 1, 0]
            timed_interleave(gen_IB(gorder[0]))
            for gi, g in enumerate(gorder):
                if gi + 1 < 4:
                    timed_interleave(gen_A(g), gen_IB(gorder[gi + 1]))
                else:
                    timed_interleave(gen_A(g))
            dump("attT", mixT[:, 0:4, :], [128, 4, S], [("mixT", "a", h, g) for h in range(8) for g in range(4)])
            P.barrier()
            A.free(*p2_tmp)
            A.free(qT, kT, iqT, ikT, Vext, iws)
            if stop_after == 2:
                A.free(mixT)
                continue

            def ln_gen(r, st, junk_, gam, bet, hn, tag, add_eng="pool"):
                ACT(P, junk_[:], r[:], AF.Identity, [(tag, "r")], [(tag, "junk"), (tag, "st")], accum=st[:, 0:1])
                yield
                TS(P, "dve", st[:, 1:2], st[:, 0:1], -1.0 / D, None, ALU.mult, None, [(tag, "st")], [(tag, "st")])
                yield
                ACT(P, junk_[:], r[:], AF.Square, [(tag, "r"), (tag, "st")], [(tag, "junk"), (tag, "st")],
                    bias=st[:, 1:2], accum=st[:, 2:3])
                yield
                ACT(P, st[:, 3:4], st[:, 2:3], AF.Ln, [(tag, "st")], [(tag, "st")], bias=LN_EPS, scale=1.0 / D)
                yield
                ACT(P, st[:, 3:4], st[:, 3:4], AF.Exp, [(tag, "st")], [(tag, "st")], scale=-0.5)
                yield
                TT(P, "dve", st[:, 4:5], st[:, 1:2], st[:, 3:4], ALU.mult, [(tag, "st")], [(tag, "st")])
                yield
                ACT(P, hn[:], r[:], AF.Identity, [(tag, "r"), (tag, "st")], [(tag, "hn")], bias=st[:, 4:5], scale=st[:, 3:4])
                yield
                TT(P, "dve", hn[:], hn[:], gam[:], ALU.mult, [(tag, "hn"), "lng"], [(tag, "hn")])
                yield
                TT(P, add_eng, hn[:], hn[:], bet[:], ALU.add, [(tag, "hn"), "lnb"], [(tag, "hn")])
                yield

            def interleave(*gens):
                gens = list(gens)
                while gens:
                    for g_ in list(gens):
                        try:
                            next(g_)
                        except StopIteration:
                            gens.remove(g_)

            def layer_norm(r, st, junk_, gam, bet, hn, tag):
                interleave(ln_gen(r, st, junk_, gam, bet, hn, tag, add_eng="dve"))

            hT = A.alloc("hT", [128, 8, S], BF16)
            gates = A.alloc("gates", [128, NT, 16], F32)
            wout = A.alloc("wout", [128, 8, D], BF16)
            lng = A.alloc("lng", [128, D], F32)
            lnb = A.alloc("lnb", [128, D], F32)
            xres = [A.alloc("xres%d" % i, [128, D], F32) for i in range(6)]
            rr = [A.alloc("rr%d" % i, [128, D], F32) for i in range(6)]
            hn = [A.alloc("hn%d" % i, [128, D], F32) for i in range(6)]
            lnj = [A.alloc("lnj%d" % i, [128, D], BF16) for i in range(6)]
            stt_ = [A.alloc("st%d" % i, [128, 8], F32) for i in range(6)]
            hT32 = []
            hlo = [A.alloc("hlo%d" % i, [128, 8, 128], BF16) for i in range(2)]
            wr_hl = A.alloc("wr_hl", [128, 2, 8, 20], BF16)
            wr_res = A.alloc("wr_res", [128, 8, 20], F32)
            wr = A.alloc("wr", [128, 8, 20], F32)
            nwc = A.alloc("nwc", [128, 4], F32)
            brp = A.alloc("brp", [128, 20], F32)
            lg = A.alloc("lg", [128, NT, 20], F32)
            rs = [A.alloc("rs%d" % i, [128, NT, 16], F32) for i in range(4)]
            rv = [A.alloc("rv%d" % i, [128, NT], F32) for i in range(8)]
            p4_tmp = [wout, lng, lnb, wr, nwc, brp, lg, wr_hl, wr_res] + hlo + xres + rr + hn + lnj + stt_ + rs + rv
            for c in range(8):
                DMA(P, "pool", wout[:, c, :], wout_d[c * 128:(c + 1) * 128, :], [], [("wout", c)])
            DMA(P, "sp", lng[:], ln1g_d, [], ["lng"])
            DMA(P, "sp", lnb[:], ln1b_d, [], ["lnb"])
            DMA(P, "sp", nwc[:], nwc_d, [], ["nwc"])
            for k in range(4):
                TS(P, "dve", wout[:, 4 + k, :], wout[:, 4 + k, :], nwc[:, k:k + 1], None, ALU.mult, None,
                   [("wout", 4 + k), "nwc"], [("wout", 4 + k)])
            DMA(P, "sp", wr[:], wr_d.rearrange("(c p) f -> p c f", p=128), [], ["wr"])
            DMA(P, "sp", brp[:], br_d, [], ["brp"])
            CP(P, "dve", wr_hl[:, 0, :, :], wr[:], ["wr"], ["wr_h"])
            TT(P, "dve", wr_res[:], wr[:], wr_hl[:, 0, :, :], ALU.subtract, ["wr", "wr_h"], ["wr_res"])
            CP(P, "dve", wr_hl[:, 1, :, :], wr_res[:], ["wr_res"], ["wr_hl"])
            mb = Rot([0, 1, 2, 3])

            def mm_part(t):
                i = t % 6
                tok = slice(t * 128, (t + 1) * 128)
                tag = ("ln1", i)
                DMA(P, "sp", xres[i][:], x_d[seq, tok, :], [], [("xres", i)])
                for half in range(2):
                    b = mb.next()
                    hs = slice(half * 512, half * 512 + 512)
                    for c in range(8):
                        MM(P, psf(b), mixT[:, c, tok], wout[:, c, hs], c == 0, c == 7, [("wout", c)], [PSR(b)], inc=(c == 7))
                    STT(P, "dve", rr[i][:, hs], xres[i][:, hs], ALPHA, psf(b), ALU.mult, ALU.add,
                        [("xres", i), PSR(b)], [(tag, "r")])

            def post_gen(t):
                i = t % 6
                tok = slice(t * 128, (t + 1) * 128)
                tag = ("ln1", i)
                yield from ln_gen(rr[i], stt_[i], lnj[i], lng, lnb, hn[i], tag, add_eng="dve")
                DMA(P, "pool", hscr_d[seq, tok, :], hn[i][:], [(tag, "hn")], [])
                yield

            def tr_part(t):
                i = t % 6
                tok = slice(t * 128, (t + 1) * 128)
                tag = ("ln1", i)
                hres_ = [("hT32a", t % 2), ("hT32b", t % 2)]
                for c in range(8):
                    b = 4 + c // 4
                    TR(P, ps[b][:, (c % 4) * 128:(c % 4 + 1) * 128], hn[i][:, c * 128:(c + 1) * 128], ident_f[:],
                       [(tag, "hn")], [PSR(b)], inc=(c % 4 == 3))
                lo_ = hlo[t % 2]
                CP(P, "act", hT[:, 0:4, tok], ps[4][:].rearrange("p (c t) -> p c t", c=4), [PSR(4)], [("hT", t, 0)])
                CP(P, "dve", hT[:, 4:8, tok], ps[5][:].rearrange("p (c t) -> p c t", c=4), [PSR(5)], [("hT", t, 1)])
                TT(P, "dve", lo_[:, 0:4, :], ps[4][:].rearrange("p (c t) -> p c t", c=4), hT[:, 0:4, tok], ALU.subtract,
                   [PSR(4), ("hT", t, 0)], [hres_[0]])
                TT(P, "dve", lo_[:, 4:8, :], ps[5][:].rearrange("p (c t) -> p c t", c=4), hT[:, 4:8, tok], ALU.subtract,
                   [PSR(5), ("hT", t, 1)], [hres_[1]])
                rb = 6 + t % 2
                rres = hres_ + [("hT", t, 0), ("hT", t, 1), "wr_hl"]
                for c in range(8):
                    MM(P, ps[rb][:, 0:20], hT[:, c, tok], wr_hl[:, 0, c, :], c == 0, False, rres, [PSR(rb)], inc=False)
                    MM(P, ps[rb][:, 0:20], hT[:, c, tok], wr_hl[:, 1, c, :], False, False, rres, [PSR(rb)], inc=False)
                    MM(P, ps[rb][:, 0:20], lo_[:, c, :], wr_hl[:, 0, c, :], False, c == 7, rres, [PSR(rb)], inc=(c == 7))
                TT(P, "dve", lg[:, t, :], ps[rb][:, 0:20], brp[:], ALU.add, [PSR(rb), "brp"], [("lg", t)])

            mm_part(0)
            mm_part(1)
            mm_part(2)
            mm_part(3)
            interleave(post_gen(0), post_gen(1))
            for tp in range(0, NT, 2):
                if tp + 2 < NT:
                    interleave(post_gen(tp + 2), post_gen(tp + 3))
                if tp + 4 < NT:
                    mm_part(tp + 4)
                    mm_part(tp + 5)
                tr_part(tp)
                tr_part(tp + 1)
            dump("hT", hT[:], [128, 8, S], [("hT", t, k_) for t in range(NT) for k_ in range(2)])
            lgr = [("lg", t) for t in range(NT)]
            gl = lg[:, :, 0:4]
            gmax, gsum, gprob, m1, m2, dd, w1, w2 = [rv[i] for i in range(8)]
            ge, ohg, pen = rs[0][:, :, 0:4], rs[1][:, :, 0:4], rs[2][:, :, 0:4]

            def red(out, in_, op, r, w):
                P.op("dve", lambda e: e.tensor_reduce(out, in_, axis=AX.X, op=op), r, w)

            def bc(v, n):
                return v[:].unsqueeze(2).to_broadcast([128, NT, n])
            red(gmax[:], gl, ALU.max, lgr, ["gmax"])
            TT(P, "dve", ge, gl, bc(gmax, 4), ALU.subtract, lgr + ["gmax"], ["ge"])
            ACT(P, ge, ge, AF.Exp, ["ge"], ["ge"])
            red(gsum[:], ge, ALU.add, ["ge"], ["gsum"])
            P.op("dve", lambda e: e.reciprocal(gprob[:], gsum[:]), ["gsum"], ["gprob"])
            TT(P, "dve", ohg, gl, bc(gmax, 4), ALU.is_ge, lgr + ["gmax"], ["ohg"])
            TS(P, "dve", pen, ohg, -1.0, 1.0e4, ALU.add, ALU.mult, ["ohg"], ["pen"])
            mel, oh1, mel2, oh2 = rs[3], rs[0], rs[1], rs[2]
            for gq in range(4):
                TT(P, "dve", mel[:, :, gq * 4:(gq + 1) * 4], lg[:, :, 4 + gq * 4:8 + gq * 4],
                   pen[:, :, gq:gq + 1].to_broadcast([128, NT, 4]), ALU.add, lgr + ["pen"], ["mel"])
            red(m1[:], mel[:], ALU.max, ["mel"], ["m1"])
            TT(P, "dve", oh1[:], mel[:], bc(m1, 16), ALU.is_ge, ["mel", "m1", "ge"], ["oh1"])
            STT(P, "dve", mel2[:], oh1[:], -1.0e4, mel[:], ALU.mult, ALU.add, ["oh1", "mel", "ohg"], ["mel2"])
            red(m2[:], mel2[:], ALU.max, ["mel2"], ["m2"])
            TT(P, "dve", oh2[:], mel2[:], bc(m2, 16), ALU.is_ge, ["mel2", "m2", "pen"], ["oh2"])
            TT(P, "dve", dd[:], m2[:], m1[:], ALU.subtract, ["m1", "m2"], ["dd"])
            ACT(P, dd[:], dd[:], AF.Exp, ["dd"], ["dd"])
            TS(P, "dve", w1[:], dd[:], 1.0, None, ALU.add, None, ["dd"], ["w1"])
            P.op("dve", lambda e: e.reciprocal(w1[:], w1[:]), ["w1"], ["w1"])
            TT(P, "dve", w2[:], dd[:], w1[:], ALU.mult, ["dd", "w1"], ["w2"])
            TT(P, "dve", w1[:], w1[:], gprob[:], ALU.mult, ["w1", "gprob", "w2"], ["w1"])
            TT(P, "dve", w2[:], w2[:], gprob[:], ALU.mult, ["w2", "gprob"], ["w2"])
            TT(P, "dve", oh1[:], oh1[:], bc(w1, 16), ALU.mult, ["oh1", "w1", "mel2"], ["oh1"])
            TT(P, "dve", oh2[:], oh2[:], bc(w2, 16), ALU.mult, ["oh2", "w2"], ["oh2"])
            TT(P, "dve", gates[:], oh1[:], oh2[:], ALU.add, ["oh1", "oh2"], ["gates"])
            dump("gates", gates[:], [128, NT, 16], ["gates"])
            P.barrier()
            A.free(*p4_tmp)
            A.free(mixT)
            if stop_after == 4:
                A.free(hT, gates)
                continue

            selc = A.alloc("selc", [16, 16 * 128], F32)
            gT = A.alloc("gT", [16, 1024], F32)
            wdn = A.alloc("wdn", [128, 16, D], BF16)
            hid = A.alloc("hid", [128, 16, 1024], BF16)
            yacc = A.alloc("yacc", [128, 8, D], F32)
            Wgu = [A.alloc("Wgu%d" % i, [128, 8, 512], BF16) for i in range(2)]
            sgt = [A.alloc("sgt%d" % i, [128, 512], BF16) for i in range(2)]
            ttm = [A.alloc("ttm%d" % i, [128, 512], BF16) for i in range(2)]
            lng = A.alloc("lng2", [128, D], F32)
            lnb = A.alloc("lnb2", [128, D], F32)
            hres = [A.alloc("hres%d" % i, [128, D], F32) for i in range(2)]
            r2 = [A.alloc("r2_%d" % i, [128, D], F32) for i in range(2)]
            on = [A.alloc("on%d" % i, [128, D], F32) for i in range(2)]
            lnj = [A.alloc("lnj2_%d" % i, [128, D], BF16) for i in range(2)]
            stt_ = [A.alloc("st2_%d" % i, [128, 8], F32) for i in range(2)]
            p5_tmp = [selc, gT, wdn, hid, yacc, lng, lnb] + Wgu + sgt + ttm + hres + r2 + on + lnj + stt_
            DMA(P, "sp", selc[:], sel_d, [], ["selc"])
            DMA(P, "sp", lng[:], ln2g_d, [], ["lng"])
            DMA(P, "sp", lnb[:], ln2b_d, [], ["lnb"])
            gub = Rot([2, 3, 4, 5])
            geb = Rot([0, 1])
            dnb = Rot([6, 7])
            wi = 0
            si = 0
            li = 0
            for hf in range(2):
                for j4 in range(2):
                    for k in range(4):
                        t = hf * 8 + j4 * 4 + k
                        TR(P, ps[j4][0:16, k * 128:(k + 1) * 128], gates[:, t, :], ident_f[:], ["gates"], [PSR(j4)], inc=(k == 3))
                    CP(P, "dve", gT[:, j4 * 512:(j4 + 1) * 512], ps[j4][0:16, :], [PSR(j4)], [("gT", j4)])
                for eh in range(2):
                    for el in range(8):
                        e = eh * 8 + el
                        W = Wgu[wi % 2]
                        wres = ("Wgu", wi % 2)
                        DMA(P, "pool", W[:, :, 0:256], wg_d[e].rearrange("(c p) f -> p c f", p=128), [], [wres])
                        DMA(P, "pool", W[:, :, 256:512], wu_d[e].rearrange("(c p) f -> p c f", p=128), [], [wres])
                        DMA(P, "pool", wdn[:, 2 * el:2 * el + 2, :], wd_d[e].rearrange("(j p) d -> p j d", p=128), [],
                            [("wdn", el)])
                        wi += 1
                        for sub in range(2):
                            ts_ = slice(hf * 1024 + sub * 512, hf * 1024 + sub * 512 + 512)
                            loc = slice(sub * 512, sub * 512 + 512)
                            gb_ = geb.next()
                            MM(P, psf(gb_), selc[:, e * 128:(e + 1) * 128], gT[:, loc], True, True,
                               ["selc", ("gT", sub)], [PSR(gb_)])
                            for j in range(2):
                                bg, bu = gub.next(), gub.next()
                                for c in range(8):
                                    MM(P, psf(bg), W[:, c, j * 128:(j + 1) * 128], hT[:, c, ts_], c == 0, c == 7,
                                       [wres], [PSR(bg)], inc=(c == 7))
                                for c in range(8):
                                    MM(P, psf(bu), W[:, c, 256 + j * 128:256 + (j + 1) * 128], hT[:, c, ts_], c == 0, c == 7,
                                       [wres], [PSR(bu)], inc=(c == 7))
                                sg_, tm_ = sgt[si % 2], ttm[si % 2]
                                ACT(P, sg_[:], psf(bg), AF.Silu, [PSR(bg)], [("sgt", si % 2)])
                                TT(P, "dve", tm_[:], sg_[:], psf(bu), ALU.mult, [("sgt", si % 2), PSR(bu)], [("ttm", si % 2)])
                                TT(P, "dve", hid[:, el * 2 + j, loc], tm_[:], psf(gb_), ALU.mult,
                                   [("ttm", si % 2), PSR(gb_)], [("hid", el * 2 + j, sub)])
                                si += 1
                    hres_all = [("hid", k, sub) for k in range(16) for sub in range(2)] + [("wdn", el) for el in range(8)]
                    for lt in range(8):
                        t = hf * 8 + lt
                        tok = slice(t * 128, (t + 1) * 128)
                        i = li % 2
                        tag = ("ln2", i)
                        if eh == 1 and lt == 0:
                            for l2 in range(2):
                                t2 = hf * 8 + l2
                                DMA(P, "sp", hres[(li + l2) % 2][:], hscr_d[seq, t2 * 128:(t2 + 1) * 128, :], [],
                                    [("hres", (li + l2) % 2)])
                        for dh in range(2):
                            b = dnb.next()
                            hs = slice(dh * 512, dh * 512 + 512)
                            for k in range(16):
                                MM(P, psf(b), hid[:, k, lt * 128:(lt + 1) * 128], wdn[:, k, hs], k == 0, k == 15,
                                   hres_all, [PSR(b)], inc=(k == 15))
                            if eh == 0:
                                CP(P, "act", yacc[:, lt, hs], psf(b), [PSR(b)], [("yacc", lt)])
                            else:
                                TT(P, "dve", r2[i][:, hs], yacc[:, lt, hs], psf(b), ALU.add, [("yacc", lt), PSR(b)], [(tag, "r0")])
                                STT(P, "dve", r2[i][:, hs], hres[i][:, hs], ALPHA, r2[i][:, hs], ALU.mult, ALU.add,
                                    [("hres", i), (tag, "r0")], [(tag, "r")])
                        if eh == 1:
                            if lt + 2 < 8:
                                t2 = hf * 8 + lt + 2
                                DMA(P, "sp", hres[i][:], hscr_d[seq, t2 * 128:(t2 + 1) * 128, :], [], [("hres", i)])
                            layer_norm(r2[i], stt_[i], lnj[i], lng, lnb, on[i], tag)
                            DMA(P, "sp", out_d[seq, tok, :], on[i][:], [(tag, "hn")], [])
                            li += 1
            P.barrier()
            A.free(*p5_tmp)
            A.free(hT, gates)
    P.emit()
    return nc, dumps


def _in_map(inp, core, consts, wfm, wtm):
    m = dict(consts)
    m["x"] = np.ascontiguousarray(inp["x"][SEQ_PER_CORE * core:SEQ_PER_CORE * (core + 1)], dtype=np.float32)
    m["wfm"] = wfm
    m["wtm"] = wtm
    cw = inp["conv_w"][0]
    m["convw"] = np.ascontiguousarray(cw.reshape(4, 6, 128).transpose(2, 1, 0).reshape(128, 24))
    m["convb"] = np.ascontiguousarray(inp["conv_b"][0].reshape(6, 128).T)
    m["dtb"] = _rep(inp["dt_bias"][0])
    m["alog"] = _rep(inp["a_log"][0])
    m["dskip"] = _rep(np.repeat(inp["d_skip"][0], 64))
    m["nw"] = _rep(inp["ssd_norm_w"][0])
    m["nwc"] = np.ascontiguousarray(inp["ssd_norm_w"][0].reshape(4, 128).T)
    m["wout"] = np.ascontiguousarray(inp["w_out"][0])
    m["ln1g"] = _rep(inp["ln1_g"][0])
    m["ln1b"] = _rep(inp["ln1_b"][0])
    m["ln2g"] = _rep(inp["ln2_g"][0])
    m["ln2b"] = _rep(inp["ln2_b"][0])
    m["wr"] = np.ascontiguousarray(np.concatenate([inp["w_route_group"][0], inp["w_route_expert"][0]], 1))
    m["br"] = _rep(np.concatenate([inp["b_route_group"][0], inp["b_route_expert"][0]]))
    m["wg"] = np.ascontiguousarray(inp["w_gate"][0])
    m["wu"] = np.ascontiguousarray(inp["w_up"][0])
    m["wd"] = np.ascontiguousarray(inp["w_down"][0])
    return m


def kernel(**inputs):
    inp = {k: np.asarray(v, dtype=np.float32) for k, v in inputs.items()}
    wfm, wtm = _layout_w_in(inp["w_in"])
    consts = _consts()
    nc, _ = build()
    in_maps = [_in_map(inp, c, consts, wfm, wtm) for c in range(NCORES)]
    res = run_bass_kernel_spmd(nc, in_maps, core_ids=list(range(NCORES)))
    out = np.concatenate([np.asarray(res.results[c]["out"], dtype=np.float32) for c in range(NCORES)], axis=0)
    return out
```

```python
import numpy as np
import concourse.bass as bass
import concourse.mybir as mybir
from concourse.bass_utils import run_bass_kernel_spmd

F32 = mybir.dt.float32
BF16 = mybir.dt.bfloat16
ALU = mybir.AluOpType
AF = mybir.ActivationFunctionType
AX = mybir.AxisListType

NCORES = 8
S = 2048
D = 1024
NT = S // 128
SEQ_PER_CORE = 2
ALPHA = 2.0 ** 0.25
LN_EPS = 1e-5
IDX_SCALE = (4 ** -0.5) * (64 ** -0.5)
NF = 22
TMW = 588
NBIS = 14
NEG = -1.0e30
MASKB = -262144.0


class Prog:
    CE = ("pe", "act", "dve", "pool")

    def __init__(self, nc, kdma=12):
        self.nc = nc
        self.ops = {e: [] for e in ("pe", "act", "dve", "pool", "sp")}
        self.cnt = {e: 0 for e in self.CE}
        self.last_w = {}
        self.readers = {}
        self.seen = {e: {} for e in self.ops}
        self.kdma = kdma
        self.ring = {"sp": [0] * kdma, "pool": [0] * kdma}
        self.ring_next = {"sp": 0, "pool": 0}
        self.nops = 0
        self.floor = {e: {} for e in self.ops}
        self.ps_last = {}

    def barrier(self):
        cur = {e: self.cnt[e] for e in self.CE if self.cnt[e] > 0}
        for q in ("sp", "pool"):
            for k in range(self.kdma):
                if self.ring[q][k] > 0:
                    cur["d%s%d" % (q, k)] = 16 * self.ring[q][k]
        for e in self.floor:
            self.floor[e] = dict(cur)

    def _deps(self, reads, writes):
        deps = {}

        def add(tok):
            if tok is None:
                return
            k, v = tok
            if deps.get(k, 0) < v:
                deps[k] = v
        for r in reads:
            add(self.last_w.get(r))
        for w in writes:
            add(self.last_w.get(w))
            for k, v in self.readers.get(w, {}).items():
                add((k, v))
        return deps

    def _commit(self, tok, reads, writes):
        for r in reads:
            d = self.readers.setdefault(r, {})
            if d.get(tok[0], 0) < tok[1]:
                d[tok[0]] = tok[1]
        for w in writes:
            self.last_w[w] = tok
            self.readers[w] = {}

    def _waits(self, eng, deps):
        waits = []
        fl = self.floor[eng]
        if fl:
            for k, v in fl.items():
                if deps.get(k, 0) < v:
                    deps[k] = v
            self.floor[eng] = {}
        for k, v in deps.items():
            if k == "pe" and eng == "pe":
                continue
            if self.seen[eng].get(k, 0) >= v:
                continue
            self.seen[eng][k] = v
            waits.append((k, v))
        return waits

    def op(self, eng, fn, reads=(), writes=(), inc=True):
        assert eng in self.CE
        if eng != "pe":
            assert inc
        deps = self._deps(reads, writes)
        idx = self.cnt[eng] + 1
        if inc:
            self.cnt[eng] = idx
        tok = (eng, idx)
        for r in reads:
            if isinstance(r, tuple) and r[0] == "ps":
                prev = self.ps_last.get(r[1])
                if prev is not None and prev[0] != eng and deps.get(prev[0], 0) < prev[1]:
                    deps[prev[0]] = prev[1]
                self.ps_last[r[1]] = tok
        waits = self._waits(eng, deps)
        self.ops[eng].append((waits, fn, (eng, 1) if inc else None))
        self._commit(tok, reads, writes)
        self.nops += 1
        return tok

    def dma(self, q, fn, reads=(), writes=()):
        deps = self._deps(reads, writes)
        k = self.ring_next[q] % self.kdma
        self.ring_next[q] += 1
        key = "d%s%d" % (q, k)
        if self.ring[q][k] > 0:
            v = 16 * self.ring[q][k]
            if deps.get(key, 0) < v:
                deps[key] = v
        waits = self._waits(q, deps)
        self.ring[q][k] += 1
        tok = (key, 16 * self.ring[q][k])
        self.ops[q].append((waits, fn, (key, 16)))
        self._commit(tok, reads, writes)
        self.nops += 1
        return tok

    def emit(self):
        nc = self.nc
        names = list(self.CE) + ["d%s%d" % (q, k) for q in ("sp", "pool") for k in range(self.kdma)]
        fin = []
        for q in ("sp", "pool"):
            for k in range(self.kdma):
                if self.ring[q][k] > 0:
                    fin.append(("d%s%d" % (q, k), 16 * self.ring[q][k]))
        for e in self.CE:
            if self.cnt[e] > 0:
                fin.append((e, self.cnt[e]))
        ops = self.ops
        import contextlib
        with contextlib.ExitStack() as st:
            sems = {n: st.enter_context(nc.semaphore("s_" + n)) for n in names}
            block = st.enter_context(nc.Block())

            def replay(eng_name):
                def run(e):
                    for waits, fn, inc in ops[eng_name]:
                        for k, v in waits:
                            e.wait_ge(sems[k], v)
                        ins = fn(e)
                        if inc is not None:
                            ins.then_inc(sems[inc[0]], inc[1])
                    if eng_name == "sp":
                        for k, v in fin:
                            e.wait_ge(sems[k], v)
                return run
            block.sync(replay("sp"))
            block.tensor(replay("pe"))
            block.scalar(replay("act"))
            block.vector(replay("dve"))
            block.gpsimd(replay("pool"))


def _rot(cols):
    return np.concatenate([cols[32:], cols[:32]])


def _layout_w_in(w_in):
    w = w_in[0]
    oq, ok, ov, oiq, oik, oiw, oz, oxbc, odt = 0, 512, 576, 640, 896, 960, 964, 1476, 2244
    tiles = []
    qh = [np.arange(oq + 64 * h, oq + 64 * (h + 1)) for h in range(8)]
    for p in range(4):
        tiles.append(np.concatenate([qh[2 * p], qh[2 * p + 1]]))
    for p in range(4):
        tiles.append(np.concatenate([_rot(qh[2 * p]), _rot(qh[2 * p + 1])]))
    kc = np.arange(ok, ok + 64)
    tiles.append(np.concatenate([kc, kc]))
    tiles.append(np.concatenate([_rot(kc), _rot(kc)]))
    ih = [np.arange(oiq + 64 * h, oiq + 64 * (h + 1)) for h in range(4)]
    for p in range(2):
        tiles.append(np.concatenate([ih[2 * p], ih[2 * p + 1]]))
    for p in range(2):
        tiles.append(np.concatenate([_rot(ih[2 * p]), _rot(ih[2 * p + 1])]))
    ikc = np.arange(oik, oik + 64)
    tiles.append(np.concatenate([ikc, ikc]))
    tiles.append(np.concatenate([_rot(ikc), _rot(ikc)]))
    for t in range(6):
        tiles.append(np.arange(oxbc + 128 * t, oxbc + 128 * (t + 1)))
    fm_cols = np.concatenate(tiles)
    assert fm_cols.shape[0] == NF * 128
    tm_cols = np.concatenate([np.arange(oz, oz + 512), np.arange(ov, ov + 64), np.arange(oiw, oiw + 4),
                              np.arange(odt, odt + 8)])
    assert tm_cols.shape[0] == TMW
    return np.ascontiguousarray(w[:, fm_cols]), np.ascontiguousarray(w[:, tm_cols])


def _consts():
    c = {}
    c["ident"] = np.eye(128, dtype=np.float32)
    s_ = np.arange(128)
    c["tri"] = (s_[:, None] <= s_[None, :]).astype(np.float32)
    c["cneg"] = np.where(s_[None, :] <= s_[:, None], 0.0, NEG).astype(np.float32)
    inv = 10000.0 ** (-np.arange(0, 64, 2, dtype=np.float32) / 64.0)
    ang = np.arange(S, dtype=np.float32)[:, None] * inv[None, :]
    cos, sin = np.cos(ang).astype(np.float32), np.sin(ang).astype(np.float32)
    cosF = np.concatenate([cos, cos], 1).T
    sinS = np.concatenate([-sin, sin], 1).T
    c["cosF"] = np.ascontiguousarray(np.concatenate([cosF, cosF], 0))
    c["sinS"] = np.ascontiguousarray(np.concatenate([sinS, sinS], 0))
    sel = np.zeros((16, 16, 128), np.float32)
    for e in range(16):
        sel[e, e, :] = 1.0
    c["sel"] = sel.reshape(16, 16 * 128)
    c["halfpow"] = np.broadcast_to((0.5 ** np.arange(1, NBIS + 3, dtype=np.float32))[None, :], (128, NBIS + 2)).copy()
    return c


def _rep(v, n=128):
    return np.ascontiguousarray(np.broadcast_to(np.asarray(v, np.float32).reshape(1, -1), (n, np.asarray(v).size)))


def MM(P, out, lhsT, rhs, start, stop, r, w, inc=True):
    return P.op("pe", lambda e: e.matmul(out, lhsT=lhsT, rhs=rhs, start=start, stop=stop), r, w, inc)


def TR(P, out, in_, ident, r, w, inc=True):
    return P.op("pe", lambda e: e.transpose(out, in_, ident), r, w, inc)


def ACT(P, out, in_, func, r, w, bias=0.0, scale=1.0, accum=None):
    if accum is None:
        return P.op("act", lambda e: e.activation(out=out, in_=in_, func=func, bias=bias, scale=scale), r, w)
    return P.op("act", lambda e: e.activation(out=out, in_=in_, func=func, bias=bias, scale=scale, accum_out=accum), r, w)


def TT(P, eng, out, a, b, op, r, w):
    return P.op(eng, lambda e: e.tensor_tensor(out, a, b, op), r, w)


def TS(P, eng, out, a, s1, s2, op0, op1, r, w, accum=None):
    if op1 is None:
        return P.op(eng, lambda e: e.tensor_scalar(out, a, s1, None, op0=op0), r, w)
    if accum is None:
        return P.op(eng, lambda e: e.tensor_scalar(out, a, s1, s2, op0=op0, op1=op1), r, w)
    return P.op(eng, lambda e: e.tensor_scalar(out, a, s1, s2, op0=op0, op1=op1, accum_out=accum), r, w)


def STT(P, eng, out, a, scalar, b, op0, op1, r, w):
    return P.op(eng, lambda e: e.scalar_tensor_tensor(out, a, scalar, b, op0=op0, op1=op1), r, w)


def CP(P, eng, out, in_, r, w):
    if eng == "act":
        return P.op("act", lambda e: e.copy(out, in_), r, w)
    return P.op(eng, lambda e: e.tensor_copy(out, in_), r, w)


def MSET(P, eng, ap, val, r, w):
    return P.op(eng, lambda e: e.memset(ap, val), r, w)


def DMA(P, q, out, in_, r, w):
    return P.dma(q, lambda e: e.dma_start(out=out, in_=in_), r, w)


class Rot:
    def __init__(self, items):
        self.items = list(items)
        self.i = 0

    def next(self):
        v = self.items[self.i % len(self.items)]
        self.i += 1
        return v


class Arena:
    def __init__(self, nc, lo=16512, hi=229344):
        self.nc = nc
        self.free_list = [(lo, hi)]
        self.live = {}
        self.uid = 0
        self.cache = {}

    def alloc(self, name, shape, dt):
        n = 1
        for d in shape[1:]:
            n *= d
        size = n * (4 if dt == F32 else 2)
        size = (size + 31) // 32 * 32
        for i, (a, b) in enumerate(self.free_list):
            if b - a >= size:
                self.free_list[i] = (a + size, b)
                if a + size == b:
                    self.free_list.pop(i)
                key = (name, tuple(shape), str(dt), a)
                h = self.cache.get(key)
                if h is None:
                    self.uid += 1
                    h = self.nc.alloc_sbuf_tensor_at("sb%d_%s" % (self.uid, name), list(shape), dt, offset=a)
                    self.cache[key] = h
                self.live[id(h)] = (a, size, h)
                return h
        raise RuntimeError("arena out of SBUF for %s (%d bytes); free=%s" % (name, size, self.free_list))

    def free(self, *hs):
        for h in hs:
            a, size, _ = self.live.pop(id(h))
            self.free_list.append((a, a + size))
        self.free_list.sort()
        merged = []
        for a, b in self.free_list:
            if merged and merged[-1][1] == a:
                merged[-1] = (merged[-1][0], b)
            else:
                merged.append((a, b))
        self.free_list = merged


def build(dbg=(), nseq=SEQ_PER_CORE, stop_after=None):
    nc = bass.Bass("TRN2", target_bir_lowering=False)
    P = Prog(nc)
    A = Arena(nc)
    dbg = set(dbg)
    dumps = {}

    def din(name, shape):
        return nc.dram_tensor(name, list(shape), F32, kind="ExternalInput").ap()

    x_d = din("x", [SEQ_PER_CORE, S, D])
    wfm_d = din("wfm", [D, NF * 128])
    wtm_d = din("wtm", [D, TMW])
    ident_d = din("ident", [128, 128])
    tri_d = din("tri", [128, 128])
    cneg_d = din("cneg", [128, 128])
    cosF_d = din("cosF", [128, S])
    sinS_d = din("sinS", [128, S])
    sel_d = din("sel", [16, 16 * 128])
    halfpow_d = din("halfpow", [128, NBIS + 2])
    convw_d = din("convw", [128, 24])
    convb_d = din("convb", [128, 6])
    dtb_d = din("dtb", [128, 8])
    alog_d = din("alog", [128, 8])
    dskip_d = din("dskip", [128, 512])
    nw_d = din("nw", [128, 512])
    nwc_d = din("nwc", [128, 4])
    wout_d = din("wout", [D, D])
    ln1g_d = din("ln1g", [128, D])
    ln1b_d = din("ln1b", [128, D])
    ln2g_d = din("ln2g", [128, D])
    ln2b_d = din("ln2b", [128, D])
    wr_d = din("wr", [D, 20])
    br_d = din("br", [128, 20])
    wg_d = din("wg", [16, D, 256])
    wu_d = din("wu", [16, D, 256])
    wd_d = din("wd", [16, 256, D])
    out_d = nc.dram_tensor("out", [SEQ_PER_CORE, S, D], F32, kind="ExternalOutput").ap()
    hscr_d = nc.dram_tensor("hscr", [SEQ_PER_CORE, S, D], F32, kind="Internal").ap()

    import contextlib
    with contextlib.ExitStack() as es:
        ps = [es.enter_context(nc.psum_tensor("ps%d" % i, [128, 512], F32)) for i in range(8)]

        def psf(b):
            return ps[b][:]

        def psb(b):
            return ps[b][:].bitcast(BF16)

        def PSR(b):
            return ("ps", b)

        def dump(name, ap, shape, reads, dt=None):
            if name not in dbg:
                return
            t = nc.dram_tensor("dbg_" + name, list(shape), dt or ap.dtype, kind="ExternalOutput").ap()
            dumps[name] = t
            DMA(P, "sp", t, ap, reads, [])

        ident_f = A.alloc("ident_f", [128, 128], F32)
        ident_b = A.alloc("ident_b", [128, 128], BF16)
        tri_f = A.alloc("tri_f", [128, 128], F32)
        tri_b = A.alloc("tri_b", [128, 128], BF16)
        cneg = A.alloc("cneg", [128, 128], F32)
        ones_f = A.alloc("ones_f", [128, 128], F32)
        DMA(P, "sp", ident_f[:], ident_d, [], ["ident_f"])
        DMA(P, "pool", ident_b[:], ident_d, [], ["ident_b"])
        DMA(P, "sp", tri_f[:], tri_d, [], ["tri_f"])
        DMA(P, "pool", tri_b[:], tri_d, [], ["tri_b"])
        DMA(P, "sp", cneg[:], cneg_d, [], ["cneg"])
        MSET(P, "dve", ones_f[:], 1.0, [], ["ones_f"])

        for seq in range(nseq):
            qT = A.alloc("qz", [128, 8, S], BF16)
            kT = A.alloc("kT", [128, S], BF16)
            iqT = A.alloc("iqz", [128, 4, S], BF16)
            ikT = A.alloc("ikT", [128, S], BF16)
            Vext = A.alloc("Vext", [128, NT, 192], BF16)
            iws = A.alloc("iws", [128, NT, 4], F32)
            xbcT = A.alloc("xbcT", [128, 6, S], BF16)
            siluz = A.alloc("siluz", [128, NT, 512], BF16)
            dtr = A.alloc("dtr", [128, NT, 8], F32)

            xT = A.alloc("xT", [128, 8, S], BF16)
            wtm = A.alloc("wtm", [128, 8, TMW], BF16)
            xb = [A.alloc("xb%d" % i, [128, D], BF16) for i in range(4)]
            for c in range(8):
                DMA(P, "pool", wtm[:, c, :], wtm_d[c * 128:(c + 1) * 128, :], [], [("wtm", c)])
            MSET(P, "pool", qT[:], 0.0, [], ["qz0"])
            MSET(P, "pool", iqT[:], 0.0, [], ["iqz0"])
            MSET(P, "pool", Vext[:, :, 64:128], 0.0, [], [("Vext0",)])
            MSET(P, "pool", Vext[:, :, 64:65], 1.0, [("Vext0",)], [("Vext0",)])
            tr_banks = Rot([0, 1])
            fm_banks = Rot([4, 5, 6, 7])
            cp_eng = Rot(["act", "dve"])
            def a_dma(t):
                tok = slice(t * 128, (t + 1) * 128)
                DMA(P, "pool", xb[t % 4][:], x_d[seq, tok, :], [], [("xb", t % 4)])

            def a_tr(t):
                tok = slice(t * 128, (t + 1) * 128)
                xbuf = xb[t % 4]
                b = tr_banks.next()
                pv = psb(b)
                for c in range(8):
                    TR(P, pv[:, c * 128:(c + 1) * 128], xbuf[:, c * 128:(c + 1) * 128], ident_b[:],
                       [("xb", t % 4), "ident_b"], [PSR(b)], inc=(c == 7))
                CP(P, cp_eng.next(), xT[:, :, tok], pv.rearrange("p (c t) -> p c t", c=8),
                   [PSR(b)], [("xT", t)])

            def a_mm(t):
                tok = slice(t * 128, (t + 1) * 128)
                bz, bv = (2, 3) if t % 2 == 0 else (4, 5)
                for c in range(8):
                    MM(P, psf(bz), xT[:, c, tok], wtm[:, c, 0:512], c == 0, c == 7,
                       [("xT", t), ("wtm", c)], [PSR(bz)], inc=(c == 7))
                for c in range(8):
                    MM(P, ps[bv][:, 0:76], xT[:, c, tok], wtm[:, c, 512:588], c == 0, c == 7,
                       [("xT", t), ("wtm", c)], [PSR(bv)], inc=(c == 7))
                ACT(P, siluz[:, t, :], psf(bz), AF.Silu, [PSR(bz)], [("siluz", t)])
                CP(P, "dve", Vext[:, t, 0:64], ps[bv][:, 0:64], [PSR(bv)], [("Vext", t)])
                CP(P, "dve", Vext[:, t, 128:192], ps[bv][:, 0:64], [PSR(bv)], [("Vext", t)])
                TS(P, "dve", iws[:, t, :], ps[bv][:, 64:68], IDX_SCALE, None, ALU.mult, None,
                   [PSR(bv)], [("iws", t)])
                CP(P, "dve", dtr[:, t, :], ps[bv][:, 68:76], [PSR(bv)], [("dtr", t)])

            for t in range(3):
                a_dma(t)
            a_tr(0)
            for t in range(NT):
                if t + 3 < NT:
                    a_dma(t + 3)
                if t + 1 < NT:
                    a_tr(t + 1)
                a_mm(t)

            P.barrier()
            A.free(wtm, *xb)
            cosF = A.alloc("cosF", [128, S], F32)
            sinS = A.alloc("sinS", [128, S], F32)
            convw = A.alloc("convw", [128, 24], F32)
            convb = A.alloc("convb", [128, 6], F32)
            diagw = A.alloc("diagw", [128, 24, 128], BF16)
            uT = A.alloc("uT", [128, 6, S + 3], BF16)
            rt1 = [A.alloc("rt1_%d" % i, [128, 512], F32) for i in range(2)]
            rt2 = [A.alloc("rt2_%d" % i, [128, 512], F32) for i in range(2)]
            wsl = [A.alloc("wsl%d" % i, [128, 8, 128], BF16) for i in range(4)]
            p1_tmp = [xT, cosF, sinS, convw, convb, diagw, uT] + rt1 + rt2 + wsl
            DMA(P, "sp", cosF[:], cosF_d, [], ["cosF"])
            DMA(P, "sp", sinS[:], sinS_d, [], ["sinS"])
            DMA(P, "sp", convw[:], convw_d, [], ["convw"])
            DMA(P, "sp", convb[:], convb_d, [], ["convb"])
            for i in range(24):
                TS(P, "dve", diagw[:, i, :], ident_f[:], convw[:, i:i + 1], None, ALU.mult, None,
                   ["ident_f", "convw"], [("diagw", i)])
            MSET(P, "pool", uT[:, :, 0:3], 0.0, [], [("uT", -1)])
            wsi = [0]

            worder = [0, 4, 1, 5, 2, 6, 3, 7, 8, 9, 10, 12, 11, 13, 14, 15, 16, 17, 18, 19, 20, 21]
            wslot = {}

            def prefetch(n):
                for _ in range(n):
                    if wsi[0] >= len(worder):
                        return
                    f = worder[wsi[0]]
                    k = wsi[0] % 4
                    wsi[0] += 1
                    wslot[f] = k
                    DMA(P, "pool", wsl[k][:], wfm_d[:, f * 128:(f + 1) * 128].rearrange("(c p) f -> p c f", p=128),
                        [], [("wsl", k)])

            def load_w(f):
                return wslot[f]

            prefetch(4)

            def fm_proj(k, tg, b):
                tokg = slice(tg * 512, tg * 512 + 512)
                for c in range(8):
                    MM(P, psf(b), wsl[k][:, c, :], xT[:, c, tokg], c == 0, c == 7,
                       [("xT", tg * 4 + i) for i in range(4)] + [("wsl", k)], [PSR(b)], inc=(c == 7))

            pairs = [(0, 4, (qT, 0), "qT0"), (1, 5, (qT, 1), "qT1"), (2, 6, (qT, 2), "qT2"), (3, 7, (qT, 3), "qT3"),
                     (8, 9, kT, "kT"), (10, 12, (iqT, 0), "iqT0"), (11, 13, (iqT, 1), "iqT1"), (14, 15, ikT, "ikT")]
            rti = 0
            for fa, fr, dstf, dname in pairs:
                ka, kr = load_w(fa), load_w(fr)
                for tg in range(4):
                    tokg = slice(tg * 512, tg * 512 + 512)
                    ba, br = fm_banks.next(), fm_banks.next()
                    fm_proj(ka, tg, ba)
                    fm_proj(kr, tg, br)
                    r1, r2 = rt1[rti % 2], rt2[rti % 2]
                    TT(P, "dve", r1[:], psf(ba), cosF[:, tokg], ALU.mult, [PSR(ba), "cosF"], [("rt1", rti % 2)])
                    TT(P, "dve", r2[:], psf(br), sinS[:, tokg], ALU.mult, [PSR(br), "sinS"], [("rt2", rti % 2)])
                    rr_ = [("rt1", rti % 2), ("rt2", rti % 2), "qz0", "iqz0"]
                    if isinstance(dstf, tuple):
                        dt_, pp = dstf
                        TT(P, "pool", dt_[0:64, 2 * pp, tokg], r1[0:64, :], r2[0:64, :], ALU.add, rr_, [(dname, tg, 0)])
                        TT(P, "pool", dt_[64:128, 2 * pp + 1, tokg], r1[64:128, :], r2[64:128, :], ALU.add, rr_,
                           [(dname, tg, 1)])
                    else:
                        TT(P, "pool", dstf[:, tokg], r1[:], r2[:], ALU.add, rr_, [(dname, tg)])
                    rti += 1
                prefetch(2)
            for ct in range(6):
                k = load_w(16 + ct)
                for tg in range(4):
                    t0 = tg * 512
                    b = fm_banks.next()
                    fm_proj(k, tg, b)
                    CP(P, "act", uT[:, ct, 3 + t0:3 + t0 + 512], psf(b), [PSR(b)], [("uT", ct, tg)])
                prefetch(1)
                for tg in range(4):
                    t0 = tg * 512
                    b = fm_banks.next()
                    rr = [("uT", ct, tg), ("uT", -1)] + ([("uT", ct, tg - 1)] if tg > 0 else [])
                    for j in range(4):
                        MM(P, psf(b), diagw[:, ct * 4 + j, :], uT[:, ct, t0 + j:t0 + j + 512], j == 0, j == 3,
                           rr + [("diagw", ct * 4 + j)], [PSR(b)], inc=(j == 3))
                    ACT(P, xbcT[:, ct, t0:t0 + 512], psf(b), AF.Silu, [PSR(b), "convb"], [("xbcT", ct, tg)],
                        bias=convb[:, ct:ct + 1])
            g4 = range(4)
            P.barrier()
            dump("qz", qT[:], [128, 8, S], [])
            dump("iqz", iqT[:], [128, 4, S], [])
            A.free(*p1_tmp)
            if stop_after == 1:
                A.free(qT, kT, iqT, ikT, Vext, iws, xbcT, siluz, dtr)
                continue

            mixT = A.alloc("mixT", [128, 8, S], BF16)
            dtb = A.alloc("dtb", [128, 8], F32)
            arep = A.alloc("arep", [128, 8], F32)
            dskip = A.alloc("dskip", [128, 512], F32)
            nwr = A.alloc("nwr", [128, 512], F32)
            dt_all = A.alloc("dt_all", [128, NT, 8], F32)
            A_all = A.alloc("A_all", [128, NT, 8], F32)
            acum = A.alloc("acum", [128, NT, 8], F32)
            tot = A.alloc("tot", [128, NT, 8], F32)
            ds_all = A.alloc("ds_all", [128, NT, 8], F32)
            cdec = A.alloc("cdec", [128, NT, 8], F32)
            state = A.alloc("state", [128, 8, 64], F32)
            NB = 2
            xdt = [A.alloc("xdt%d" % i, [128, 8, 64], BF16) for i in range(NB)]
            xdtd = [A.alloc("xdtd%d" % i, [128, 8, 64], BF16) for i in range(NB)]
            dsk = [A.alloc("dsk%d" % i, [128, 512], F32) for i in range(NB)]
            Btok = [A.alloc("Btok%d" % i, [128, 128], BF16) for i in range(NB)]
            Gm = [A.alloc("Gm%d" % i, [128, 2, 128], BF16) for i in range(NB)]
            Ab = [A.alloc("Ab%d" % i, [128, 2, 8, 128], BF16) for i in range(NB)]
            A_hl = A.alloc("A_hl", [128, 2, NT, 8], BF16)
            A_res = A.alloc("A_res", [128, NT, 8], F32)
            Dm = [A.alloc("Dm%d" % i, [128, 8, 128], F32) for i in range(NB)]
            Lm = [A.alloc("Lm%d" % i, [128, 8, 128], BF16) for i in range(NB)]
            Eb = [A.alloc("Eb%d" % i, [128, 8, 128], BF16) for i in range(NB)]
            MT = [A.alloc("MT%d" % i, [128, 8, 128], BF16) for i in range(NB)]
            Cp = [A.alloc("Cp%d" % i, [128, 8, 128], BF16) for i in range(NB)]
            CTz = [A.alloc("CTz%d" % i, [128, 2, 128], BF16) for i in range(NB)]
            prevb = [A.alloc("prevb%d" % i, [128, 8, 64], BF16) for i in range(NB)]
            y1 = [A.alloc("y1_%d" % i, [128, 512], F32) for i in range(NB)]
            y2 = [A.alloc("y2_%d" % i, [128, 512], F32) for i in range(NB)]
            yo = [A.alloc("yo%d" % i, [128, 512], BF16) for i in range(NB)]
            junk = [A.alloc("junk%d" % i, [128, 512], BF16) for i in range(NB)]
            ms = [A.alloc("ms%d" % i, [128, 2], F32) for i in range(NB)]
            p3_tmp = ([A_hl, A_res, dtb, arep, dskip, nwr, dt_all, A_all, acum, tot, ds_all, cdec, state] + xdt + xdtd + dsk + Btok + Gm
                      + Ab + Dm + Lm + Eb + MT + Cp + CTz + prevb + y1 + y2 + yo + junk + ms)

            for i in range(NB):
                MSET(P, "pool", Cp[i][:], 0.0, [], [("Cp", i, 0), ("Cp", i, 1)])
                MSET(P, "pool", CTz[i][:], 0.0, [], [("CTz", i)])
            DMA(P, "sp", dtb[:], dtb_d, [], ["dtb"])
            DMA(P, "sp", arep[:], alog_d, [], ["arep"])
            DMA(P, "sp", dskip[:], dskip_d, [], ["dskip"])
            DMA(P, "sp", nwr[:], nw_d, [], ["nwr"])
            ACT(P, arep[:], arep[:], AF.Exp, ["arep"], ["arep"])
            TS(P, "dve", arep[:], arep[:], -1.0, None, ALU.mult, None, ["arep"], ["arep"])
            alldtr = [("dtr", t) for t in range(NT)]
            TT(P, "dve", dt_all[:], dtr[:], dtb[:].unsqueeze(1).to_broadcast([128, NT, 8]), ALU.add,
               alldtr + ["dtb"], ["dt_all"])
            ACT(P, dt_all[:], dt_all[:], AF.Exp, ["dt_all"], ["dt_all"])
            ACT(P, dt_all[:], dt_all[:], AF.Ln, ["dt_all"], ["dt_all"], bias=1.0)
            TT(P, "dve", A_all[:], dt_all[:], arep[:].unsqueeze(1).to_broadcast([128, NT, 8]), ALU.mult,
               ["dt_all", "arep"], ["A_all"])
            CP(P, "dve", A_hl[:, 0, :, :], A_all[:], ["A_all"], ["A_hl0"])
            TT(P, "dve", A_res[:], A_all[:], A_hl[:, 0, :, :], ALU.subtract, ["A_all", "A_hl0"], ["A_res"])
            CP(P, "dve", A_hl[:, 1, :, :], A_res[:], ["A_res"], ["A_hl1"])
            A2 = A_all[:].rearrange("p c h -> p (c h)")
            MM(P, ps[0][:, 0:128], tri_f[:], A2, True, True, ["tri_f", "A_all"], [PSR(0)])
            MM(P, ps[1][:, 0:128], ones_f[:], A2, True, True, ["ones_f", "A_all"], [PSR(1)])
            CP(P, "dve", acum[:].rearrange("p c h -> p (c h)"), ps[0][:, 0:128], [PSR(0)], ["acum"])
            CP(P, "dve", tot[:].rearrange("p c h -> p (c h)"), ps[1][:, 0:128], [PSR(1)], ["tot"])
            TT(P, "dve", ds_all[:], tot[:], acum[:], ALU.subtract, ["tot", "acum"], ["ds_all"])
            ACT(P, ds_all[:], ds_all[:], AF.Exp, ["ds_all"], ["ds_all"])
            ACT(P, cdec[:], tot[:], AF.Exp, ["tot"], ["cdec"])
            trb = Rot([0, 1])
            def ssd_r(c):
                    i = c % NB
                    CP(P, "dve", Ab[i][:], A_hl[:, :, c, :].unsqueeze(3).to_broadcast([128, 2, 8, 128]), ["A_hl0", "A_hl1"],
                       [("Ab", i)])
                    for h in range(8):
                        bb = 3 + h // 4
                        MM(P, ps[bb][:, (h % 4) * 128:(h % 4 + 1) * 128], Ab[i][:, 0, h, :], tri_b[:], True, False,
                           [("Ab", i), "tri_b"], [PSR(bb)], inc=False)
                        MM(P, ps[bb][:, (h % 4) * 128:(h % 4 + 1) * 128], Ab[i][:, 1, h, :], tri_b[:], False, True,
                           [("Ab", i), "tri_b"], [PSR(bb)], inc=(h % 4 == 3))

            def ssd_s1(c):
                    i = c % NB
                    tok = slice(c * 128, (c + 1) * 128)
                    g4 = c // 4
                    xres = [("xbcT", ct, g4) for ct in range(6)]
                    b = trb.next()
                    pv = psb(b)
                    for ct in range(5):
                        TR(P, pv[:, ct * 128:(ct + 1) * 128], xbcT[:, ct, tok], ident_b[:], xres + ["ident_b"], [PSR(b)],
                           inc=(ct == 4))
                    xs_ps = pv[:, 0:512].rearrange("p (h d) -> p h d", h=8)
                    TT(P, "dve", xdt[i][:], xs_ps, dt_all[:, c, :].unsqueeze(2).to_broadcast([128, 8, 64]), ALU.mult,
                       [PSR(b), "dt_all"], [("xdt", i)])
                    TT(P, "dve", dsk[i][:], pv[:, 0:512], dskip[:], ALU.mult, [PSR(b), "dskip"], [("dsk", i)])
                    CP(P, "act", Btok[i][:], pv[:, 512:640], [PSR(b)], [("Btok", i)])
                    TT(P, "dve", xdtd[i][:], xdt[i][:], ds_all[:, c, :].unsqueeze(2).to_broadcast([128, 8, 64]), ALU.mult,
                       [("xdt", i), "ds_all"], [("xdtd", i)])
                    for g in range(2):
                        CP(P, "pool", CTz[i][64 * g:64 * g + 64, g, :], xbcT[64 * g:64 * g + 64, 5, tok], xres, [("CTz", i)])
                    for g in range(2):
                        MM(P, ps[2][:, g * 128:(g + 1) * 128], xbcT[:, 4, tok], CTz[i][:, g, :],
                           True, True, xres + [("CTz", i)], [PSR(2)], inc=(g == 1))
                    TT(P, "dve", Gm[i][:], ps[2][:, 0:256].rearrange("p (g l) -> p g l", g=2),
                       tri_f[:].unsqueeze(1).to_broadcast([128, 2, 128]), ALU.mult, [PSR(2), "tri_f"], [("Gm", i)])
                    for g in range(2):
                        TT(P, "dve", Dm[i][:, 4 * g:4 * g + 4, :], ps[3 + g][:].rearrange("p (h l) -> p h l", h=4),
                           acum[:, c, 4 * g:4 * g + 4].unsqueeze(2).to_broadcast([128, 4, 128]), ALU.subtract,
                           [PSR(3 + g), "acum"], [("Dm", i)])
                    ACT(P, Dm[i][:], Dm[i][:], AF.Relu, [("Dm", i)], [("Dm", i)], scale=-1.0)
                    ACT(P, Lm[i][:], Dm[i][:], AF.Exp, [("Dm", i)], [("Lm", i)], scale=-1.0)
                    for g in range(2):
                        ACT(P, Eb[i][:, g * 4:(g + 1) * 4, :], ps[3 + g][:].rearrange("p (h l) -> p h l", h=4), AF.Exp,
                            [PSR(3 + g)], [("Eb", i, g)])
                    for g in range(2):
                        TT(P, "dve", MT[i][:, g * 4:(g + 1) * 4, :], Lm[i][:, g * 4:(g + 1) * 4, :],
                           Gm[i][:, g, :].unsqueeze(1).to_broadcast([128, 4, 128]), ALU.mult,
                           [("Lm", i), ("Gm", i)], [("MT", i, g)])
                        TT(P, "dve", Cp[i][64 * g:64 * g + 64, g * 4:(g + 1) * 4, :], Eb[i][64 * g:64 * g + 64, g * 4:(g + 1) * 4, :],
                           xbcT[64 * g:64 * g + 64, 5, tok].unsqueeze(1).to_broadcast([64, 4, 128]), ALU.mult,
                           [("Eb", i, g)] + xres, [("Cp", i, g)])

            def ssd_s2(c):
                    i = c % NB
                    tok = slice(c * 128, (c + 1) * 128)
                    g4 = c // 4
                    xres = [("xbcT", ct, g4) for ct in range(6)]
                    if c > 0:
                        CP(P, "pool", prevb[i][:], state[:], ["state"], [("prevb", i)])
                    for h in range(8):
                        g = h // 4
                        MM(P, ps[5][:, h * 64:(h + 1) * 64], MT[i][:, h, :], xdt[i][:, h, :], True, c == 0,
                           [("MT", i, g), ("xdt", i)], [PSR(5)], inc=(c == 0 and h == 7))
                        if c > 0:
                            MM(P, ps[5][:, h * 64:(h + 1) * 64], Cp[i][:, h, :],
                               prevb[i][:, h, :], False, True, [("Cp", i, g), ("prevb", i)], [PSR(5)],
                               inc=(h == 7))
                    if c < NT - 1:
                        for h in range(8):
                            MM(P, ps[6][:, h * 64:(h + 1) * 64], Btok[i][:], xdtd[i][:, h, :], True, True,
                               [("Btok", i), ("xdtd", i)], [PSR(6)], inc=(h == 7))
                        st2 = state[:].rearrange("p h d -> p (h d)")
                        if c == 0:
                            CP(P, "dve", st2, psf(6), [PSR(6)], ["state"])
                        else:
                            TT(P, "dve", state[:], state[:], cdec[:, c, :].unsqueeze(2).to_broadcast([128, 8, 64]), ALU.mult,
                               ["state", "cdec", ("prevb", i)], ["state"])
                            TT(P, "dve", st2, st2, psf(6), ALU.add, ["state", PSR(6)], ["state"])
                    TT(P, "dve", y1[i][:], psf(5), dsk[i][:], ALU.add, [PSR(5), ("dsk", i)], [("y1", i)])
                    TT(P, "pool", y2[i][:], y1[i][:], siluz[:, c, :], ALU.mult, [("y1", i), ("siluz", c)], [("y2", i)])
                    ACT(P, junk[i][:], y2[i][:], AF.Square, [("y2", i)], [("junk", i), ("ms", i)], scale=float(512 ** -0.5),
                        accum=ms[i][:, 0:1])
                    ACT(P, ms[i][:, 1:2], ms[i][:, 0:1], AF.Ln, [("ms", i)], [("ms2", i)], bias=LN_EPS)
                    ACT(P, ms[i][:, 1:2], ms[i][:, 1:2], AF.Exp, [("ms2", i)], [("ms2", i)], scale=-0.5)
                    ACT(P, yo[i][:], y2[i][:], AF.Copy, [("y2", i), ("ms2", i)], [("yo", i)], scale=ms[i][:, 1:2])

            def ssd_s3(c):
                    i = c % NB
                    tok = slice(c * 128, (c + 1) * 128)
                    pv7 = psb(7)
                    for k in range(4):
                        TR(P, pv7[:, k * 128:(k + 1) * 128], yo[i][:, k * 128:(k + 1) * 128], ident_b[:], [("yo", i), "ident_b"],
                           [PSR(7)], inc=(k == 3))
                    CP(P, "act", mixT[:, 4:8, tok], pv7[:, 0:512].rearrange("p (k t) -> p k t", k=4), [PSR(7)], [("mixT", "s", c)])


            ssd_r(0)
            ssd_s1(0)
            ssd_r(1)
            for c in range(NT):
                if c + 1 < NT:
                    ssd_s1(c + 1)
                    if c + 2 < NT:
                        ssd_r(c + 2)
                ssd_s2(c)
                if c > 0:
                    ssd_s3(c - 1)
            ssd_s3(NT - 1)
            dump("ssdT", mixT[:, 4:8, :], [128, 4, S], [("mixT", "s", c) for c in range(NT)])
            P.barrier()
            A.free(*p3_tmp)
            A.free(xbcT, siluz, dtr)
            if stop_after == 3:
                A.free(mixT, qT, kT, iqT, ikT, Vext, iws)
                continue

            I4 = A.alloc("I4", [128, 4, S], F32)
            junkb = A.alloc("junkb", [128, S], BF16)
            selb = [A.alloc("selb%d" % i, [128, S], BF16) for i in range(2)]
            Rt = [A.alloc("Rt%d" % i, [128, 512], F32) for i in range(4)]
            maskT = [A.alloc("maskT%d" % i, [128, NT, 512], BF16) for i in range(2)]
            Pexp = [A.alloc("Pexp%d" % i, [128, 512], BF16) for i in range(3)]
            Pm = []
            Osb = [A.alloc("Osb%d" % i, [128, 512], F32) for i in range(3)]
            rec = []
            Sden = A.alloc("Sden", [128, 2, 128], F32)
            halfpow = A.alloc("halfpow", [128, NBIS + 2], F32)
            aw = A.alloc("aw", [128, 4, 4], F32)
            sg = A.alloc("sg", [128, 4, 4], F32)
            lo0 = A.alloc("lo0", [128, 4], F32)
            hi0 = A.alloc("hi0", [128, 4], F32)
            wd = A.alloc("wd", [128, 4, NBIS + 2], F32)
            mid = A.alloc("mid", [128, 4], F32)
            cnt = A.alloc("cnt", [128, 4], F32)
            sacc = A.alloc("sacc", [128, 2], F32)
            lhalf = A.alloc("lhalf", [128, 2], F32)
            junka = A.alloc("junka", [128, S], BF16)
            u2 = A.alloc("u2", [128, 4], F32)
            tq = A.alloc("tq", [128, 4], F32)
            thr = A.alloc("thr", [128, 4], F32)
            p2_tmp = maskT + [sacc, lhalf, junka, I4, junkb, Sden, halfpow, aw, sg, lo0, hi0, wd, mid, cnt, u2, tq, thr] + selb + Rt + Pexp + Pm + Osb + rec
            DMA(P, "sp", halfpow[:], halfpow_d, [], ["halfpow"])
            MSET(P, "pool", Sden[:], 0.0, [], ["Sden"])
            MSET(P, "pool", Sden[64:65, 0, :], 1.0, ["Sden"], ["Sden"])
            MSET(P, "pool", Sden[0:1, 1, :], 1.0, ["Sden"], ["Sden"])
            ibank = Rot([0, 1, 2])
            oi_box = [0]

            def gen_IB(g):
                mT = maskT[g % 2]
                nkb = g + 1
                Ls = []
                for b in range(4):
                    qi = 4 * g + b
                    L = (qi + 1) * 128
                    Ls.append(L)
                    qtok = slice(qi * 128, (qi + 1) * 128)
                    STT(P, "dve", aw[:, b, :], iws[:, qi, :], -1.0, iws[:, qi, :], ALU.mult, ALU.max, [], [("aw", b)])
                    TS(P, "dve", sg[:, b, :], iws[:, qi, :], 0.0, 2.0, ALU.is_ge, ALU.mult, [], [("sg", b)])
                    TS(P, "dve", sg[:, b, :], sg[:, b, :], -1.0, None, ALU.add, None, [("sg", b)], [("sg", b)])
                    for kb in range(nkb):
                        w = min(512, L - kb * 512)
                        ks = slice(kb * 512, kb * 512 + w)
                        for h in range(4):
                            bk = ibank.next()
                            MM(P, ps[bk][:, 0:w], iqT[:, h, qtok], ikT[:, ks], True, True, [], [PSR(bk)])
                            ACT(P, Rt[h][:, 0:w], ps[bk][:, 0:w], AF.Relu, [PSR(bk), ("aw", b)], [("Rt", h)],
                                scale=aw[:, b, h:h + 1])
                        TS(P, "dve", I4[:, b, ks], Rt[0][:, 0:w], sg[:, b, 0:1], None, ALU.mult, None,
                           [("Rt", 0), ("sg", b)], [("I4", b)])
                        for h in range(1, 4):
                            STT(P, "dve", I4[:, b, ks], Rt[h][:, 0:w], sg[:, b, h:h + 1], I4[:, b, ks], ALU.mult, ALU.add,
                                [("Rt", h), ("sg", b), ("I4", b)], [("I4", b)])
                        yield 3.0 * w / 512 + 0.5
                    P.op("dve", lambda e, o=lo0[:, b:b + 1], i_=I4[:, b, 0:L]: e.tensor_reduce(o, i_, axis=AX.X, op=ALU.min),
                         [("I4", b)], [("lo0", b)])
                    TT(P, "dve", I4[:, b, qi * 128:(qi + 1) * 128], I4[:, b, qi * 128:(qi + 1) * 128], cneg[:], ALU.add,
                       [("I4", b), ("lo0", b), "cneg"], [("I4", b)])
                    P.op("dve", lambda e, o=hi0[:, b:b + 1], i_=I4[:, b, 0:L]: e.tensor_reduce(o, i_, axis=AX.X, op=ALU.max),
                         [("I4", b)], [("hi0", b)])
                    yield 2.2 * L / 1024 + 0.6
                allb = [("lo0", b) for b in range(4)] + [("hi0", b) for b in range(4)]
                TT(P, "dve", tq[:], hi0[:], lo0[:], ALU.subtract, allb, ["tq"])
                TT(P, "dve", wd[:], tq[:].unsqueeze(2).to_broadcast([128, 4, NBIS + 2]),
                   halfpow[:].unsqueeze(1).to_broadcast([128, 4, NBIS + 2]), ALU.mult, ["tq", "halfpow"], ["wd"])
                TT(P, "dve", mid[:], lo0[:], wd[:, :, 0], ALU.add, allb + ["wd"], ["mid"])
                yield 1.0
                MSET(P, "pool", lhalf[:, 0:1], Ls[2] / 2.0, [], ["lhalf"])
                MSET(P, "pool", lhalf[:, 1:2], Ls[3] / 2.0, [], ["lhalf"])
                for k in range(NBIS):
                    for b in (2, 3):
                        ACT(P, junka[:, 0:Ls[b]], I4[:, b, 0:Ls[b]], AF.Sign, [("I4", b), "mid"], ["junka", ("sacc", b)],
                            bias=mid[:, b:b + 1], scale=-1.0, accum=sacc[:, b - 2:b - 1])
                    for b in (0, 1):
                        TS(P, "dve", junkb[:, 0:Ls[b]], I4[:, b, 0:Ls[b]], mid[:, b:b + 1], 0.0, ALU.is_ge, ALU.add,
                           [("I4", b), "mid"], ["junkb", ("cnt", b)], accum=cnt[:, b:b + 1])
                    STT(P, "dve", cnt[:, 2:4], sacc[:, 0:2], -0.5, lhalf[:, 0:2], ALU.mult, ALU.add,
                        [("sacc", 2), ("sacc", 3), "lhalf"], [("cnt", 2)])
                    TS(P, "dve", u2[:], cnt[:], 256.0, 2.0, ALU.is_ge, ALU.mult, [("cnt", 0), ("cnt", 1), ("cnt", 2)], ["u2"])
                    STT(P, "dve", tq[:], u2[:], -1.0, wd[:, :, k + 1], ALU.add, ALU.mult, ["u2", "wd"], ["tq"])
                    TT(P, "dve", mid[:], mid[:], tq[:], ALU.add, ["mid", "tq"], ["mid"])
                    yield (Ls[0] + Ls[1]) / 960.0 + 1.2
                TT(P, "dve", thr[:], mid[:], wd[:, :, NBIS - 2], ALU.subtract, ["mid", "wd"], ["thr"])
                for b in range(4):
                    qi = 4 * g + b
                    sb_ = selb[b % 2]
                    TS(P, "dve", sb_[:, 0:Ls[b]], I4[:, b, 0:Ls[b]], thr[:, b:b + 1], MASKB, ALU.is_lt, ALU.mult,
                       [("I4", b), "thr"], [("selb", b % 2)])
                    for c0 in range(0, qi + 1, 8):
                        n = min(8, qi + 1 - c0)
                        pv = psb(3)
                        for j in range(n):
                            TR(P, pv[:, j * 128:(j + 1) * 128], sb_[:, (c0 + j) * 128:(c0 + j + 1) * 128], ident_b[:],
                               [("selb", b % 2)], [PSR(3)], inc=(j == n - 1))
                        CP(P, "act", mT[:, c0:c0 + n, b * 128:(b + 1) * 128],
                           pv[:, 0:n * 128].rearrange("p (c q) -> p c q", c=n), [PSR(3)], [("maskT", g % 2, b)])
                    yield Ls[b] / 1024.0 + 1.0

            def gen_A(g):
                mT = maskT[g % 2]
                mres = [("maskT", g % 2, b) for b in range(4)]
                nkc = 4 * g + 4
                items = [(h, c) for h in range(8) for c in range(nkc)]

                def emit_S(idx):
                    h, c = items[idx]
                    cs = max(0, c - 4 * g) * 128
                    qs = slice(g * 512 + cs, g * 512 + 512)
                    bk = 4 + idx % 3
                    MM(P, ps[bk][:, cs:512], kT[:, c * 128:(c + 1) * 128], qT[:, h, qs], True, False, [], [PSR(bk)], inc=False)
                    MM(P, ps[bk][:, cs:512], ident_b[:], mT[:, c, cs:512], False, True, mres, [PSR(bk)])
                    ACT(P, Pexp[idx % 3][:, cs:512], ps[bk][:, cs:512], AF.Exp, [PSR(bk)], [("Pexp", idx % 3)], scale=0.125)

                def emit_PV(idx):
                    h, c = items[idx]
                    par = h % 2
                    rows = slice(64 * par, 64 * par + 64)
                    vcols = slice(0, 128) if par == 0 else slice(64, 192)
                    cs = max(0, c - 4 * g) * 128
                    MM(P, ps[7][:, cs:512], Vext[:, c, vcols], Pexp[idx % 3][:, cs:512], c == 0, c == nkc - 1,
                       [("Pexp", idx % 3)], [PSR(7)], inc=True)
                    if c == nkc - 1:
                        o_ = oi_box[0]
                        ob = Osb[o_ % 3]
                        ores = ("Osb", o_ % 3)
                        dr = slice(64, 65) if par == 0 else slice(0, 1)
                        CP(P, "act", ob[:], psf(7), [PSR(7)], [ores])
                        ACT(P, ob[dr, :], ob[dr, :], AF.Ln, [ores], [ores])
                        ACT(P, ob[dr, :], ob[dr, :], AF.Exp, [ores], [ores], scale=-1.0)

                        def fin1(ob=ob, ores=ores, par=par):
                            MM(P, psf(3), Sden[:, par, :], ob[:], True, True, [ores, "Sden"], [PSR(3)])

                        def fin2(ob=ob, ores=ores, rows=rows, h=h):
                            TT(P, "dve", mixT[rows, h // 2, g * 512:(g + 1) * 512], ob[rows, :], ps[3][rows, :], ALU.mult,
                               [ores, PSR(3)], [("mixT", "a", h, g)])
                        pending.append([2, fin1, fin2])
                        oi_box[0] += 1

                pending = []

                def run_pending(force=False):
                    for p_ in list(pending):
                        p_[0] -= 1
                        if p_[0] <= 0 or force:
                            p_[1]()
                            p_[2]()
                            pending.remove(p_)

                emit_S(0)
                emit_S(1)
                yield 1.4
                for idx in range(len(items)):
                    emit_PV(idx)
                    if idx + 2 < len(items):
                        emit_S(idx + 2)
                    run_pending()
                    yield 0.75
                run_pending(force=True)

            def timed_interleave(*gens):
                gens = [[0.0, g_] for g_ in gens]
                while gens:
                    gens.sort(key=lambda x: x[0])
                    cur = gens[0]
                    try:
                        cur[0] += next(cur[1])
                    except StopIteration:
                        gens.remove(cur)

            gorder = [3, 2, 1, 0]
            timed_interleave(gen_IB(gorder[0]))
            for gi, g in enumerate(gorder):
                if gi + 1 < 4:
                    timed_interleave(gen_A(g), gen_IB(gorder[gi + 1]))
                else:
                    timed_interleave(gen_A(g))
            dump("attT", mixT[:, 0:4, :], [128, 4, S], [("mixT", "a", h, g) for h in range(8) for g in range(4)])
            P.barrier()
            A.free(*p2_tmp)
            A.free(qT, kT, iqT, ikT, Vext, iws)
            if stop_after == 2:
                A.free(mixT)
                continue

            def ln_gen(r, st, junk_, gam, bet, hn, tag, add_eng="pool"):
                ACT(P, junk_[:], r[:], AF.Identity, [(tag, "r")], [(tag, "junk"), (tag, "st")], accum=st[:, 0:1])
                yield
                TS(P, "dve", st[:, 1:2], st[:, 0:1], -1.0 / D, None, ALU.mult, None, [(tag, "st")], [(tag, "st")])
                yield
                ACT(P, junk_[:], r[:], AF.Square, [(tag, "r"), (tag, "st")], [(tag, "junk"), (tag, "st")],
                    bias=st[:, 1:2], accum=st[:, 2:3])
                yield
                ACT(P, st[:, 3:4], st[:, 2:3], AF.Ln, [(tag, "st")], [(tag, "st")], bias=LN_EPS, scale=1.0 / D)
                yield
                ACT(P, st[:, 3:4], st[:, 3:4], AF.Exp, [(tag, "st")], [(tag, "st")], scale=-0.5)
                yield
                TT(P, "dve", st[:, 4:5], st[:, 1:2], st[:, 3:4], ALU.mult, [(tag, "st")], [(tag, "st")])
                yield
                ACT(P, hn[:], r[:], AF.Identity, [(tag, "r"), (tag, "st")], [(tag, "hn")], bias=st[:, 4:5], scale=st[:, 3:4])
                yield
                TT(P, "dve", hn[:], hn[:], gam[:], ALU.mult, [(tag, "hn"), "lng"], [(tag, "hn")])
                yield
                TT(P, add_eng, hn[:], hn[:], bet[:], ALU.add, [(tag, "hn"), "lnb"], [(tag, "hn")])
                yield

            def interleave(*gens):
                gens = list(gens)
                while gens:
                    for g_ in list(gens):
                        try:
                            next(g_)
                        except StopIteration:
                            gens.remove(g_)

            def layer_norm(r, st, junk_, gam, bet, hn, tag):
                interleave(ln_gen(r, st, junk_, gam, bet, hn, tag, add_eng="dve"))

            hT = A.alloc("hT", [128, 8, S], BF16)
            gates = A.alloc("gates", [128, NT, 16], F32)
            wout = A.alloc("wout", [128, 8, D], BF16)
            lng = A.alloc("lng", [128, D], F32)
            lnb = A.alloc("lnb", [128, D], F32)
            xres = [A.alloc("xres%d" % i, [128, D], F32) for i in range(6)]
            rr = [A.alloc("rr%d" % i, [128, D], F32) for i in range(6)]
            hn = [A.alloc("hn%d" % i, [128, D], F32) for i in range(6)]
            lnj = [A.alloc("lnj%d" % i, [128, D], BF16) for i in range(6)]
            stt_ = [A.alloc("st%d" % i, [128, 8], F32) for i in range(6)]
            hT32 = []
            hlo = [A.alloc("hlo%d" % i, [128, 8, 128], BF16) for i in range(2)]
            wr_hl = A.alloc("wr_hl", [128, 2, 8, 20], BF16)
            wr_res = A.alloc("wr_res", [128, 8, 20], F32)
            wr = A.alloc("wr", [128, 8, 20], F32)
            nwc = A.alloc("nwc", [128, 4], F32)
            brp = A.alloc("brp", [128, 20], F32)
            lg = A.alloc("lg", [128, NT, 20], F32)
            rs = [A.alloc("rs%d" % i, [128, NT, 16], F32) for i in range(4)]
            rv = [A.alloc("rv%d" % i, [128, NT], F32) for i in range(8)]
            p4_tmp = [wout, lng, lnb, wr, nwc, brp, lg, wr_hl, wr_res] + hlo + xres + rr + hn + lnj + stt_ + rs + rv
            for c in range(8):
                DMA(P, "pool", wout[:, c, :], wout_d[c * 128:(c + 1) * 128, :], [], [("wout", c)])
            DMA(P, "sp", lng[:], ln1g_d, [], ["lng"])
            DMA(P, "sp", lnb[:], ln1b_d, [], ["lnb"])
            DMA(P, "sp", nwc[:], nwc_d, [], ["nwc"])
            for k in range(4):
                TS(P, "dve", wout[:, 4 + k, :], wout[:, 4 + k, :], nwc[:, k:k + 1], None, ALU.mult, None,
                   [("wout", 4 + k), "nwc"], [("wout", 4 + k)])
            DMA(P, "sp", wr[:], wr_d.rearrange("(c p) f -> p c f", p=128), [], ["wr"])
            DMA(P, "sp", brp[:], br_d, [], ["brp"])
            CP(P, "dve", wr_hl[:, 0, :, :], wr[:], ["wr"], ["wr_h"])
            TT(P, "dve", wr_res[:], wr[:], wr_hl[:, 0, :, :], ALU.subtract, ["wr", "wr_h"], ["wr_res"])
            CP(P, "dve", wr_hl[:, 1, :, :], wr_res[:], ["wr_res"], ["wr_hl"])
            mb = Rot([0, 1, 2, 3])

            def mm_part(t):
                i = t % 6
                tok = slice(t * 128, (t + 1) * 128)
                tag = ("ln1", i)
                DMA(P, "sp", xres[i][:], x_d[seq, tok, :], [], [("xres", i)])
                for half in range(2):
                    b = mb.next()
                    hs = slice(half * 512, half * 512 + 512)
                    for c in range(8):
                        MM(P, psf(b), mixT[:, c, tok], wout[:, c, hs], c == 0, c == 7, [("wout", c)], [PSR(b)], inc=(c == 7))
                    STT(P, "dve", rr[i][:, hs], xres[i][:, hs], ALPHA, psf(b), ALU.mult, ALU.add,
                        [("xres", i), PSR(b)], [(tag, "r")])

            def post_gen(t):
                i = t % 6
                tok = slice(t * 128, (t + 1) * 128)
                tag = ("ln1", i)
                yield from ln_gen(rr[i], stt_[i], lnj[i], lng, lnb, hn[i], tag, add_eng="dve")
                DMA(P, "pool", hscr_d[seq, tok, :], hn[i][:], [(tag, "hn")], [])
                yield

            def tr_part(t):
                i = t % 6
                tok = slice(t * 128, (t + 1) * 128)
                tag = ("ln1", i)
                hres_ = [("hT32a", t % 2), ("hT32b", t % 2)]
                for c in range(8):
                    b = 4 + c // 4
                    TR(P, ps[b][:, (c % 4) * 128:(c % 4 + 1) * 128], hn[i][:, c * 128:(c + 1) * 128], ident_f[:],
                       [(tag, "hn")], [PSR(b)], inc=(c % 4 == 3))
                lo_ = hlo[t % 2]
                CP(P, "act", hT[:, 0:4, tok], ps[4][:].rearrange("p (c t) -> p c t", c=4), [PSR(4)], [("hT", t, 0)])
                CP(P, "dve", hT[:, 4:8, tok], ps[5][:].rearrange("p (c t) -> p c t", c=4), [PSR(5)], [("hT", t, 1)])
                TT(P, "dve", lo_[:, 0:4, :], ps[4][:].rearrange("p (c t) -> p c t", c=4), hT[:, 0:4, tok], ALU.subtract,
                   [PSR(4), ("hT", t, 0)], [hres_[0]])
                TT(P, "dve", lo_[:, 4:8, :], ps[5][:].rearrange("p (c t) -> p c t", c=4), hT[:, 4:8, tok], ALU.subtract,
                   [PSR(5), ("hT", t, 1)], [hres_[1]])
                rb = 6 + t % 2
                rres = hres_ + [("hT", t, 0), ("hT", t, 1), "wr_hl"]
                for c in range(8):
                    MM(P, ps[rb][:, 0:20], hT[:, c, tok], wr_hl[:, 0, c, :], c == 0, False, rres, [PSR(rb)], inc=False)
                    MM(P, ps[rb][:, 0:20], hT[:, c, tok], wr_hl[:, 1, c, :], False, False, rres, [PSR(rb)], inc=False)
                    MM(P, ps[rb][:, 0:20], lo_[:, c, :], wr_hl[:, 0, c, :], False, c == 7, rres, [PSR(rb)], inc=(c == 7))
                TT(P, "dve", lg[:, t, :], ps[rb][:, 0:20], brp[:], ALU.add, [PSR(rb), "brp"], [("lg", t)])

            mm_part(0)
            mm_part(1)
            mm_part(2)
            mm_part(3)
            interleave(post_gen(0), post_gen(1))
            for tp in range(0, NT, 2):
                if tp + 2 < NT:
                    interleave(post_gen(tp + 2), post_gen(tp + 3))
                if tp + 4 < NT:
                    mm_part(tp + 4)
                    mm_part(tp + 5)
                tr_part(tp)
                tr_part(tp + 1)
            dump("hT", hT[:], [128, 8, S], [("hT", t, k_) for t in range(NT) for k_ in range(2)])
            lgr = [("lg", t) for t in range(NT)]
            gl = lg[:, :, 0:4]
            gmax, gsum, gprob, m1, m2, dd, w1, w2 = [rv[i] for i in range(8)]
            ge, ohg, pen = rs[0][:, :, 0:4], rs[1][:, :, 0:4], rs[2][:, :, 0:4]

            def red(out, in_, op, r, w):
                P.op("dve", lambda e: e.tensor_reduce(out, in_, axis=AX.X, op=op), r, w)

            def bc(v, n):
                return v[:].unsqueeze(2).to_broadcast([128, NT, n])
            red(gmax[:], gl, ALU.max, lgr, ["gmax"])
            TT(P, "dve", ge, gl, bc(gmax, 4), ALU.subtract, lgr + ["gmax"], ["ge"])
            ACT(P, ge, ge, AF.Exp, ["ge"], ["ge"])
            red(gsum[:], ge, ALU.add, ["ge"], ["gsum"])
            P.op("dve", lambda e: e.reciprocal(gprob[:], gsum[:]), ["gsum"], ["gprob"])
            TT(P, "dve", ohg, gl, bc(gmax, 4), ALU.is_ge, lgr + ["gmax"], ["ohg"])
            TS(P, "dve", pen, ohg, -1.0, 1.0e4, ALU.add, ALU.mult, ["ohg"], ["pen"])
            mel, oh1, mel2, oh2 = rs[3], rs[0], rs[1], rs[2]
            for gq in range(4):
                TT(P, "dve", mel[:, :, gq * 4:(gq + 1) * 4], lg[:, :, 4 + gq * 4:8 + gq * 4],
                   pen[:, :, gq:gq + 1].to_broadcast([128, NT, 4]), ALU.add, lgr + ["pen"], ["mel"])
            red(m1[:], mel[:], ALU.max, ["mel"], ["m1"])
            TT(P, "dve", oh1[:], mel[:], bc(m1, 16), ALU.is_ge, ["mel", "m1", "ge"], ["oh1"])
            STT(P, "dve", mel2[:], oh1[:], -1.0e4, mel[:], ALU.mult, ALU.add, ["oh1", "mel", "ohg"], ["mel2"])
            red(m2[:], mel2[:], ALU.max, ["mel2"], ["m2"])
            TT(P, "dve", oh2[:], mel2[:], bc(m2, 16), ALU.is_ge, ["mel2", "m2", "pen"], ["oh2"])
            TT(P, "dve", dd[:], m2[:], m1[:], ALU.subtract, ["m1", "m2"], ["dd"])
            ACT(P, dd[:], dd[:], AF.Exp, ["dd"], ["dd"])
            TS(P, "dve", w1[:], dd[:], 1.0, None, ALU.add, None, ["dd"], ["w1"])
            P.op("dve", lambda e: e.reciprocal(w1[:], w1[:]), ["w1"], ["w1"])
            TT(P, "dve", w2[:], dd[:], w1[:], ALU.mult, ["dd", "w1"], ["w2"])
            TT(P, "dve", w1[:], w1[:], gprob[:], ALU.mult, ["w1", "gprob", "w2"], ["w1"])
            TT(P, "dve", w2[:], w2[:], gprob[:], ALU.mult, ["w2", "gprob"], ["w2"])
            TT(P, "dve", oh1[:], oh1[:], bc(w1, 16), ALU.mult, ["oh1", "w1", "mel2"], ["oh1"])
            TT(P, "dve", oh2[:], oh2[:], bc(w2, 16), ALU.mult, ["oh2", "w2"], ["oh2"])
            TT(P, "dve", gates[:], oh1[:], oh2[:], ALU.add, ["oh1", "oh2"], ["gates"])
            dump("gates", gates[:], [128, NT, 16], ["gates"])
            P.barrier()
            A.free(*p4_tmp)
            A.free(mixT)
            if stop_after == 4:
                A.free(hT, gates)
                continue

            selc = A.alloc("selc", [16, 16 * 128], F32)
            gT = A.alloc("gT", [16, 1024], F32)
            wdn = A.alloc("wdn", [128, 16, D], BF16)
            hid = A.alloc("hid", [128, 16, 1024], BF16)
            yacc = A.alloc("yacc", [128, 8, D], F32)
            Wgu = [A.alloc("Wgu%d" % i, [128, 8, 512], BF16) for i in range(2)]
            sgt = [A.alloc("sgt%d" % i, [128, 512], BF16) for i in range(2)]
            ttm = [A.alloc("ttm%d" % i, [128, 512], BF16) for i in range(2)]
            lng = A.alloc("lng2", [128, D], F32)
            lnb = A.alloc("lnb2", [128, D], F32)
            hres = [A.alloc("hres%d" % i, [128, D], F32) for i in range(2)]
            r2 = [A.alloc("r2_%d" % i, [128, D], F32) for i in range(2)]
            on = [A.alloc("on%d" % i, [128, D], F32) for i in range(2)]
            lnj = [A.alloc("lnj2_%d" % i, [128, D], BF16) for i in range(2)]
            stt_ = [A.alloc("st2_%d" % i, [128, 8], F32) for i in range(2)]
            p5_tmp = [selc, gT, wdn, hid, yacc, lng, lnb] + Wgu + sgt + ttm + hres + r2 + on + lnj + stt_
            DMA(P, "sp", selc[:], sel_d, [], ["selc"])
            DMA(P, "sp", lng[:], ln2g_d, [], ["lng"])
            DMA(P, "sp", lnb[:], ln2b_d, [], ["lnb"])
            gub = Rot([2, 3, 4, 5])
            geb = Rot([0, 1])
            dnb = Rot([6, 7])
            wi = 0
            si = 0
            li = 0
            for hf in range(2):
                for j4 in range(2):
                    for k in range(4):
                        t = hf * 8 + j4 * 4 + k
                        TR(P, ps[j4][0:16, k * 128:(k + 1) * 128], gates[:, t, :], ident_f[:], ["gates"], [PSR(j4)], inc=(k == 3))
                    CP(P, "dve", gT[:, j4 * 512:(j4 + 1) * 512], ps[j4][0:16, :], [PSR(j4)], [("gT", j4)])
                for eh in range(2):
                    for el in range(8):
                        e = eh * 8 + el
                        W = Wgu[wi % 2]
                        wres = ("Wgu", wi % 2)
                        DMA(P, "pool", W[:, :, 0:256], wg_d[e].rearrange("(c p) f -> p c f", p=128), [], [wres])
                        DMA(P, "pool", W[:, :, 256:512], wu_d[e].rearrange("(c p) f -> p c f", p=128), [], [wres])
                        DMA(P, "pool", wdn[:, 2 * el:2 * el + 2, :], wd_d[e].rearrange("(j p) d -> p j d", p=128), [],
                            [("wdn", el)])
                        wi += 1
                        for sub in range(2):
                            ts_ = slice(hf * 1024 + sub * 512, hf * 1024 + sub * 512 + 512)
                            loc = slice(sub * 512, sub * 512 + 512)
                            gb_ = geb.next()
                            MM(P, psf(gb_), selc[:, e * 128:(e + 1) * 128], gT[:, loc], True, True,
                               ["selc", ("gT", sub)], [PSR(gb_)])
                            for j in range(2):
                                bg, bu = gub.next(), gub.next()
                                for c in range(8):
                                    MM(P, psf(bg), W[:, c, j * 128:(j + 1) * 128], hT[:, c, ts_], c == 0, c == 7,
                                       [wres], [PSR(bg)], inc=(c == 7))
                                for c in range(8):
                                    MM(P, psf(bu), W[:, c, 256 + j * 128:256 + (j + 1) * 128], hT[:, c, ts_], c == 0, c == 7,
                                       [wres], [PSR(bu)], inc=(c == 7))
                                sg_, tm_ = sgt[si % 2], ttm[si % 2]
                                ACT(P, sg_[:], psf(bg), AF.Silu, [PSR(bg)], [("sgt", si % 2)])
                                TT(P, "dve", tm_[:], sg_[:], psf(bu), ALU.mult, [("sgt", si % 2), PSR(bu)], [("ttm", si % 2)])
                                TT(P, "dve", hid[:, el * 2 + j, loc], tm_[:], psf(gb_), ALU.mult,
                                   [("ttm", si % 2), PSR(gb_)], [("hid", el * 2 + j, sub)])
                                si += 1
                    hres_all = [("hid", k, sub) for k in range(16) for sub in range(2)] + [("wdn", el) for el in range(8)]
                    for lt in range(8):
                        t = hf * 8 + lt
                        tok = slice(t * 128, (t + 1) * 128)
                        i = li % 2
                        tag = ("ln2", i)
                        if eh == 1 and lt == 0:
                            for l2 in range(2):
                                t2 = hf * 8 + l2
                                DMA(P, "sp", hres[(li + l2) % 2][:], hscr_d[seq, t2 * 128:(t2 + 1) * 128, :], [],
                                    [("hres", (li + l2) % 2)])
                        for dh in range(2):
                            b = dnb.next()
                            hs = slice(dh * 512, dh * 512 + 512)
                            for k in range(16):
                                MM(P, psf(b), hid[:, k, lt * 128:(lt + 1) * 128], wdn[:, k, hs], k == 0, k == 15,
                                   hres_all, [PSR(b)], inc=(k == 15))
                            if eh == 0:
                                CP(P, "act", yacc[:, lt, hs], psf(b), [PSR(b)], [("yacc", lt)])
                            else:
                                TT(P, "dve", r2[i][:, hs], yacc[:, lt, hs], psf(b), ALU.add, [("yacc", lt), PSR(b)], [(tag, "r0")])
                                STT(P, "dve", r2[i][:, hs], hres[i][:, hs], ALPHA, r2[i][:, hs], ALU.mult, ALU.add,
                                    [("hres", i), (tag, "r0")], [(tag, "r")])
                        if eh == 1:
                            if lt + 2 < 8:
                                t2 = hf * 8 + lt + 2
                                DMA(P, "sp", hres[i][:], hscr_d[seq, t2 * 128:(t2 + 1) * 128, :], [], [("hres", i)])
                            layer_norm(r2[i], stt_[i], lnj[i], lng, lnb, on[i], tag)
                            DMA(P, "sp", out_d[seq, tok, :], on[i][:], [(tag, "hn")], [])
                            li += 1
            P.barrier()
            A.free(*p5_tmp)
            A.free(hT, gates)
    P.emit()
    return nc, dumps


def _in_map(inp, core, consts, wfm, wtm):
    m = dict(consts)
    m["x"] = np.ascontiguousarray(inp["x"][SEQ_PER_CORE * core:SEQ_PER_CORE * (core + 1)], dtype=np.float32)
    m["wfm"] = wfm
    m["wtm"] = wtm
    cw = inp["conv_w"][0]
    m["convw"] = np.ascontiguousarray(cw.reshape(4, 6, 128).transpose(2, 1, 0).reshape(128, 24))
    m["convb"] = np.ascontiguousarray(inp["conv_b"][0].reshape(6, 128).T)
    m["dtb"] = _rep(inp["dt_bias"][0])
    m["alog"] = _rep(inp["a_log"][0])
    m["dskip"] = _rep(np.repeat(inp["d_skip"][0], 64))
    m["nw"] = _rep(inp["ssd_norm_w"][0])
    m["nwc"] = np.ascontiguousarray(inp["ssd_norm_w"][0].reshape(4, 128).T)
    m["wout"] = np.ascontiguousarray(inp["w_out"][0])
    m["ln1g"] = _rep(inp["ln1_g"][0])
    m["ln1b"] = _rep(inp["ln1_b"][0])
    m["ln2g"] = _rep(inp["ln2_g"][0])
    m["ln2b"] = _rep(inp["ln2_b"][0])
    m["wr"] = np.ascontiguousarray(np.concatenate([inp["w_route_group"][0], inp["w_route_expert"][0]], 1))
    m["br"] = _rep(np.concatenate([inp["b_route_group"][0], inp["b_route_expert"][0]]))
    m["wg"] = np.ascontiguousarray(inp["w_gate"][0])
    m["wu"] = np.ascontiguousarray(inp["w_up"][0])
    m["wd"] = np.ascontiguousarray(inp["w_down"][0])
    return m


def kernel(**inputs):
    inp = {k: np.asarray(v, dtype=np.float32) for k, v in inputs.items()}
    wfm, wtm = _layout_w_in(inp["w_in"])
    consts = _consts()
    nc, _ = build()
    in_maps = [_in_map(inp, c, consts, wfm, wtm) for c in range(NCORES)]
    res = run_bass_kernel_spmd(nc, in_maps, core_ids=list(range(NCORES)))
    out = np.concatenate([np.asarray(res.results[c]["out"], dtype=np.float32) for c in range(NCORES)], axis=0)
    return out
```

```python
import numpy as np
import concourse.bass as bass
import concourse.mybir as mybir
from concourse.bass_utils import run_bass_kernel_spmd

F32 = mybir.dt.float32
BF16 = mybir.dt.bfloat16
ALU = mybir.AluOpType
AF = mybir.ActivationFunctionType
AX = mybir.AxisListType

NCORES = 8
S = 2048
D = 1024
NT = S // 128
SEQ_PER_CORE = 2
ALPHA = 2.0 ** 0.25
LN_EPS = 1e-5
IDX_SCALE = (4 ** -0.5) * (64 ** -0.5)
NF = 22
TMW = 588
NBIS = 14
NEG = -1.0e30
MASKB = -262144.0


class Prog:
    CE = ("pe", "act", "dve", "pool")

    def __init__(self, nc, kdma=12):
        self.nc = nc
        self.ops = {e: [] for e in ("pe", "act", "dve", "pool", "sp")}
        self.cnt = {e: 0 for e in self.CE}
        self.last_w = {}
        self.readers = {}
        self.seen = {e: {} for e in self.ops}
        self.kdma = kdma
        self.ring = {"sp": [0] * kdma, "pool": [0] * kdma}
        self.ring_next = {"sp": 0, "pool": 0}
        self.nops = 0
        self.floor = {e: {} for e in self.ops}
        self.ps_last = {}

    def barrier(self):
        cur = {e: self.cnt[e] for e in self.CE if self.cnt[e] > 0}
        for q in ("sp", "pool"):
            for k in range(self.kdma):
                if self.ring[q][k] > 0:
                    cur["d%s%d" % (q, k)] = 16 * self.ring[q][k]
        for e in self.floor:
            self.floor[e] = dict(cur)

    def _deps(self, reads, writes):
        deps = {}

        def add(tok):
            if tok is None:
                return
            k, v = tok
            if deps.get(k, 0) < v:
                deps[k] = v
        for r in reads:
            add(self.last_w.get(r))
        for w in writes:
            add(self.last_w.get(w))
            for k, v in self.readers.get(w, {}).items():
                add((k, v))
        return deps

    def _commit(self, tok, reads, writes):
        for r in reads:
            d = self.readers.setdefault(r, {})
            if d.get(tok[0], 0) < tok[1]:
                d[tok[0]] = tok[1]
        for w in writes:
            self.last_w[w] = tok
            self.readers[w] = {}

    def _waits(self, eng, deps):
        waits = []
        fl = self.floor[eng]
        if fl:
            for k, v in fl.items():
                if deps.get(k, 0) < v:
                    deps[k] = v
            self.floor[eng] = {}
        for k, v in deps.items():
            if k == "pe" and eng == "pe":
                continue
            if self.seen[eng].get(k, 0) >= v:
                continue
            self.seen[eng][k] = v
            waits.append((k, v))
        return waits

    def op(self, eng, fn, reads=(), writes=(), inc=True):
        assert eng in self.CE
        if eng != "pe":
            assert inc
        deps = self._deps(reads, writes)
        idx = self.cnt[eng] + 1
        if inc:
            self.cnt[eng] = idx
        tok = (eng, idx)
        for r in reads:
            if isinstance(r, tuple) and r[0] == "ps":
                prev = self.ps_last.get(r[1])
                if prev is not None and prev[0] != eng and deps.get(prev[0], 0) < prev[1]:
                    deps[prev[0]] = prev[1]
                self.ps_last[r[1]] = tok
        waits = self._waits(eng, deps)
        self.ops[eng].append((waits, fn, (eng, 1) if inc else None))
        self._commit(tok, reads, writes)
        self.nops += 1
        return tok

    def dma(self, q, fn, reads=(), writes=()):
        deps = self._deps(reads, writes)
        k = self.ring_next[q] % self.kdma
        self.ring_next[q] += 1
        key = "d%s%d" % (q, k)
        if self.ring[q][k] > 0:
            v = 16 * self.ring[q][k]
            if deps.get(key, 0) < v:
                deps[key] = v
        waits = self._waits(q, deps)
        self.ring[q][k] += 1
        tok = (key, 16 * self.ring[q][k])
        self.ops[q].append((waits, fn, (key, 16)))
        self._commit(tok, reads, writes)
        self.nops += 1
        return tok

    def emit(self):
        nc = self.nc
        names = list(self.CE) + ["d%s%d" % (q, k) for q in ("sp", "pool") for k in range(self.kdma)]
        fin = []
        for q in ("sp", "pool"):
            for k in range(self.kdma):
                if self.ring[q][k] > 0:
                    fin.append(("d%s%d" % (q, k), 16 * self.ring[q][k]))
        for e in self.CE:
            if self.cnt[e] > 0:
                fin.append((e, self.cnt[e]))
        ops = self.ops
        import contextlib
        with contextlib.ExitStack() as st:
            sems = {n: st.enter_context(nc.semaphore("s_" + n)) for n in names}
            block = st.enter_context(nc.Block())

            def replay(eng_name):
                def run(e):
                    for waits, fn, inc in ops[eng_name]:
                        for k, v in waits:
                            e.wait_ge(sems[k], v)
                        ins = fn(e)
                        if inc is not None:
                            ins.then_inc(sems[inc[0]], inc[1])
                    if eng_name == "sp":
                        for k, v in fin:
                            e.wait_ge(sems[k], v)
                return run
            block.sync(replay("sp"))
            block.tensor(replay("pe"))
            block.scalar(replay("act"))
            block.vector(replay("dve"))
            block.gpsimd(replay("pool"))


def _rot(cols):
    return np.concatenate([cols[32:], cols[:32]])


def _layout_w_in(w_in):
    w = w_in[0]
    oq, ok, ov, oiq, oik, oiw, oz, oxbc, odt = 0, 512, 576, 640, 896, 960, 964, 1476, 2244
    tiles = []
    qh = [np.arange(oq + 64 * h, oq + 64 * (h + 1)) for h in range(8)]
    for p in range(4):
        tiles.append(np.concatenate([qh[2 * p], qh[2 * p + 1]]))
    for p in range(4):
        tiles.append(np.concatenate([_rot(qh[2 * p]), _rot(qh[2 * p + 1])]))
    kc = np.arange(ok, ok + 64)
    tiles.append(np.concatenate([kc, kc]))
    tiles.append(np.concatenate([_rot(kc), _rot(kc)]))
    ih = [np.arange(oiq + 64 * h, oiq + 64 * (h + 1)) for h in range(4)]
    for p in range(2):
        tiles.append(np.concatenate([ih[2 * p], ih[2 * p + 1]]))
    for p in range(2):
        tiles.append(np.concatenate([_rot(ih[2 * p]), _rot(ih[2 * p + 1])]))
    ikc = np.arange(oik, oik + 64)
    tiles.append(np.concatenate([ikc, ikc]))
    tiles.append(np.concatenate([_rot(ikc), _rot(ikc)]))
    for t in range(6):
        tiles.append(np.arange(oxbc + 128 * t, oxbc + 128 * (t + 1)))
    fm_cols = np.concatenate(tiles)
    assert fm_cols.shape[0] == NF * 128
    tm_cols = np.concatenate([np.arange(oz, oz + 512), np.arange(ov, ov + 64), np.arange(oiw, oiw + 4),
                              np.arange(odt, odt + 8)])
    assert tm_cols.shape[0] == TMW
    return np.ascontiguousarray(w[:, fm_cols]), np.ascontiguousarray(w[:, tm_cols])


def _consts():
    c = {}
    c["ident"] = np.eye(128, dtype=np.float32)
    s_ = np.arange(128)
    c["tri"] = (s_[:, None] <= s_[None, :]).astype(np.float32)
    c["cneg"] = np.where(s_[None, :] <= s_[:, None], 0.0, NEG).astype(np.float32)
    inv = 10000.0 ** (-np.arange(0, 64, 2, dtype=np.float32) / 64.0)
    ang = np.arange(S, dtype=np.float32)[:, None] * inv[None, :]
    cos, sin = np.cos(ang).astype(np.float32), np.sin(ang).astype(np.float32)
    cosF = np.concatenate([cos, cos], 1).T
    sinS = np.concatenate([-sin, sin], 1).T
    c["cosF"] = np.ascontiguousarray(np.concatenate([cosF, cosF], 0))
    c["sinS"] = np.ascontiguousarray(np.concatenate([sinS, sinS], 0))
    sel = np.zeros((16, 16, 128), np.float32)
    for e in range(16):
        sel[e, e, :] = 1.0
    c["sel"] = sel.reshape(16, 16 * 128)
    c["halfpow"] = np.broadcast_to((0.5 ** np.arange(1, NBIS + 3, dtype=np.float32))[None, :], (128, NBIS + 2)).copy()
    return c


def _rep(v, n=128):
    return np.ascontiguousarray(np.broadcast_to(np.asarray(v, np.float32).reshape(1, -1), (n, np.asarray(v).size)))


def MM(P, out, lhsT, rhs, start, stop, r, w, inc=True):
    return P.op("pe", lambda e: e.matmul(out, lhsT=lhsT, rhs=rhs, start=start, stop=stop), r, w, inc)


def TR(P, out, in_, ident, r, w, inc=True):
    return P.op("pe", lambda e: e.transpose(out, in_, ident), r, w, inc)


def ACT(P, out, in_, func, r, w, bias=0.0, scale=1.0, accum=None):
    if accum is None:
        return P.op("act", lambda e: e.activation(out=out, in_=in_, func=func, bias=bias, scale=scale), r, w)
    return P.op("act", lambda e: e.activation(out=out, in_=in_, func=func, bias=bias, scale=scale, accum_out=accum), r, w)


def TT(P, eng, out, a, b, op, r, w):
    return P.op(eng, lambda e: e.tensor_tensor(out, a, b, op), r, w)


def TS(P, eng, out, a, s1, s2, op0, op1, r, w, accum=None):
    if op1 is None:
        return P.op(eng, lambda e: e.tensor_scalar(out, a, s1, None, op0=op0), r, w)
    if accum is None:
        return P.op(eng, lambda e: e.tensor_scalar(out, a, s1, s2, op0=op0, op1=op1), r, w)
    return P.op(eng, lambda e: e.tensor_scalar(out, a, s1, s2, op0=op0, op1=op1, accum_out=accum), r, w)


def STT(P, eng, out, a, scalar, b, op0, op1, r, w):
    return P.op(eng, lambda e: e.scalar_tensor_tensor(out, a, scalar, b, op0=op0, op1=op1), r, w)


def CP(P, eng, out, in_, r, w):
    if eng == "act":
        return P.op("act", lambda e: e.copy(out, in_), r, w)
    return P.op(eng, lambda e: e.tensor_copy(out, in_), r, w)


def MSET(P, eng, ap, val, r, w):
    return P.op(eng, lambda e: e.memset(ap, val), r, w)


def DMA(P, q, out, in_, r, w):
    return P.dma(q, lambda e: e.dma_start(out=out, in_=in_), r, w)


class Rot:
    def __init__(self, items):
        self.items = list(items)
        self.i = 0

    def next(self):
        v = self.items[self.i % len(self.items)]
        self.i += 1
        return v


class Arena:
    def __init__(self, nc, lo=16512, hi=229344):
        self.nc = nc
        self.free_list = [(lo, hi)]
        self.live = {}
        self.uid = 0
        self.cache = {}

    def alloc(self, name, shape, dt):
        n = 1
        for d in shape[1:]:
            n *= d
        size = n * (4 if dt == F32 else 2)
        size = (size + 31) // 32 * 32
        for i, (a, b) in enumerate(self.free_list):
            if b - a >= size:
                self.free_list[i] = (a + size, b)
                if a + size == b:
                    self.free_list.pop(i)
                key = (name, tuple(shape), str(dt), a)
                h = self.cache.get(key)
                if h is None:
                    self.uid += 1
                    h = self.nc.alloc_sbuf_tensor_at("sb%d_%s" % (self.uid, name), list(shape), dt, offset=a)
                    self.cache[key] = h
                self.live[id(h)] = (a, size, h)
                return h
        raise RuntimeError("arena out of SBUF for %s (%d bytes); free=%s" % (name, size, self.free_list))

    def free(self, *hs):
        for h in hs:
            a, size, _ = self.live.pop(id(h))
            self.free_list.append((a, a + size))
        self.free_list.sort()
        merged = []
        for a, b in self.free_list:
            if merged and merged[-1][1] == a:
                merged[-1] = (merged[-1][0], b)
            else:
                merged.append((a, b))
        self.free_list = merged


def build(dbg=(), nseq=SEQ_PER_CORE, stop_after=None):
    nc = bass.Bass("TRN2", target_bir_lowering=False)
    P = Prog(nc)
    A = Arena(nc)
    dbg = set(dbg)
    dumps = {}

    def din(name, shape):
        return nc.dram_tensor(name, list(shape), F32, kind="ExternalInput").ap()

    x_d = din("x", [SEQ_PER_CORE, S, D])
    wfm_d = din("wfm", [D, NF * 128])
    wtm_d = din("wtm", [D, TMW])
    ident_d = din("ident", [128, 128])
    tri_d = din("tri", [128, 128])
    cneg_d = din("cneg", [128, 128])
    cosF_d = din("cosF", [128, S])
    sinS_d = din("sinS", [128, S])
    sel_d = din("sel", [16, 16 * 128])
    halfpow_d = din("halfpow", [128, NBIS + 2])
    convw_d = din("convw", [128, 24])
    convb_d = din("convb", [128, 6])
    dtb_d = din("dtb", [128, 8])
    alog_d = din("alog", [128, 8])
    dskip_d = din("dskip", [128, 512])
    nw_d = din("nw", [128, 512])
    nwc_d = din("nwc", [128, 4])
    wout_d = din("wout", [D, D])
    ln1g_d = din("ln1g", [128, D])
    ln1b_d = din("ln1b", [128, D])
    ln2g_d = din("ln2g", [128, D])
    ln2b_d = din("ln2b", [128, D])
    wr_d = din("wr", [D, 20])
    br_d = din("br", [128, 20])
    wg_d = din("wg", [16, D, 256])
    wu_d = din("wu", [16, D, 256])
    wd_d = din("wd", [16, 256, D])
    out_d = nc.dram_tensor("out", [SEQ_PER_CORE, S, D], F32, kind="ExternalOutput").ap()
    hscr_d = nc.dram_tensor("hscr", [SEQ_PER_CORE, S, D], F32, kind="Internal").ap()

    import contextlib
    with contextlib.ExitStack() as es:
        ps = [es.enter_context(nc.psum_tensor("ps%d" % i, [128, 512], F32)) for i in range(8)]

        def psf(b):
            return ps[b][:]

        def psb(b):
            return ps[b][:].bitcast(BF16)

        def PSR(b):
            return ("ps", b)

        def dump(name, ap, shape, reads, dt=None):
            if name not in dbg:
                return
            t = nc.dram_tensor("dbg_" + name, list(shape), dt or ap.dtype, kind="ExternalOutput").ap()
            dumps[name] = t
            DMA(P, "sp", t, ap, reads, [])

        ident_f = A.alloc("ident_f", [128, 128], F32)
        ident_b = A.alloc("ident_b", [128, 128], BF16)
        tri_f = A.alloc("tri_f", [128, 128], F32)
        tri_b = A.alloc("tri_b", [128, 128], BF16)
        cneg = A.alloc("cneg", [128, 128], F32)
        ones_f = A.alloc("ones_f", [128, 128], F32)
        DMA(P, "sp", ident_f[:], ident_d, [], ["ident_f"])
        DMA(P, "pool", ident_b[:], ident_d, [], ["ident_b"])
        DMA(P, "sp", tri_f[:], tri_d, [], ["tri_f"])
        DMA(P, "pool", tri_b[:], tri_d, [], ["tri_b"])
        DMA(P, "sp", cneg[:], cneg_d, [], ["cneg"])
        MSET(P, "dve", ones_f[:], 1.0, [], ["ones_f"])

        wtm_pref = [None]
        for seq in range(nseq):
            qT = A.alloc("qz", [128, 8, S], BF16)
            kT = A.alloc("kT", [128, S], BF16)
            iqT = A.alloc("iqz", [128, 4, S], BF16)
            ikT = A.alloc("ikT", [128, S], BF16)
            Vext = A.alloc("Vext", [128, NT, 192], BF16)
            iws = A.alloc("iws", [128, NT, 4], F32)
            xbcT = A.alloc("xbcT", [128, 6, S], BF16)
            siluz = A.alloc("siluz", [128, NT, 512], BF16)
            dtr = A.alloc("dtr", [128, NT, 8], F32)

            xT = A.alloc("xT", [128, 8, S], BF16)
            if wtm_pref[0] is not None:
                wtm = wtm_pref[0]
                wtm_pref[0] = None
            else:
                wtm = A.alloc("wtm", [128, 8, TMW], BF16)
                for c in range(8):
                    DMA(P, "pool", wtm[:, c, :], wtm_d[c * 128:(c + 1) * 128, :], [], [("wtm", c)])
            xb = [A.alloc("xb%d" % i, [128, D], BF16) for i in range(4)]
            MSET(P, "pool", qT[:], 0.0, [], ["qz0"])
            MSET(P, "pool", iqT[:], 0.0, [], ["iqz0"])
            MSET(P, "pool", Vext[:, :, 64:128], 0.0, [], [("Vext0",)])
            MSET(P, "pool", Vext[:, :, 64:65], 1.0, [("Vext0",)], [("Vext0",)])
            tr_banks = Rot([0, 1])
            fm_banks = Rot([4, 5, 6, 7])
            cp_eng = Rot(["act", "dve"])
            def a_dma(t):
                tok = slice(t * 128, (t + 1) * 128)
                DMA(P, "pool", xb[t % 4][:], x_d[seq, tok, :], [], [("xb", t % 4)])

            def a_tr(t):
                tok = slice(t * 128, (t + 1) * 128)
                xbuf = xb[t % 4]
                b = tr_banks.next()
                pv = psb(b)
                for c in range(8):
                    TR(P, pv[:, c * 128:(c + 1) * 128], xbuf[:, c * 128:(c + 1) * 128], ident_b[:],
                       [("xb", t % 4), "ident_b"], [PSR(b)], inc=(c == 7))
                CP(P, cp_eng.next(), xT[:, :, tok], pv.rearrange("p (c t) -> p c t", c=8),
                   [PSR(b)], [("xT", t)])

            def a_mm(t):
                tok = slice(t * 128, (t + 1) * 128)
                bz, bv = (2, 3) if t % 2 == 0 else (4, 5)
                for c in range(8):
                    MM(P, psf(bz), xT[:, c, tok], wtm[:, c, 0:512], c == 0, c == 7,
                       [("xT", t), ("wtm", c)], [PSR(bz)], inc=(c == 7))
                for c in range(8):
                    MM(P, ps[bv][:, 0:76], xT[:, c, tok], wtm[:, c, 512:588], c == 0, c == 7,
                       [("xT", t), ("wtm", c)], [PSR(bv)], inc=(c == 7))
                ACT(P, siluz[:, t, :], psf(bz), AF.Silu, [PSR(bz)], [("siluz", t)])
                CP(P, "dve", Vext[:, t, 0:64], ps[bv][:, 0:64], [PSR(bv)], [("Vext", t)])
                CP(P, "dve", Vext[:, t, 128:192], ps[bv][:, 0:64], [PSR(bv)], [("Vext", t)])
                TS(P, "dve", iws[:, t, :], ps[bv][:, 64:68], IDX_SCALE, None, ALU.mult, None,
                   [PSR(bv)], [("iws", t)])
                CP(P, "dve", dtr[:, t, :], ps[bv][:, 68:76], [PSR(bv)], [("dtr", t)])

            for t in range(3):
                a_dma(t)
            a_tr(0)
            for t in range(NT):
                if t + 3 < NT:
                    a_dma(t + 3)
                if t + 1 < NT:
                    a_tr(t + 1)
                a_mm(t)

            P.barrier()
            A.free(wtm, *xb)
            cosF = A.alloc("cosF", [128, S], F32)
            sinS = A.alloc("sinS", [128, S], F32)
            convw = A.alloc("convw", [128, 24], F32)
            convb = A.alloc("convb", [128, 6], F32)
            diagw = A.alloc("diagw", [128, 24, 128], BF16)
            uT = A.alloc("uT", [128, 6, S + 3], BF16)
            rt1 = [A.alloc("rt1_%d" % i, [128, 512], F32) for i in range(2)]
            rt2 = [A.alloc("rt2_%d" % i, [128, 512], F32) for i in range(2)]
            wsl = [A.alloc("wsl%d" % i, [128, 8, 128], BF16) for i in range(4)]
            p1_tmp = [xT, cosF, sinS, convw, convb, diagw, uT] + rt1 + rt2 + wsl
            DMA(P, "sp", cosF[:], cosF_d, [], ["cosF"])
            DMA(P, "sp", sinS[:], sinS_d, [], ["sinS"])
            DMA(P, "sp", convw[:], convw_d, [], ["convw"])
            DMA(P, "sp", convb[:], convb_d, [], ["convb"])
            for i in range(24):
                TS(P, "dve", diagw[:, i, :], ident_f[:], convw[:, i:i + 1], None, ALU.mult, None,
                   ["ident_f", "convw"], [("diagw", i)])
            MSET(P, "pool", uT[:, :, 0:3], 0.0, [], [("uT", -1)])
            wsi = [0]

            worder = [0, 4, 1, 5, 2, 6, 3, 7, 8, 9, 10, 12, 11, 13, 14, 15, 16, 17, 18, 19, 20, 21]
            wslot = {}

            def prefetch(n):
                for _ in range(n):
                    if wsi[0] >= len(worder):
                        return
                    f = worder[wsi[0]]
                    k = wsi[0] % 4
                    wsi[0] += 1
                    wslot[f] = k
                    DMA(P, "pool", wsl[k][:], wfm_d[:, f * 128:(f + 1) * 128].rearrange("(c p) f -> p c f", p=128),
                        [], [("wsl", k)])

            def load_w(f):
                return wslot[f]

            prefetch(4)

            def fm_proj(k, tg, b):
                tokg = slice(tg * 512, tg * 512 + 512)
                for c in range(8):
                    MM(P, psf(b), wsl[k][:, c, :], xT[:, c, tokg], c == 0, c == 7,
                       [("xT", tg * 4 + i) for i in range(4)] + [("wsl", k)], [PSR(b)], inc=(c == 7))

            pairs = [(0, 4, (qT, 0), "qT0"), (1, 5, (qT, 1), "qT1"), (2, 6, (qT, 2), "qT2"), (3, 7, (qT, 3), "qT3"),
                     (8, 9, kT, "kT"), (10, 12, (iqT, 0), "iqT0"), (11, 13, (iqT, 1), "iqT1"), (14, 15, ikT, "ikT")]
            rti = 0
            for fa, fr, dstf, dname in pairs:
                ka, kr = load_w(fa), load_w(fr)
                for tg in range(4):
                    tokg = slice(tg * 512, tg * 512 + 512)
                    ba, br = fm_banks.next(), fm_banks.next()
                    fm_proj(ka, tg, ba)
                    fm_proj(kr, tg, br)
                    r1, r2 = rt1[rti % 2], rt2[rti % 2]
                    TT(P, "dve", r1[:], psf(ba), cosF[:, tokg], ALU.mult, [PSR(ba), "cosF"], [("rt1", rti % 2)])
                    TT(P, "dve", r2[:], psf(br), sinS[:, tokg], ALU.mult, [PSR(br), "sinS"], [("rt2", rti % 2)])
                    rr_ = [("rt1", rti % 2), ("rt2", rti % 2), "qz0", "iqz0"]
                    if isinstance(dstf, tuple):
                        dt_, pp = dstf
                        TT(P, "pool", dt_[0:64, 2 * pp, tokg], r1[0:64, :], r2[0:64, :], ALU.add, rr_, [(dname, tg, 0)])
                        TT(P, "pool", dt_[64:128, 2 * pp + 1, tokg], r1[64:128, :], r2[64:128, :], ALU.add, rr_,
                           [(dname, tg, 1)])
                    else:
                        TT(P, "pool", dstf[:, tokg], r1[:], r2[:], ALU.add, rr_, [(dname, tg)])
                    rti += 1
                prefetch(2)
            for ct in range(6):
                k = load_w(16 + ct)
                for tg in range(4):
                    t0 = tg * 512
                    b = fm_banks.next()
                    fm_proj(k, tg, b)
                    CP(P, "act", uT[:, ct, 3 + t0:3 + t0 + 512], psf(b), [PSR(b)], [("uT", ct, tg)])
                prefetch(1)
                for tg in range(4):
                    t0 = tg * 512
                    b = fm_banks.next()
                    rr = [("uT", ct, tg), ("uT", -1)] + ([("uT", ct, tg - 1)] if tg > 0 else [])
                    for j in range(4):
                        MM(P, psf(b), diagw[:, ct * 4 + j, :], uT[:, ct, t0 + j:t0 + j + 512], j == 0, j == 3,
                           rr + [("diagw", ct * 4 + j)], [PSR(b)], inc=(j == 3))
                    ACT(P, xbcT[:, ct, t0:t0 + 512], psf(b), AF.Silu, [PSR(b), "convb"], [("xbcT", ct, tg)],
                        bias=convb[:, ct:ct + 1])
            g4 = range(4)
            P.barrier()
            dump("qz", qT[:], [128, 8, S], [])
            dump("iqz", iqT[:], [128, 4, S], [])
            A.free(*p1_tmp)
            if stop_after == 1:
                A.free(qT, kT, iqT, ikT, Vext, iws, xbcT, siluz, dtr)
                continue

            mixT = A.alloc("mixT", [128, 8, S], BF16)
            dtb = A.alloc("dtb", [128, 8], F32)
            arep = A.alloc("arep", [128, 8], F32)
            dskip = A.alloc("dskip", [128, 512], F32)
            nwr = A.alloc("nwr", [128, 512], F32)
            dt_all = A.alloc("dt_all", [128, NT, 8], F32)
            A_all = A.alloc("A_all", [128, NT, 8], F32)
            acum = A.alloc("acum", [128, NT, 8], F32)
            tot = A.alloc("tot", [128, NT, 8], F32)
            ds_all = A.alloc("ds_all", [128, NT, 8], F32)
            cdec = A.alloc("cdec", [128, NT, 8], F32)
            state = A.alloc("state", [128, 8, 64], F32)
            NB = 2
            xdt = [A.alloc("xdt%d" % i, [128, 8, 64], BF16) for i in range(NB)]
            xdtd = [A.alloc("xdtd%d" % i, [128, 8, 64], BF16) for i in range(NB)]
            dsk = [A.alloc("dsk%d" % i, [128, 512], F32) for i in range(NB)]
            Btok = [A.alloc("Btok%d" % i, [128, 128], BF16) for i in range(NB)]
            Gm = [A.alloc("Gm%d" % i, [128, 2, 128], BF16) for i in range(NB)]
            Ab = [A.alloc("Ab%d" % i, [128, 2, 8, 128], BF16) for i in range(NB)]
            A_hl = A.alloc("A_hl", [128, 2, NT, 8], BF16)
            A_res = A.alloc("A_res", [128, NT, 8], F32)
            Dm = [A.alloc("Dm%d" % i, [128, 8, 128], F32) for i in range(NB)]
            Lm = [A.alloc("Lm%d" % i, [128, 8, 128], BF16) for i in range(NB)]
            Eb = [A.alloc("Eb%d" % i, [128, 8, 128], BF16) for i in range(NB)]
            MT = [A.alloc("MT%d" % i, [128, 8, 128], BF16) for i in range(NB)]
            Cp = [A.alloc("Cp%d" % i, [128, 8, 128], BF16) for i in range(NB)]
            CTz = [A.alloc("CTz%d" % i, [128, 2, 128], BF16) for i in range(NB)]
            prevb = [A.alloc("prevb%d" % i, [128, 8, 64], BF16) for i in range(NB)]
            y1 = [A.alloc("y1_%d" % i, [128, 512], F32) for i in range(NB)]
            y2 = [A.alloc("y2_%d" % i, [128, 512], F32) for i in range(NB)]
            yo = [A.alloc("yo%d" % i, [128, 512], BF16) for i in range(NB)]
            junk = [A.alloc("junk%d" % i, [128, 512], BF16) for i in range(NB)]
            ms = [A.alloc("ms%d" % i, [128, 2], F32) for i in range(NB)]
            p3_tmp = ([A_hl, A_res, dtb, arep, dskip, nwr, dt_all, A_all, acum, tot, ds_all, cdec, state] + xdt + xdtd + dsk + Btok + Gm
                      + Ab + Dm + Lm + Eb + MT + Cp + CTz + prevb + y1 + y2 + yo + junk + ms)

            for i in range(NB):
                MSET(P, "pool", Cp[i][:], 0.0, [], [("Cp", i, 0), ("Cp", i, 1)])
                MSET(P, "pool", CTz[i][:], 0.0, [], [("CTz", i)])
            DMA(P, "sp", dtb[:], dtb_d, [], ["dtb"])
            DMA(P, "sp", arep[:], alog_d, [], ["arep"])
            DMA(P, "sp", dskip[:], dskip_d, [], ["dskip"])
            DMA(P, "sp", nwr[:], nw_d, [], ["nwr"])
            ACT(P, arep[:], arep[:], AF.Exp, ["arep"], ["arep"])
            TS(P, "dve", arep[:], arep[:], -1.0, None, ALU.mult, None, ["arep"], ["arep"])
            alldtr = [("dtr", t) for t in range(NT)]
            TT(P, "dve", dt_all[:], dtr[:], dtb[:].unsqueeze(1).to_broadcast([128, NT, 8]), ALU.add,
               alldtr + ["dtb"], ["dt_all"])
            ACT(P, dt_all[:], dt_all[:], AF.Exp, ["dt_all"], ["dt_all"])
            ACT(P, dt_all[:], dt_all[:], AF.Ln, ["dt_all"], ["dt_all"], bias=1.0)
            TT(P, "dve", A_all[:], dt_all[:], arep[:].unsqueeze(1).to_broadcast([128, NT, 8]), ALU.mult,
               ["dt_all", "arep"], ["A_all"])
            CP(P, "dve", A_hl[:, 0, :, :], A_all[:], ["A_all"], ["A_hl0"])
            TT(P, "dve", A_res[:], A_all[:], A_hl[:, 0, :, :], ALU.subtract, ["A_all", "A_hl0"], ["A_res"])
            CP(P, "dve", A_hl[:, 1, :, :], A_res[:], ["A_res"], ["A_hl1"])
            A2 = A_all[:].rearrange("p c h -> p (c h)")
            MM(P, ps[0][:, 0:128], tri_f[:], A2, True, True, ["tri_f", "A_all"], [PSR(0)])
            MM(P, ps[1][:, 0:128], ones_f[:], A2, True, True, ["ones_f", "A_all"], [PSR(1)])
            CP(P, "dve", acum[:].rearrange("p c h -> p (c h)"), ps[0][:, 0:128], [PSR(0)], ["acum"])
            CP(P, "dve", tot[:].rearrange("p c h -> p (c h)"), ps[1][:, 0:128], [PSR(1)], ["tot"])
            TT(P, "dve", ds_all[:], tot[:], acum[:], ALU.subtract, ["tot", "acum"], ["ds_all"])
            ACT(P, ds_all[:], ds_all[:], AF.Exp, ["ds_all"], ["ds_all"])
            ACT(P, cdec[:], tot[:], AF.Exp, ["tot"], ["cdec"])
            trb = Rot([0, 1])
            def ssd_s1(c):
                    i = c % NB
                    tok = slice(c * 128, (c + 1) * 128)
                    g4 = c // 4
                    xres = [("xbcT", ct, g4) for ct in range(6)]
                    CP(P, "dve", Ab[i][:], A_hl[:, :, c, :].unsqueeze(3).to_broadcast([128, 2, 8, 128]), ["A_hl0", "A_hl1"],
                       [("Ab", i)])
                    for h in range(8):
                        bb = 3 + h // 4
                        MM(P, ps[bb][:, (h % 4) * 128:(h % 4 + 1) * 128], Ab[i][:, 0, h, :], tri_b[:], True, False,
                           [("Ab", i), "tri_b"], [PSR(bb)], inc=False)
                        MM(P, ps[bb][:, (h % 4) * 128:(h % 4 + 1) * 128], Ab[i][:, 1, h, :], tri_b[:], False, True,
                           [("Ab", i), "tri_b"], [PSR(bb)], inc=(h % 4 == 3))
                    b = trb.next()
                    pv = psb(b)
                    for ct in range(5):
                        TR(P, pv[:, ct * 128:(ct + 1) * 128], xbcT[:, ct, tok], ident_b[:], xres + ["ident_b"], [PSR(b)],
                           inc=(ct == 4))
                    xs_ps = pv[:, 0:512].rearrange("p (h d) -> p h d", h=8)
                    TT(P, "dve", xdt[i][:], xs_ps, dt_all[:, c, :].unsqueeze(2).to_broadcast([128, 8, 64]), ALU.mult,
                       [PSR(b), "dt_all"], [("xdt", i)])
                    TT(P, "dve", dsk[i][:], pv[:, 0:512], dskip[:], ALU.mult, [PSR(b), "dskip"], [("dsk", i)])
                    CP(P, "act", Btok[i][:], pv[:, 512:640], [PSR(b)], [("Btok", i)])
                    TT(P, "dve", xdtd[i][:], xdt[i][:], ds_all[:, c, :].unsqueeze(2).to_broadcast([128, 8, 64]), ALU.mult,
                       [("xdt", i), "ds_all"], [("xdtd", i)])
                    for g in range(2):
                        CP(P, "pool", CTz[i][64 * g:64 * g + 64, g, :], xbcT[64 * g:64 * g + 64, 5, tok], xres, [("CTz", i)])
                    for g in range(2):
                        MM(P, ps[2][:, g * 128:(g + 1) * 128], xbcT[:, 4, tok], CTz[i][:, g, :],
                           True, True, xres + [("CTz", i)], [PSR(2)], inc=(g == 1))
                    TT(P, "dve", Gm[i][:], ps[2][:, 0:256].rearrange("p (g l) -> p g l", g=2),
                       tri_f[:].unsqueeze(1).to_broadcast([128, 2, 128]), ALU.mult, [PSR(2), "tri_f"], [("Gm", i)])
                    for g in range(2):
                        TT(P, "dve", Dm[i][:, 4 * g:4 * g + 4, :], ps[3 + g][:].rearrange("p (h l) -> p h l", h=4),
                           acum[:, c, 4 * g:4 * g + 4].unsqueeze(2).to_broadcast([128, 4, 128]), ALU.subtract,
                           [PSR(3 + g), "acum"], [("Dm", i)])
                    ACT(P, Dm[i][:], Dm[i][:], AF.Relu, [("Dm", i)], [("Dm", i)], scale=-1.0)
                    ACT(P, Lm[i][:], Dm[i][:], AF.Exp, [("Dm", i)], [("Lm", i)], scale=-1.0)
                    for g in range(2):
                        ACT(P, Eb[i][:, g * 4:(g + 1) * 4, :], ps[3 + g][:].rearrange("p (h l) -> p h l", h=4), AF.Exp,
                            [PSR(3 + g)], [("Eb", i, g)])
                    for g in range(2):
                        TT(P, "dve", MT[i][:, g * 4:(g + 1) * 4, :], Lm[i][:, g * 4:(g + 1) * 4, :],
                           Gm[i][:, g, :].unsqueeze(1).to_broadcast([128, 4, 128]), ALU.mult,
                           [("Lm", i), ("Gm", i)], [("MT", i, g)])
                        TT(P, "dve", Cp[i][64 * g:64 * g + 64, g * 4:(g + 1) * 4, :], Eb[i][64 * g:64 * g + 64, g * 4:(g + 1) * 4, :],
                           xbcT[64 * g:64 * g + 64, 5, tok].unsqueeze(1).to_broadcast([64, 4, 128]), ALU.mult,
                           [("Eb", i, g)] + xres, [("Cp", i, g)])

            def ssd_s2(c):
                    i = c % NB
                    tok = slice(c * 128, (c + 1) * 128)
                    g4 = c // 4
                    xres = [("xbcT", ct, g4) for ct in range(6)]
                    if c > 0:
                        CP(P, "pool", prevb[i][:], state[:], ["state"], [("prevb", i)])
                    for h in range(8):
                        g = h // 4
                        MM(P, ps[5][:, h * 64:(h + 1) * 64], MT[i][:, h, :], xdt[i][:, h, :], True, c == 0,
                           [("MT", i, g), ("xdt", i)], [PSR(5)], inc=(c == 0 and h == 7))
                        if c > 0:
                            MM(P, ps[5][:, h * 64:(h + 1) * 64], Cp[i][:, h, :],
                               prevb[i][:, h, :], False, True, [("Cp", i, g), ("prevb", i)], [PSR(5)],
                               inc=(h == 7))
                    if c < NT - 1:
                        for h in range(8):
                            MM(P, ps[6][:, h * 64:(h + 1) * 64], Btok[i][:], xdtd[i][:, h, :], True, True,
                               [("Btok", i), ("xdtd", i)], [PSR(6)], inc=(h == 7))
                        st2 = state[:].rearrange("p h d -> p (h d)")
                        if c == 0:
                            CP(P, "dve", st2, psf(6), [PSR(6)], ["state"])
                        else:
                            TT(P, "dve", state[:], state[:], cdec[:, c, :].unsqueeze(2).to_broadcast([128, 8, 64]), ALU.mult,
                               ["state", "cdec", ("prevb", i)], ["state"])
                            TT(P, "dve", st2, st2, psf(6), ALU.add, ["state", PSR(6)], ["state"])
                    TT(P, "dve", y1[i][:], psf(5), dsk[i][:], ALU.add, [PSR(5), ("dsk", i)], [("y1", i)])
                    TT(P, "pool", y2[i][:], y1[i][:], siluz[:, c, :], ALU.mult, [("y1", i), ("siluz", c)], [("y2", i)])
                    ACT(P, junk[i][:], y2[i][:], AF.Square, [("y2", i)], [("junk", i), ("ms", i)], scale=float(512 ** -0.5),
                        accum=ms[i][:, 0:1])
                    ACT(P, ms[i][:, 1:2], ms[i][:, 0:1], AF.Ln, [("ms", i)], [("ms2", i)], bias=LN_EPS)
                    ACT(P, ms[i][:, 1:2], ms[i][:, 1:2], AF.Exp, [("ms2", i)], [("ms2", i)], scale=-0.5)

            def ssd_s3(c):
                    i = c % NB
                    tok = slice(c * 128, (c + 1) * 128)
                    ACT(P, yo[i][:], y2[i][:], AF.Copy, [("y2", i), ("ms2", i)], [("yo", i)], scale=ms[i][:, 1:2])
                    pv7 = psb(7)
                    for k in range(4):
                        TR(P, pv7[:, k * 128:(k + 1) * 128], yo[i][:, k * 128:(k + 1) * 128], ident_b[:], [("yo", i), "ident_b"],
                           [PSR(7)], inc=(k == 3))
                    CP(P, "act", mixT[:, 4:8, tok], pv7[:, 0:512].rearrange("p (k t) -> p k t", k=4), [PSR(7)], [("mixT", "s", c)])


            ssd_s1(0)
            for c in range(NT):
                if c + 1 < NT:
                    ssd_s1(c + 1)
                if c > 0:
                    ssd_s3(c - 1)
                ssd_s2(c)
            ssd_s3(NT - 1)
            dump("ssdT", mixT[:, 4:8, :], [128, 4, S], [("mixT", "s", c) for c in range(NT)])
            P.barrier()
            A.free(*p3_tmp)
            A.free(xbcT, siluz, dtr)
            if stop_after == 3:
                A.free(mixT, qT, kT, iqT, ikT, Vext, iws)
                continue

            I4 = A.alloc("I4", [128, 4, S], F32)
            junkb = A.alloc("junkb", [128, S], BF16)
            selb = [A.alloc("selb%d" % i, [128, S], BF16) for i in range(2)]
            Rt = [A.alloc("Rt%d" % i, [128, 512], F32) for i in range(4)]
            maskT = [A.alloc("maskT%d" % i, [128, NT, 512], BF16) for i in range(2)]
            Pexp = [A.alloc("Pexp%d" % i, [128, 512], BF16) for i in range(3)]
            Pm = []
            Osb = [A.alloc("Osb%d" % i, [128, 512], F32) for i in range(3)]
            rec = []
            Sden = A.alloc("Sden", [128, 2, 128], F32)
            halfpow = A.alloc("halfpow", [128, NBIS + 2], F32)
            aw = A.alloc("aw", [128, 4, 4], F32)
            sg = A.alloc("sg", [128, 4, 4], F32)
            lo0 = A.alloc("lo0", [128, 4], F32)
            hi0 = A.alloc("hi0", [128, 4], F32)
            wd = A.alloc("wd", [128, 4, NBIS + 2], F32)
            mid = A.alloc("mid", [128, 4], F32)
            cnt = A.alloc("cnt", [128, 4], F32)
            sacc = A.alloc("sacc", [128, 2], F32)
            lhalf = A.alloc("lhalf", [128, 2], F32)
            junka = A.alloc("junka", [128, S], BF16)
            u2 = A.alloc("u2", [128, 4], F32)
            tq = A.alloc("tq", [128, 4], F32)
            thr = A.alloc("thr", [128, 4], F32)
            p2_tmp = maskT + [sacc, lhalf, junka, I4, junkb, Sden, halfpow, aw, sg, lo0, hi0, wd, mid, cnt, u2, tq, thr] + selb + Rt + Pexp + Pm + Osb + rec
            DMA(P, "sp", halfpow[:], halfpow_d, [], ["halfpow"])
            MSET(P, "pool", Sden[:], 0.0, [], ["Sden"])
            MSET(P, "pool", Sden[64:65, 0, :], 1.0, ["Sden"], ["Sden"])
            MSET(P, "pool", Sden[0:1, 1, :], 1.0, ["Sden"], ["Sden"])
            ibank = Rot([0, 1, 2])
            oi_box = [0]

            def gen_IB(g):
                mT = maskT[g % 2]
                nkb = g + 1
                Ls = []
                for b in range(4):
                    qi = 4 * g + b
                    L = (qi + 1) * 128
                    Ls.append(L)
                    qtok = slice(qi * 128, (qi + 1) * 128)
                    STT(P, "dve", aw[:, b, :], iws[:, qi, :], -1.0, iws[:, qi, :], ALU.mult, ALU.max, [], [("aw", b)])
                    TS(P, "dve", sg[:, b, :], iws[:, qi, :], 0.0, 2.0, ALU.is_ge, ALU.mult, [], [("sg", b)])
                    TS(P, "dve", sg[:, b, :], sg[:, b, :], -1.0, None, ALU.add, None, [("sg", b)], [("sg", b)])
                    for kb in range(nkb):
                        w = min(512, L - kb * 512)
                        ks = slice(kb * 512, kb * 512 + w)
                        for h in range(4):
                            bk = ibank.next()
                            MM(P, ps[bk][:, 0:w], iqT[:, h, qtok], ikT[:, ks], True, True, [], [PSR(bk)])
                            ACT(P, Rt[h][:, 0:w], ps[bk][:, 0:w], AF.Relu, [PSR(bk), ("aw", b)], [("Rt", h)],
                                scale=aw[:, b, h:h + 1])
                        TS(P, "dve", I4[:, b, ks], Rt[0][:, 0:w], sg[:, b, 0:1], None, ALU.mult, None,
                           [("Rt", 0), ("sg", b)], [("I4", b)])
                        for h in range(1, 4):
                            STT(P, "dve", I4[:, b, ks], Rt[h][:, 0:w], sg[:, b, h:h + 1], I4[:, b, ks], ALU.mult, ALU.add,
                                [("Rt", h), ("sg", b), ("I4", b)], [("I4", b)])
                        yield 3.0 * w / 512 + 0.5
                    P.op("dve", lambda e, o=lo0[:, b:b + 1], i_=I4[:, b, 0:L]: e.tensor_reduce(o, i_, axis=AX.X, op=ALU.min),
                         [("I4", b)], [("lo0", b)])
                    TT(P, "dve", I4[:, b, qi * 128:(qi + 1) * 128], I4[:, b, qi * 128:(qi + 1) * 128], cneg[:], ALU.add,
                       [("I4", b), ("lo0", b), "cneg"], [("I4", b)])
                    P.op("dve", lambda e, o=hi0[:, b:b + 1], i_=I4[:, b, 0:L]: e.tensor_reduce(o, i_, axis=AX.X, op=ALU.max),
                         [("I4", b)], [("hi0", b)])
                    yield 2.2 * L / 1024 + 0.6
                allb = [("lo0", b) for b in range(4)] + [("hi0", b) for b in range(4)]
                TT(P, "dve", tq[:], hi0[:], lo0[:], ALU.subtract, allb, ["tq"])
                TT(P, "dve", wd[:], tq[:].unsqueeze(2).to_broadcast([128, 4, NBIS + 2]),
                   halfpow[:].unsqueeze(1).to_broadcast([128, 4, NBIS + 2]), ALU.mult, ["tq", "halfpow"], ["wd"])
                TT(P, "dve", mid[:], lo0[:], wd[:, :, 0], ALU.add, allb + ["wd"], ["mid"])
                yield 1.0
                MSET(P, "pool", lhalf[:, 0:1], Ls[2] / 2.0, [], ["lhalf"])
                MSET(P, "pool", lhalf[:, 1:2], Ls[3] / 2.0, [], ["lhalf"])
                for k in range(NBIS):
                    for b in (2, 3):
                        ACT(P, junka[:, 0:Ls[b]], I4[:, b, 0:Ls[b]], AF.Sign, [("I4", b), "mid"], ["junka", ("sacc", b)],
                            bias=mid[:, b:b + 1], scale=-1.0, accum=sacc[:, b - 2:b - 1])
                    for b in (0, 1):
                        TS(P, "dve", junkb[:, 0:Ls[b]], I4[:, b, 0:Ls[b]], mid[:, b:b + 1], 0.0, ALU.is_ge, ALU.add,
                           [("I4", b), "mid"], ["junkb", ("cnt", b)], accum=cnt[:, b:b + 1])
                    STT(P, "dve", cnt[:, 2:4], sacc[:, 0:2], -0.5, lhalf[:, 0:2], ALU.mult, ALU.add,
                        [("sacc", 2), ("sacc", 3), "lhalf"], [("cnt", 2)])
                    TS(P, "dve", u2[:], cnt[:], 256.0, 2.0, ALU.is_ge, ALU.mult, [("cnt", 0), ("cnt", 1), ("cnt", 2)], ["u2"])
                    STT(P, "dve", tq[:], u2[:], -1.0, wd[:, :, k + 1], ALU.add, ALU.mult, ["u2", "wd"], ["tq"])
                    TT(P, "dve", mid[:], mid[:], tq[:], ALU.add, ["mid", "tq"], ["mid"])
                    yield (Ls[0] + Ls[1]) / 960.0 + 1.2
                TT(P, "dve", thr[:], mid[:], wd[:, :, NBIS - 2], ALU.subtract, ["mid", "wd"], ["thr"])
                for b in range(4):
                    qi = 4 * g + b
                    sb_ = selb[b % 2]
                    TS(P, "dve", sb_[:, 0:Ls[b]], I4[:, b, 0:Ls[b]], thr[:, b:b + 1], MASKB, ALU.is_lt, ALU.mult,
                       [("I4", b), "thr"], [("selb", b % 2)])
                    for c0 in range(0, qi + 1, 8):
                        n = min(8, qi + 1 - c0)
                        pv = psb(3)
                        for j in range(n):
                            TR(P, pv[:, j * 128:(j + 1) * 128], sb_[:, (c0 + j) * 128:(c0 + j + 1) * 128], ident_b[:],
                               [("selb", b % 2)], [PSR(3)], inc=(j == n - 1))
                        CP(P, "act", mT[:, c0:c0 + n, b * 128:(b + 1) * 128],
                           pv[:, 0:n * 128].rearrange("p (c q) -> p c q", c=n), [PSR(3)], [("maskT", g % 2, b)])
                    yield Ls[b] / 1024.0 + 1.0

            def gen_A(g):
                mT = maskT[g % 2]
                mres = [("maskT", g % 2, b) for b in range(4)]
                nkc = 4 * g + 4
                items = [(h, c) for h in range(8) for c in range(nkc)]

                def emit_S(idx):
                    h, c = items[idx]
                    cs = max(0, c - 4 * g) * 128
                    qs = slice(g * 512 + cs, g * 512 + 512)
                    bk = 4 + idx % 3
                    MM(P, ps[bk][:, cs:512], kT[:, c * 128:(c + 1) * 128], qT[:, h, qs], True, False, [], [PSR(bk)], inc=False)
                    MM(P, ps[bk][:, cs:512], ident_b[:], mT[:, c, cs:512], False, True, mres, [PSR(bk)])
                    ACT(P, Pexp[idx % 3][:, cs:512], ps[bk][:, cs:512], AF.Exp, [PSR(bk)], [("Pexp", idx % 3)], scale=0.125)

                def emit_PV(idx):
                    h, c = items[idx]
                    par = h % 2
                    rows = slice(64 * par, 64 * par + 64)
                    vcols = slice(0, 128) if par == 0 else slice(64, 192)
                    cs = max(0, c - 4 * g) * 128
                    MM(P, ps[7][:, cs:512], Vext[:, c, vcols], Pexp[idx % 3][:, cs:512], c == 0, c == nkc - 1,
                       [("Pexp", idx % 3)], [PSR(7)], inc=True)
                    if c == nkc - 1:
                        o_ = oi_box[0]
                        ob = Osb[o_ % 3]
                        ores = ("Osb", o_ % 3)
                        dr = slice(64, 65) if par == 0 else slice(0, 1)
                        CP(P, "act", ob[:], psf(7), [PSR(7)], [ores])
                        ACT(P, ob[dr, :], ob[dr, :], AF.Ln, [ores], [ores])
                        ACT(P, ob[dr, :], ob[dr, :], AF.Exp, [ores], [ores], scale=-1.0)

                        def fin1(ob=ob, ores=ores, par=par):
                            MM(P, psf(3), Sden[:, par, :], ob[:], True, True, [ores, "Sden"], [PSR(3)])

                        def fin2(ob=ob, ores=ores, rows=rows, h=h):
                            TT(P, "dve", mixT[rows, h // 2, g * 512:(g + 1) * 512], ob[rows, :], ps[3][rows, :], ALU.mult,
                               [ores, PSR(3)], [("mixT", "a", h, g)])
                        pending.append([2, fin1, fin2])
                        oi_box[0] += 1

                pending = []

                def run_pending(force=False):
                    for p_ in list(pending):
                        p_[0] -= 1
                        if p_[0] <= 0 or force:
                            p_[1]()
                            p_[2]()
                            pending.remove(p_)

                emit_S(0)
                emit_S(1)
                yield 1.4
                for idx in range(len(items)):
                    emit_PV(idx)
                    if idx + 2 < len(items):
                        emit_S(idx + 2)
                    run_pending()
                    yield 0.75
                run_pending(force=True)

            def timed_interleave(*gens):
                gens = [[0.0, g_] for g_ in gens]
                while gens:
                    gens.sort(key=lambda x: x[0])
                    cur = gens[0]
                    try:
                        cur[0] += next(cur[1])
                    except StopIteration:
                        gens.remove(cur)

            gorder = [3, 2, 1, 0]
            timed_interleave(gen_IB(gorder[0]))
            for gi, g in enumerate(gorder):
                if gi + 1 < 4:
                    timed_interleave(gen_A(g), gen_IB(gorder[gi + 1]))
                else:
                    timed_interleave(gen_A(g))
            dump("attT", mixT[:, 0:4, :], [128, 4, S], [("mixT", "a", h, g) for h in range(8) for g in range(4)])
            P.barrier()
            A.free(*p2_tmp)
            A.free(qT, kT, iqT, ikT, Vext, iws)
            if stop_after == 2:
                A.free(mixT)
                continue

            def ln_gen(r, st, junk_, gam, bet, hn, tag, add_eng="pool"):
                ACT(P, junk_[:], r[:], AF.Identity, [(tag, "r")], [(tag, "junk"), (tag, "st")], accum=st[:, 0:1])
                yield
                TS(P, "dve", st[:, 1:2], st[:, 0:1], -1.0 / D, None, ALU.mult, None, [(tag, "st")], [(tag, "st")])
                yield
                ACT(P, junk_[:], r[:], AF.Square, [(tag, "r"), (tag, "st")], [(tag, "junk"), (tag, "st")],
                    bias=st[:, 1:2], accum=st[:, 2:3])
                yield
                ACT(P, st[:, 3:4], st[:, 2:3], AF.Ln, [(tag, "st")], [(tag, "st")], bias=LN_EPS, scale=1.0 / D)
                yield
                ACT(P, st[:, 3:4], st[:, 3:4], AF.Exp, [(tag, "st")], [(tag, "st")], scale=-0.5)
                yield
                TT(P, "dve", st[:, 4:5], st[:, 1:2], st[:, 3:4], ALU.mult, [(tag, "st")], [(tag, "st")])
                yield
                ACT(P, hn[:], r[:], AF.Identity, [(tag, "r"), (tag, "st")], [(tag, "hn")], bias=st[:, 4:5], scale=st[:, 3:4])
                yield
                TT(P, "dve", hn[:], hn[:], gam[:], ALU.mult, [(tag, "hn"), "lng"], [(tag, "hn")])
                yield
                TT(P, add_eng, hn[:], hn[:], bet[:], ALU.add, [(tag, "hn"), "lnb"], [(tag, "hn")])
                yield

            def interleave(*gens):
                gens = list(gens)
                while gens:
                    for g_ in list(gens):
                        try:
                            next(g_)
                        except StopIteration:
                            gens.remove(g_)

            def layer_norm(r, st, junk_, gam, bet, hn, tag):
                interleave(ln_gen(r, st, junk_, gam, bet, hn, tag, add_eng="dve"))

            hT = A.alloc("hT", [128, 8, S], BF16)
            gates = A.alloc("gates", [128, NT, 16], F32)
            Wgu = [A.alloc("Wgu%d" % i, [128, 8, 512], BF16) for i in range(2)]
            wout = A.alloc("wout", [128, 8, D], BF16)
            lng = A.alloc("lng", [128, D], F32)
            lnb = A.alloc("lnb", [128, D], F32)
            xres = [A.alloc("xres%d" % i, [128, D], F32) for i in range(6)]
            rr = [A.alloc("rr%d" % i, [128, D], F32) for i in range(6)]
            hn = [A.alloc("hn%d" % i, [128, D], F32) for i in range(6)]
            lnj = [A.alloc("lnj%d" % i, [128, D], BF16) for i in range(6)]
            stt_ = [A.alloc("st%d" % i, [128, 8], F32) for i in range(6)]
            hT32 = []
            hlo = [A.alloc("hlo%d" % i, [128, 8, 128], BF16) for i in range(2)]
            wr_hl = A.alloc("wr_hl", [128, 2, 8, 20], BF16)
            wr_res = A.alloc("wr_res", [128, 8, 20], F32)
            wr = A.alloc("wr", [128, 8, 20], F32)
            nwc = A.alloc("nwc", [128, 4], F32)
            brp = A.alloc("brp", [128, 20], F32)
            lg = A.alloc("lg", [128, NT, 20], F32)
            rs = [A.alloc("rs%d" % i, [128, NT, 16], F32) for i in range(4)]
            rv = [A.alloc("rv%d" % i, [128, NT], F32) for i in range(8)]
            p4_tmp = [wout, lng, lnb, wr, nwc, brp, lg, wr_hl, wr_res] + hlo + xres + rr + hn + lnj + stt_ + rs + rv
            for c in range(8):
                DMA(P, "pool", wout[:, c, :], wout_d[c * 128:(c + 1) * 128, :], [], [("wout", c)])
            for e0 in range(2):
                DMA(P, "pool", Wgu[e0][:, :, 0:256], wg_d[e0].rearrange("(c p) f -> p c f", p=128), [], [("Wgu", e0)])
                DMA(P, "pool", Wgu[e0][:, :, 256:512], wu_d[e0].rearrange("(c p) f -> p c f", p=128), [], [("Wgu", e0)])
            DMA(P, "sp", lng[:], ln1g_d, [], ["lng"])
            DMA(P, "sp", lnb[:], ln1b_d, [], ["lnb"])
            DMA(P, "sp", nwc[:], nwc_d, [], ["nwc"])
            for k in range(4):
                TS(P, "dve", wout[:, 4 + k, :], wout[:, 4 + k, :], nwc[:, k:k + 1], None, ALU.mult, None,
                   [("wout", 4 + k), "nwc"], [("wout", 4 + k)])
            DMA(P, "sp", wr[:], wr_d.rearrange("(c p) f -> p c f", p=128), [], ["wr"])
            DMA(P, "sp", brp[:], br_d, [], ["brp"])
            CP(P, "dve", wr_hl[:, 0, :, :], wr[:], ["wr"], ["wr_h"])
            TT(P, "dve", wr_res[:], wr[:], wr_hl[:, 0, :, :], ALU.subtract, ["wr", "wr_h"], ["wr_res"])
            CP(P, "dve", wr_hl[:, 1, :, :], wr_res[:], ["wr_res"], ["wr_hl"])
            mb = Rot([0, 1, 2, 3])

            def mm_part(t):
                i = t % 6
                tok = slice(t * 128, (t + 1) * 128)
                tag = ("ln1", i)
                DMA(P, "sp", xres[i][:], x_d[seq, tok, :], [], [("xres", i)])
                for half in range(2):
                    b = mb.next()
                    hs = slice(half * 512, half * 512 + 512)
                    for c in range(8):
                        MM(P, psf(b), mixT[:, c, tok], wout[:, c, hs], c == 0, c == 7, [("wout", c)], [PSR(b)], inc=(c == 7))
                    STT(P, "dve", rr[i][:, hs], xres[i][:, hs], ALPHA, psf(b), ALU.mult, ALU.add,
                        [("xres", i), PSR(b)], [(tag, "r")])

            def post_gen(t):
                i = t % 6
                tok = slice(t * 128, (t + 1) * 128)
                tag = ("ln1", i)
                yield from ln_gen(rr[i], stt_[i], lnj[i], lng, lnb, hn[i], tag, add_eng="dve")
                DMA(P, "pool", hscr_d[seq, tok, :], hn[i][:], [(tag, "hn")], [])
                yield

            def tr_part(t):
                i = t % 6
                tok = slice(t * 128, (t + 1) * 128)
                tag = ("ln1", i)
                hres_ = [("hT32a", t % 2), ("hT32b", t % 2)]
                for c in range(8):
                    b = 4 + c // 4
                    TR(P, ps[b][:, (c % 4) * 128:(c % 4 + 1) * 128], hn[i][:, c * 128:(c + 1) * 128], ident_f[:],
                       [(tag, "hn")], [PSR(b)], inc=(c % 4 == 3))
                lo_ = hlo[t % 2]
                CP(P, "act", hT[:, 0:4, tok], ps[4][:].rearrange("p (c t) -> p c t", c=4), [PSR(4)], [("hT", t, 0)])
                CP(P, "dve", hT[:, 4:8, tok], ps[5][:].rearrange("p (c t) -> p c t", c=4), [PSR(5)], [("hT", t, 1)])
                TT(P, "dve", lo_[:, 0:4, :], ps[4][:].rearrange("p (c t) -> p c t", c=4), hT[:, 0:4, tok], ALU.subtract,
                   [PSR(4), ("hT", t, 0)], [hres_[0]])
                TT(P, "dve", lo_[:, 4:8, :], ps[5][:].rearrange("p (c t) -> p c t", c=4), hT[:, 4:8, tok], ALU.subtract,
                   [PSR(5), ("hT", t, 1)], [hres_[1]])
                rb = 6 + t % 2
                rres = hres_ + [("hT", t, 0), ("hT", t, 1), "wr_hl"]
                for c in range(8):
                    MM(P, ps[rb][:, 0:20], hT[:, c, tok], wr_hl[:, 0, c, :], c == 0, False, rres, [PSR(rb)], inc=False)
                    MM(P, ps[rb][:, 0:20], hT[:, c, tok], wr_hl[:, 1, c, :], False, False, rres, [PSR(rb)], inc=False)
                    MM(P, ps[rb][:, 0:20], lo_[:, c, :], wr_hl[:, 0, c, :], False, c == 7, rres, [PSR(rb)], inc=(c == 7))
                TT(P, "dve", lg[:, t, :], ps[rb][:, 0:20], brp[:], ALU.add, [PSR(rb), "brp"], [("lg", t)])

            mm_part(0)
            mm_part(1)
            mm_part(2)
            mm_part(3)
            interleave(post_gen(0), post_gen(1))
            for tp in range(0, NT, 2):
                if tp + 2 < NT:
                    interleave(post_gen(tp + 2), post_gen(tp + 3))
                if tp + 4 < NT:
                    mm_part(tp + 4)
                    mm_part(tp + 5)
                tr_part(tp)
                tr_part(tp + 1)
            dump("hT", hT[:], [128, 8, S], [("hT", t, k_) for t in range(NT) for k_ in range(2)])
            lgr = [("lg", t) for t in range(NT)]
            gl = lg[:, :, 0:4]
            gmax, gsum, gprob, m1, m2, dd, w1, w2 = [rv[i] for i in range(8)]
            ge, ohg, pen = rs[0][:, :, 0:4], rs[1][:, :, 0:4], rs[2][:, :, 0:4]

            def red(out, in_, op, r, w):
                P.op("dve", lambda e: e.tensor_reduce(out, in_, axis=AX.X, op=op), r, w)

            def bc(v, n):
                return v[:].unsqueeze(2).to_broadcast([128, NT, n])
            red(gmax[:], gl, ALU.max, lgr, ["gmax"])
            TT(P, "dve", ge, gl, bc(gmax, 4), ALU.subtract, lgr + ["gmax"], ["ge"])
            ACT(P, ge, ge, AF.Exp, ["ge"], ["ge"])
            red(gsum[:], ge, ALU.add, ["ge"], ["gsum"])
            P.op("dve", lambda e: e.reciprocal(gprob[:], gsum[:]), ["gsum"], ["gprob"])
            TT(P, "dve", ohg, gl, bc(gmax, 4), ALU.is_ge, lgr + ["gmax"], ["ohg"])
            TS(P, "dve", pen, ohg, -1.0, 1.0e4, ALU.add, ALU.mult, ["ohg"], ["pen"])
            mel, oh1, mel2, oh2 = rs[3], rs[0], rs[1], rs[2]
            for gq in range(4):
                TT(P, "dve", mel[:, :, gq * 4:(gq + 1) * 4], lg[:, :, 4 + gq * 4:8 + gq * 4],
                   pen[:, :, gq:gq + 1].to_broadcast([128, NT, 4]), ALU.add, lgr + ["pen"], ["mel"])
            red(m1[:], mel[:], ALU.max, ["mel"], ["m1"])
            TT(P, "dve", oh1[:], mel[:], bc(m1, 16), ALU.is_ge, ["mel", "m1", "ge"], ["oh1"])
            STT(P, "dve", mel2[:], oh1[:], -1.0e4, mel[:], ALU.mult, ALU.add, ["oh1", "mel", "ohg"], ["mel2"])
            red(m2[:], mel2[:], ALU.max, ["mel2"], ["m2"])
            TT(P, "dve", oh2[:], mel2[:], bc(m2, 16), ALU.is_ge, ["mel2", "m2", "pen"], ["oh2"])
            TT(P, "dve", dd[:], m2[:], m1[:], ALU.subtract, ["m1", "m2"], ["dd"])
            ACT(P, dd[:], dd[:], AF.Exp, ["dd"], ["dd"])
            TS(P, "dve", w1[:], dd[:], 1.0, None, ALU.add, None, ["dd"], ["w1"])
            P.op("dve", lambda e: e.reciprocal(w1[:], w1[:]), ["w1"], ["w1"])
            TT(P, "dve", w2[:], dd[:], w1[:], ALU.mult, ["dd", "w1"], ["w2"])
            TT(P, "dve", w1[:], w1[:], gprob[:], ALU.mult, ["w1", "gprob", "w2"], ["w1"])
            TT(P, "dve", w2[:], w2[:], gprob[:], ALU.mult, ["w2", "gprob"], ["w2"])
            TT(P, "dve", oh1[:], oh1[:], bc(w1, 16), ALU.mult, ["oh1", "w1", "mel2"], ["oh1"])
            TT(P, "dve", oh2[:], oh2[:], bc(w2, 16), ALU.mult, ["oh2", "w2"], ["oh2"])
            TT(P, "dve", gates[:], oh1[:], oh2[:], ALU.add, ["oh1", "oh2"], ["gates"])
            dump("gates", gates[:], [128, NT, 16], ["gates"])
            P.barrier()
            A.free(*p4_tmp)
            A.free(mixT)
            if stop_after == 4:
                A.free(hT, gates)
                continue

            selc = A.alloc("selc", [16, 16 * 128], F32)
            gT = A.alloc("gT", [16, 1024], F32)
            wdn = A.alloc("wdn", [128, 16, D], BF16)
            hid = A.alloc("hid", [128, 16, 1024], BF16)
            yacc = A.alloc("yacc", [128, 8, D], F32)
            sgt = [A.alloc("sgt%d" % i, [128, 512], BF16) for i in range(2)]
            ttm = [A.alloc("ttm%d" % i, [128, 512], BF16) for i in range(2)]
            lng = A.alloc("lng2", [128, D], F32)
            lnb = A.alloc("lnb2", [128, D], F32)
            hres = [A.alloc("hres%d" % i, [128, D], F32) for i in range(2)]
            r2 = [A.alloc("r2_%d" % i, [128, D], F32) for i in range(2)]
            on = [A.alloc("on%d" % i, [128, D], F32) for i in range(2)]
            lnj = [A.alloc("lnj2_%d" % i, [128, D], BF16) for i in range(2)]
            stt_ = [A.alloc("st2_%d" % i, [128, 8], F32) for i in range(2)]
            p5_tmp = [selc, gT, wdn, hid, yacc, lng, lnb] + Wgu + sgt + ttm + hres + r2 + on + lnj + stt_
            DMA(P, "sp", selc[:], sel_d, [], ["selc"])
            DMA(P, "sp", lng[:], ln2g_d, [], ["lng"])
            DMA(P, "sp", lnb[:], ln2b_d, [], ["lnb"])
            gub = Rot([2, 3, 4, 5])
            geb = Rot([0, 1])
            dnb = Rot([6, 7])
            wi = 0
            si = 0
            li = 0
            for hf in range(2):
                for j4 in range(2):
                    for k in range(4):
                        t = hf * 8 + j4 * 4 + k
                        TR(P, ps[j4][0:16, k * 128:(k + 1) * 128], gates[:, t, :], ident_f[:], ["gates"], [PSR(j4)], inc=(k == 3))
                    CP(P, "dve", gT[:, j4 * 512:(j4 + 1) * 512], ps[j4][0:16, :], [PSR(j4)], [("gT", j4)])
                for eh in range(2):
                    for el in range(8):
                        e = eh * 8 + el
                        W = Wgu[wi % 2]
                        wres = ("Wgu", wi % 2)
                        if not (hf == 0 and eh == 0 and el < 2):
                            DMA(P, "pool", W[:, :, 0:256], wg_d[e].rearrange("(c p) f -> p c f", p=128), [], [wres])
                            DMA(P, "pool", W[:, :, 256:512], wu_d[e].rearrange("(c p) f -> p c f", p=128), [], [wres])
                        DMA(P, "pool", wdn[:, 2 * el:2 * el + 2, :], wd_d[e].rearrange("(j p) d -> p j d", p=128), [],
                            [("wdn", el)])
                        wi += 1
                        for sub in range(2):
                            ts_ = slice(hf * 1024 + sub * 512, hf * 1024 + sub * 512 + 512)
                            loc = slice(sub * 512, sub * 512 + 512)
                            gb_ = geb.next()
                            MM(P, psf(gb_), selc[:, e * 128:(e + 1) * 128], gT[:, loc], True, True,
                               ["selc", ("gT", sub)], [PSR(gb_)])
                            for j in range(2):
                                bg, bu = gub.next(), gub.next()
                                for c in range(8):
                                    MM(P, psf(bg), W[:, c, j * 128:(j + 1) * 128], hT[:, c, ts_], c == 0, c == 7,
                                       [wres], [PSR(bg)], inc=(c == 7))
                                for c in range(8):
                                    MM(P, psf(bu), W[:, c, 256 + j * 128:256 + (j + 1) * 128], hT[:, c, ts_], c == 0, c == 7,
                                       [wres], [PSR(bu)], inc=(c == 7))
                                sg_, tm_ = sgt[si % 2], ttm[si % 2]
                                ACT(P, sg_[:], psf(bg), AF.Silu, [PSR(bg)], [("sgt", si % 2)])
                                TT(P, "dve", tm_[:], sg_[:], psf(bu), ALU.mult, [("sgt", si % 2), PSR(bu)], [("ttm", si % 2)])
                                TT(P, "dve", hid[:, el * 2 + j, loc], tm_[:], psf(gb_), ALU.mult,
                                   [("ttm", si % 2), PSR(gb_)], [("hid", el * 2 + j, sub)])
                                si += 1
                    hres_all = [("hid", k, sub) for k in range(16) for sub in range(2)] + [("wdn", el) for el in range(8)]
                    for lt in range(8):
                        t = hf * 8 + lt
                        tok = slice(t * 128, (t + 1) * 128)
                        i = li % 2
                        tag = ("ln2", i)
                        if eh == 1 and lt == 0:
                            for l2 in range(2):
                                t2 = hf * 8 + l2
                                DMA(P, "sp", hres[(li + l2) % 2][:], hscr_d[seq, t2 * 128:(t2 + 1) * 128, :], [],
                                    [("hres", (li + l2) % 2)])
                        for dh in range(2):
                            b = dnb.next()
                            hs = slice(dh * 512, dh * 512 + 512)
                            for k in range(16):
                                MM(P, psf(b), hid[:, k, lt * 128:(lt + 1) * 128], wdn[:, k, hs], k == 0, k == 15,
                                   hres_all, [PSR(b)], inc=(k == 15))
                            if eh == 0:
                                CP(P, "act", yacc[:, lt, hs], psf(b), [PSR(b)], [("yacc", lt)])
                            else:
                                TT(P, "dve", r2[i][:, hs], yacc[:, lt, hs], psf(b), ALU.add, [("yacc", lt), PSR(b)], [(tag, "r0")])
                                STT(P, "dve", r2[i][:, hs], hres[i][:, hs], ALPHA, r2[i][:, hs], ALU.mult, ALU.add,
                                    [("hres", i), (tag, "r0")], [(tag, "r")])
                        if eh == 1:
                            if lt + 2 < 8:
                                t2 = hf * 8 + lt + 2
                                DMA(P, "sp", hres[i][:], hscr_d[seq, t2 * 128:(t2 + 1) * 128, :], [], [("hres", i)])
                            layer_norm(r2[i], stt_[i], lnj[i], lng, lnb, on[i], tag)
                            DMA(P, "sp", out_d[seq, tok, :], on[i][:], [(tag, "hn")], [])
                            li += 1
            P.barrier()
            A.free(*p5_tmp)
            A.free(hT, gates)
    P.emit()
    return nc, dumps


def _in_map(inp, core, consts, wfm, wtm):
    m = dict(consts)
    m["x"] = np.ascontiguousarray(inp["x"][SEQ_PER_CORE * core:SEQ_PER_CORE * (core + 1)], dtype=np.float32)
    m["wfm"] = wfm
    m["wtm"] = wtm
    cw = inp["conv_w"][0]
    m["convw"] = np.ascontiguousarray(cw.reshape(4, 6, 128).transpose(2, 1, 0).reshape(128, 24))
    m["convb"] = np.ascontiguousarray(inp["conv_b"][0].reshape(6, 128).T)
    m["dtb"] = _rep(inp["dt_bias"][0])
    m["alog"] = _rep(inp["a_log"][0])
    m["dskip"] = _rep(np.repeat(inp["d_skip"][0], 64))
    m["nw"] = _rep(inp["ssd_norm_w"][0])
    m["nwc"] = np.ascontiguousarray(inp["ssd_norm_w"][0].reshape(4, 128).T)
    m["wout"] = np.ascontiguousarray(inp["w_out"][0])
    m["ln1g"] = _rep(inp["ln1_g"][0])
    m["ln1b"] = _rep(inp["ln1_b"][0])
    m["ln2g"] = _rep(inp["ln2_g"][0])
    m["ln2b"] = _rep(inp["ln2_b"][0])
    m["wr"] = np.ascontiguousarray(np.concatenate([inp["w_route_group"][0], inp["w_route_expert"][0]], 1))
    m["br"] = _rep(np.concatenate([inp["b_route_group"][0], inp["b_route_expert"][0]]))
    m["wg"] = np.ascontiguousarray(inp["w_gate"][0])
    m["wu"] = np.ascontiguousarray(inp["w_up"][0])
    m["wd"] = np.ascontiguousarray(inp["w_down"][0])
    return m


def kernel(**inputs):
    inp = {k: np.asarray(v, dtype=np.float32) for k, v in inputs.items()}
    wfm, wtm = _layout_w_in(inp["w_in"])
    consts = _consts()
    nc, _ = build()
    in_maps = [_in_map(inp, c, consts, wfm, wtm) for c in range(NCORES)]
    res = run_bass_kernel_spmd(nc, in_maps, core_ids=list(range(NCORES)))
    out = np.concatenate([np.asarray(res.results[c]["out"], dtype=np.float32) for c in range(NCORES)], axis=0)
    return out
```

```python
import numpy as np
import concourse.bass as bass
import concourse.mybir as mybir
from concourse.bass_utils import run_bass_kernel_spmd

F32 = mybir.dt.float32
BF16 = mybir.dt.bfloat16
ALU = mybir.AluOpType
AF = mybir.ActivationFunctionType
AX = mybir.AxisListType

NCORES = 8
S = 2048
D = 1024
NT = S // 128
SEQ_PER_CORE = 2
ALPHA = 2.0 ** 0.25
LN_EPS = 1e-5
IDX_SCALE = (4 ** -0.5) * (64 ** -0.5)
NF = 22
TMW = 588
NBIS = 14
NEG = -1.0e30
MASKB = -262144.0


class Prog:
    CE = ("pe", "act", "dve", "pool")

    def __init__(self, nc, kdma=12):
        self.nc = nc
        self.ops = {e: [] for e in ("pe", "act", "dve", "pool", "sp")}
        self.cnt = {e: 0 for e in self.CE}
        self.last_w = {}
        self.readers = {}
        self.seen = {e: {} for e in self.ops}
        self.kdma = kdma
        self.ring = {"sp": [0] * kdma, "pool": [0] * kdma}
        self.ring_next = {"sp": 0, "pool": 0}
        self.nops = 0
        self.floor = {e: {} for e in self.ops}
        self.ps_last = {}

    def barrier(self):
        cur = {e: self.cnt[e] for e in self.CE if self.cnt[e] > 0}
        for q in ("sp", "pool"):
            for k in range(self.kdma):
                if self.ring[q][k] > 0:
                    cur["d%s%d" % (q, k)] = 16 * self.ring[q][k]
        for e in self.floor:
            self.floor[e] = dict(cur)

    def _deps(self, reads, writes):
        deps = {}

        def add(tok):
            if tok is None:
                return
            k, v = tok
            if deps.get(k, 0) < v:
                deps[k] = v
        for r in reads:
            add(self.last_w.get(r))
        for w in writes:
            add(self.last_w.get(w))
            for k, v in self.readers.get(w, {}).items():
                add((k, v))
        return deps

    def _commit(self, tok, reads, writes):
        for r in reads:
            d = self.readers.setdefault(r, {})
            if d.get(tok[0], 0) < tok[1]:
                d[tok[0]] = tok[1]
        for w in writes:
            self.last_w[w] = tok
            self.readers[w] = {}

    def _waits(self, eng, deps):
        waits = []
        fl = self.floor[eng]
        if fl:
            for k, v in fl.items():
                if deps.get(k, 0) < v:
                    deps[k] = v
            self.floor[eng] = {}
        for k, v in deps.items():
            if k == "pe" and eng == "pe":
                continue
            if self.seen[eng].get(k, 0) >= v:
                continue
            self.seen[eng][k] = v
            waits.append((k, v))
        return waits

    def op(self, eng, fn, reads=(), writes=(), inc=True):
        assert eng in self.CE
        if eng != "pe":
            assert inc
        deps = self._deps(reads, writes)
        idx = self.cnt[eng] + 1
        if inc:
            self.cnt[eng] = idx
        tok = (eng, idx)
        for r in reads:
            if isinstance(r, tuple) and r[0] == "ps":
                prev = self.ps_last.get(r[1])
                if prev is not None and prev[0] != eng and deps.get(prev[0], 0) < prev[1]:
                    deps[prev[0]] = prev[1]
                self.ps_last[r[1]] = tok
        waits = self._waits(eng, deps)
        self.ops[eng].append((waits, fn, (eng, 1) if inc else None))
        self._commit(tok, reads, writes)
        self.nops += 1
        return tok

    def dma(self, q, fn, reads=(), writes=()):
        deps = self._deps(reads, writes)
        k = self.ring_next[q] % self.kdma
        self.ring_next[q] += 1
        key = "d%s%d" % (q, k)
        if self.ring[q][k] > 0:
            v = 16 * self.ring[q][k]
            if deps.get(key, 0) < v:
                deps[key] = v
        waits = self._waits(q, deps)
        self.ring[q][k] += 1
        tok = (key, 16 * self.ring[q][k])
        self.ops[q].append((waits, fn, (key, 16)))
        self._commit(tok, reads, writes)
        self.nops += 1
        return tok

    def emit(self):
        nc = self.nc
        names = list(self.CE) + ["d%s%d" % (q, k) for q in ("sp", "pool") for k in range(self.kdma)]
        fin = []
        for q in ("sp", "pool"):
            for k in range(self.kdma):
                if self.ring[q][k] > 0:
                    fin.append(("d%s%d" % (q, k), 16 * self.ring[q][k]))
        for e in self.CE:
            if self.cnt[e] > 0:
                fin.append((e, self.cnt[e]))
        ops = self.ops
        import contextlib
        with contextlib.ExitStack() as st:
            sems = {n: st.enter_context(nc.semaphore("s_" + n)) for n in names}
            block = st.enter_context(nc.Block())

            def replay(eng_name):
                def run(e):
                    for waits, fn, inc in ops[eng_name]:
                        for k, v in waits:
                            e.wait_ge(sems[k], v)
                        ins = fn(e)
                        if inc is not None:
                            ins.then_inc(sems[inc[0]], inc[1])
                    if eng_name == "sp":
                        for k, v in fin:
                            e.wait_ge(sems[k], v)
                return run
            block.sync(replay("sp"))
            block.tensor(replay("pe"))
            block.scalar(replay("act"))
            block.vector(replay("dve"))
            block.gpsimd(replay("pool"))


def _rot(cols):
    return np.concatenate([cols[32:], cols[:32]])


def _layout_w_in(w_in):
    w = w_in[0]
    oq, ok, ov, oiq, oik, oiw, oz, oxbc, odt = 0, 512, 576, 640, 896, 960, 964, 1476, 2244
    tiles = []
    qh = [np.arange(oq + 64 * h, oq + 64 * (h + 1)) for h in range(8)]
    for p in range(4):
        tiles.append(np.concatenate([qh[2 * p], qh[2 * p + 1]]))
    for p in range(4):
        tiles.append(np.concatenate([_rot(qh[2 * p]), _rot(qh[2 * p + 1])]))
    kc = np.arange(ok, ok + 64)
    tiles.append(np.concatenate([kc, kc]))
    tiles.append(np.concatenate([_rot(kc), _rot(kc)]))
    ih = [np.arange(oiq + 64 * h, oiq + 64 * (h + 1)) for h in range(4)]
    for p in range(2):
        tiles.append(np.concatenate([ih[2 * p], ih[2 * p + 1]]))
    for p in range(2):
        tiles.append(np.concatenate([_rot(ih[2 * p]), _rot(ih[2 * p + 1])]))
    ikc = np.arange(oik, oik + 64)
    tiles.append(np.concatenate([ikc, ikc]))
    tiles.append(np.concatenate([_rot(ikc), _rot(ikc)]))
    for t in range(6):
        tiles.append(np.arange(oxbc + 128 * t, oxbc + 128 * (t + 1)))
    fm_cols = np.concatenate(tiles)
    assert fm_cols.shape[0] == NF * 128
    tm_cols = np.concatenate([np.arange(oz, oz + 512), np.arange(ov, ov + 64), np.arange(oiw, oiw + 4),
                              np.arange(odt, odt + 8)])
    assert tm_cols.shape[0] == TMW
    return np.ascontiguousarray(w[:, fm_cols]), np.ascontiguousarray(w[:, tm_cols])


def _consts():
    c = {}
    c["ident"] = np.eye(128, dtype=np.float32)
    s_ = np.arange(128)
    c["tri"] = (s_[:, None] <= s_[None, :]).astype(np.float32)
    c["cneg"] = np.where(s_[None, :] <= s_[:, None], 0.0, NEG).astype(np.float32)
    inv = 10000.0 ** (-np.arange(0, 64, 2, dtype=np.float32) / 64.0)
    ang = np.arange(S, dtype=np.float32)[:, None] * inv[None, :]
    cos, sin = np.cos(ang).astype(np.float32), np.sin(ang).astype(np.float32)
    cosF = np.concatenate([cos, cos], 1).T
    sinS = np.concatenate([-sin, sin], 1).T
    c["cosF"] = np.ascontiguousarray(np.concatenate([cosF, cosF], 0))
    c["sinS"] = np.ascontiguousarray(np.concatenate([sinS, sinS], 0))
    sel = np.zeros((16, 16, 128), np.float32)
    for e in range(16):
        sel[e, e, :] = 1.0
    c["sel"] = sel.reshape(16, 16 * 128)
    c["halfpow"] = np.broadcast_to((0.5 ** np.arange(1, NBIS + 3, dtype=np.float32))[None, :], (128, NBIS + 2)).copy()
    return c


def _rep(v, n=128):
    return np.ascontiguousarray(np.broadcast_to(np.asarray(v, np.float32).reshape(1, -1), (n, np.asarray(v).size)))


def MM(P, out, lhsT, rhs, start, stop, r, w, inc=True):
    return P.op("pe", lambda e: e.matmul(out, lhsT=lhsT, rhs=rhs, start=start, stop=stop), r, w, inc)


def TR(P, out, in_, ident, r, w, inc=True):
    return P.op("pe", lambda e: e.transpose(out, in_, ident), r, w, inc)


def ACT(P, out, in_, func, r, w, bias=0.0, scale=1.0, accum=None):
    if accum is None:
        return P.op("act", lambda e: e.activation(out=out, in_=in_, func=func, bias=bias, scale=scale), r, w)
    return P.op("act", lambda e: e.activation(out=out, in_=in_, func=func, bias=bias, scale=scale, accum_out=accum), r, w)


def TT(P, eng, out, a, b, op, r, w):
    return P.op(eng, lambda e: e.tensor_tensor(out, a, b, op), r, w)


def TS(P, eng, out, a, s1, s2, op0, op1, r, w, accum=None):
    if op1 is None:
        return P.op(eng, lambda e: e.tensor_scalar(out, a, s1, None, op0=op0), r, w)
    if accum is None:
        return P.op(eng, lambda e: e.tensor_scalar(out, a, s1, s2, op0=op0, op1=op1), r, w)
    return P.op(eng, lambda e: e.tensor_scalar(out, a, s1, s2, op0=op0, op1=op1, accum_out=accum), r, w)


def STT(P, eng, out, a, scalar, b, op0, op1, r, w):
    return P.op(eng, lambda e: e.scalar_tensor_tensor(out, a, scalar, b, op0=op0, op1=op1), r, w)


def CP(P, eng, out, in_, r, w):
    if eng == "act":
        return P.op("act", lambda e: e.copy(out, in_), r, w)
    return P.op(eng, lambda e: e.tensor_copy(out, in_), r, w)


def MSET(P, eng, ap, val, r, w):
    return P.op(eng, lambda e: e.memset(ap, val), r, w)


def DMA(P, q, out, in_, r, w):
    return P.dma(q, lambda e: e.dma_start(out=out, in_=in_), r, w)


class Rot:
    def __init__(self, items):
        self.items = list(items)
        self.i = 0

    def next(self):
        v = self.items[self.i % len(self.items)]
        self.i += 1
        return v


class Arena:
    def __init__(self, nc, lo=16512, hi=229344):
        self.nc = nc
        self.free_list = [(lo, hi)]
        self.live = {}
        self.uid = 0
        self.cache = {}

    def alloc(self, name, shape, dt):
        n = 1
        for d in shape[1:]:
            n *= d
        size = n * (4 if dt == F32 else 2)
        size = (size + 31) // 32 * 32
        for i, (a, b) in enumerate(self.free_list):
            if b - a >= size:
                self.free_list[i] = (a + size, b)
                if a + size == b:
                    self.free_list.pop(i)
                key = (name, tuple(shape), str(dt), a)
                h = self.cache.get(key)
                if h is None:
                    self.uid += 1
                    h = self.nc.alloc_sbuf_tensor_at("sb%d_%s" % (self.uid, name), list(shape), dt, offset=a)
                    self.cache[key] = h
                self.live[id(h)] = (a, size, h)
                return h
        raise RuntimeError("arena out of SBUF for %s (%d bytes); free=%s" % (name, size, self.free_list))

    def free(self, *hs):
        for h in hs:
            a, size, _ = self.live.pop(id(h))
            self.free_list.append((a, a + size))
        self.free_list.sort()
        merged = []
        for a, b in self.free_list:
            if merged and merged[-1][1] == a:
                merged[-1] = (merged[-1][0], b)
            else:
                merged.append((a, b))
        self.free_list = merged


def build(dbg=(), nseq=SEQ_PER_CORE, stop_after=None):
    nc = bass.Bass("TRN2", target_bir_lowering=False)
    P = Prog(nc)
    A = Arena(nc)
    dbg = set(dbg)
    dumps = {}

    def din(name, shape):
        return nc.dram_tensor(name, list(shape), F32, kind="ExternalInput").ap()

    x_d = din("x", [SEQ_PER_CORE, S, D])
    wfm_d = din("wfm", [D, NF * 128])
    wtm_d = din("wtm", [D, TMW])
    ident_d = din("ident", [128, 128])
    tri_d = din("tri", [128, 128])
    cneg_d = din("cneg", [128, 128])
    cosF_d = din("cosF", [128, S])
    sinS_d = din("sinS", [128, S])
    sel_d = din("sel", [16, 16 * 128])
    halfpow_d = din("halfpow", [128, NBIS + 2])
    convw_d = din("convw", [128, 24])
    convb_d = din("convb", [128, 6])
    dtb_d = din("dtb", [128, 8])
    alog_d = din("alog", [128, 8])
    dskip_d = din("dskip", [128, 512])
    nw_d = din("nw", [128, 512])
    nwc_d = din("nwc", [128, 4])
    wout_d = din("wout", [D, D])
    ln1g_d = din("ln1g", [128, D])
    ln1b_d = din("ln1b", [128, D])
    ln2g_d = din("ln2g", [128, D])
    ln2b_d = din("ln2b", [128, D])
    wr_d = din("wr", [D, 20])
    br_d = din("br", [128, 20])
    wg_d = din("wg", [16, D, 256])
    wu_d = din("wu", [16, D, 256])
    wd_d = din("wd", [16, 256, D])
    out_d = nc.dram_tensor("out", [SEQ_PER_CORE, S, D], F32, kind="ExternalOutput").ap()
    hscr_d = nc.dram_tensor("hscr", [SEQ_PER_CORE, S, D], F32, kind="Internal").ap()

    import contextlib
    with contextlib.ExitStack() as es:
        ps = [es.enter_context(nc.psum_tensor("ps%d" % i, [128, 512], F32)) for i in range(8)]

        def psf(b):
            return ps[b][:]

        def psb(b):
            return ps[b][:].bitcast(BF16)

        def PSR(b):
            return ("ps", b)

        def dump(name, ap, shape, reads, dt=None):
            if name not in dbg:
                return
            t = nc.dram_tensor("dbg_" + name, list(shape), dt or ap.dtype, kind="ExternalOutput").ap()
            dumps[name] = t
            DMA(P, "sp", t, ap, reads, [])

        ident_f = A.alloc("ident_f", [128, 128], F32)
        ident_b = A.alloc("ident_b", [128, 128], BF16)
        tri_f = A.alloc("tri_f", [128, 128], F32)
        tri_b = A.alloc("tri_b", [128, 128], BF16)
        cneg = A.alloc("cneg", [128, 128], F32)
        ones_f = A.alloc("ones_f", [128, 128], F32)
        DMA(P, "sp", ident_f[:], ident_d, [], ["ident_f"])
        DMA(P, "pool", ident_b[:], ident_d, [], ["ident_b"])
        DMA(P, "sp", tri_f[:], tri_d, [], ["tri_f"])
        DMA(P, "pool", tri_b[:], tri_d, [], ["tri_b"])
        DMA(P, "sp", cneg[:], cneg_d, [], ["cneg"])
        MSET(P, "dve", ones_f[:], 1.0, [], ["ones_f"])

        wtm_pref = [None]
        for seq in range(nseq):
            qT = A.alloc("qz", [128, 8, S], BF16)
            kT = A.alloc("kT", [128, S], BF16)
            iqT = A.alloc("iqz", [128, 4, S], BF16)
            ikT = A.alloc("ikT", [128, S], BF16)
            Vext = A.alloc("Vext", [128, NT, 192], BF16)
            iws = A.alloc("iws", [128, NT, 4], F32)
            xbcT = A.alloc("xbcT", [128, 6, S], BF16)
            siluz = A.alloc("siluz", [128, NT, 512], BF16)
            dtr = A.alloc("dtr", [128, NT, 8], F32)

            xT = A.alloc("xT", [128, 8, S], BF16)
            if wtm_pref[0] is not None:
                wtm = wtm_pref[0]
                wtm_pref[0] = None
            else:
                wtm = A.alloc("wtm", [128, 8, TMW], BF16)
                for c in range(8):
                    DMA(P, "pool", wtm[:, c, :], wtm_d[c * 128:(c + 1) * 128, :], [], [("wtm", c)])
            xb = [A.alloc("xb%d" % i, [128, D], BF16) for i in range(4)]
            MSET(P, "pool", qT[:], 0.0, [], ["qz0"])
            MSET(P, "pool", iqT[:], 0.0, [], ["iqz0"])
            MSET(P, "pool", Vext[:, :, 64:128], 0.0, [], [("Vext0",)])
            MSET(P, "pool", Vext[:, :, 64:65], 1.0, [("Vext0",)], [("Vext0",)])
            tr_banks = Rot([0, 1])
            fm_banks = Rot([4, 5, 6, 7])
            cp_eng = Rot(["act", "dve"])
            def a_dma(t):
                tok = slice(t * 128, (t + 1) * 128)
                DMA(P, "pool", xb[t % 4][:], x_d[seq, tok, :], [], [("xb", t % 4)])

            def a_tr(t):
                tok = slice(t * 128, (t + 1) * 128)
                xbuf = xb[t % 4]
                b = tr_banks.next()
                pv = psb(b)
                for c in range(8):
                    TR(P, pv[:, c * 128:(c + 1) * 128], xbuf[:, c * 128:(c + 1) * 128], ident_b[:],
                       [("xb", t % 4), "ident_b"], [PSR(b)], inc=(c == 7))
                CP(P, cp_eng.next(), xT[:, :, tok], pv.rearrange("p (c t) -> p c t", c=8),
                   [PSR(b)], [("xT", t)])

            def a_mm(t):
                tok = slice(t * 128, (t + 1) * 128)
                bz, bv = (2, 3) if t % 2 == 0 else (4, 5)
                for c in range(8):
                    MM(P, psf(bz), xT[:, c, tok], wtm[:, c, 0:512], c == 0, c == 7,
                       [("xT", t), ("wtm", c)], [PSR(bz)], inc=(c == 7))
                for c in range(8):
                    MM(P, ps[bv][:, 0:76], xT[:, c, tok], wtm[:, c, 512:588], c == 0, c == 7,
                       [("xT", t), ("wtm", c)], [PSR(bv)], inc=(c == 7))
                ACT(P, siluz[:, t, :], psf(bz), AF.Silu, [PSR(bz)], [("siluz", t)])
                CP(P, "dve", Vext[:, t, 0:64], ps[bv][:, 0:64], [PSR(bv)], [("Vext", t)])
                CP(P, "dve", Vext[:, t, 128:192], ps[bv][:, 0:64], [PSR(bv)], [("Vext", t)])
                TS(P, "dve", iws[:, t, :], ps[bv][:, 64:68], IDX_SCALE, None, ALU.mult, None,
                   [PSR(bv)], [("iws", t)])
                CP(P, "dve", dtr[:, t, :], ps[bv][:, 68:76], [PSR(bv)], [("dtr", t)])

            for t in range(3):
                a_dma(t)
            a_tr(0)
            for t in range(NT):
                if t + 3 < NT:
                    a_dma(t + 3)
                if t + 1 < NT:
                    a_tr(t + 1)
                a_mm(t)

            P.barrier()
            A.free(wtm, *xb)
            cosF = A.alloc("cosF", [128, S], F32)
            sinS = A.alloc("sinS", [128, S], F32)
            convw = A.alloc("convw", [128, 24], F32)
            convb = A.alloc("convb", [128, 6], F32)
            diagw = A.alloc("diagw", [128, 24, 128], BF16)
            uT = A.alloc("uT", [128, 6, S + 3], BF16)
            rt1 = [A.alloc("rt1_%d" % i, [128, 512], F32) for i in range(2)]
            rt2 = [A.alloc("rt2_%d" % i, [128, 512], F32) for i in range(2)]
            wsl = [A.alloc("wsl%d" % i, [128, 8, 128], BF16) for i in range(4)]
            p1_tmp = [xT, cosF, sinS, convw, convb, diagw, uT] + rt1 + rt2 + wsl
            DMA(P, "sp", cosF[:], cosF_d, [], ["cosF"])
            DMA(P, "sp", sinS[:], sinS_d, [], ["sinS"])
            DMA(P, "sp", convw[:], convw_d, [], ["convw"])
            DMA(P, "sp", convb[:], convb_d, [], ["convb"])
            for i in range(24):
                TS(P, "dve", diagw[:, i, :], ident_f[:], convw[:, i:i + 1], None, ALU.mult, None,
                   ["ident_f", "convw"], [("diagw", i)])
            MSET(P, "pool", uT[:, :, 0:3], 0.0, [], [("uT", -1)])
            wsi = [0]

            worder = [0, 4, 1, 5, 2, 6, 3, 7, 8, 9, 10, 12, 11, 13, 14, 15, 16, 17, 18, 19, 20, 21]
            wslot = {}

            def prefetch(n):
                for _ in range(n):
                    if wsi[0] >= len(worder):
                        return
                    f = worder[wsi[0]]
                    k = wsi[0] % 4
                    wsi[0] += 1
                    wslot[f] = k
                    DMA(P, "pool", wsl[k][:], wfm_d[:, f * 128:(f + 1) * 128].rearrange("(c p) f -> p c f", p=128),
                        [], [("wsl", k)])

            def load_w(f):
                return wslot[f]

            prefetch(4)

            def fm_proj(k, tg, b):
                tokg = slice(tg * 512, tg * 512 + 512)
                for c in range(8):
                    MM(P, psf(b), wsl[k][:, c, :], xT[:, c, tokg], c == 0, c == 7,
                       [("xT", tg * 4 + i) for i in range(4)] + [("wsl", k)], [PSR(b)], inc=(c == 7))

            pairs = [(0, 4, (qT, 0), "qT0"), (1, 5, (qT, 1), "qT1"), (2, 6, (qT, 2), "qT2"), (3, 7, (qT, 3), "qT3"),
                     (8, 9, kT, "kT"), (10, 12, (iqT, 0), "iqT0"), (11, 13, (iqT, 1), "iqT1"), (14, 15, ikT, "ikT")]
            rti = 0
            for fa, fr, dstf, dname in pairs:
                ka, kr = load_w(fa), load_w(fr)
                for tg in range(4):
                    tokg = slice(tg * 512, tg * 512 + 512)
                    ba, br = fm_banks.next(), fm_banks.next()
                    fm_proj(ka, tg, ba)
                    fm_proj(kr, tg, br)
                    r1, r2 = rt1[rti % 2], rt2[rti % 2]
                    TT(P, "dve", r1[:], psf(ba), cosF[:, tokg], ALU.mult, [PSR(ba), "cosF"], [("rt1", rti % 2)])
                    TT(P, "dve", r2[:], psf(br), sinS[:, tokg], ALU.mult, [PSR(br), "sinS"], [("rt2", rti % 2)])
                    rr_ = [("rt1", rti % 2), ("rt2", rti % 2), "qz0", "iqz0"]
                    if isinstance(dstf, tuple):
                        dt_, pp = dstf
                        TT(P, "pool", dt_[0:64, 2 * pp, tokg], r1[0:64, :], r2[0:64, :], ALU.add, rr_, [(dname, tg, 0)])
                        TT(P, "pool", dt_[64:128, 2 * pp + 1, tokg], r1[64:128, :], r2[64:128, :], ALU.add, rr_,
                           [(dname, tg, 1)])
                    else:
                        TT(P, "pool", dstf[:, tokg], r1[:], r2[:], ALU.add, rr_, [(dname, tg)])
                    rti += 1
                prefetch(2)
            for ct in range(6):
                k = load_w(16 + ct)
                for tg in range(4):
                    t0 = tg * 512
                    b = fm_banks.next()
                    fm_proj(k, tg, b)
                    CP(P, "act", uT[:, ct, 3 + t0:3 + t0 + 512], psf(b), [PSR(b)], [("uT", ct, tg)])
                prefetch(1)
                for tg in range(4):
                    t0 = tg * 512
                    b = fm_banks.next()
                    rr = [("uT", ct, tg), ("uT", -1)] + ([("uT", ct, tg - 1)] if tg > 0 else [])
                    for j in range(4):
                        MM(P, psf(b), diagw[:, ct * 4 + j, :], uT[:, ct, t0 + j:t0 + j + 512], j == 0, j == 3,
                           rr + [("diagw", ct * 4 + j)], [PSR(b)], inc=(j == 3))
                    ACT(P, xbcT[:, ct, t0:t0 + 512], psf(b), AF.Silu, [PSR(b), "convb"], [("xbcT", ct, tg)],
                        bias=convb[:, ct:ct + 1])
            g4 = range(4)
            P.barrier()
            dump("qz", qT[:], [128, 8, S], [])
            dump("iqz", iqT[:], [128, 4, S], [])
            A.free(*p1_tmp)
            if stop_after == 1:
                A.free(qT, kT, iqT, ikT, Vext, iws, xbcT, siluz, dtr)
                continue

            mixT = A.alloc("mixT", [128, 8, S], BF16)
            dtb = A.alloc("dtb", [128, 8], F32)
            arep = A.alloc("arep", [128, 8], F32)
            dskip = A.alloc("dskip", [128, 512], F32)
            nwr = A.alloc("nwr", [128, 512], F32)
            dt_all = A.alloc("dt_all", [128, NT, 8], F32)
            A_all = A.alloc("A_all", [128, NT, 8], F32)
            acum = A.alloc("acum", [128, NT, 8], F32)
            tot = A.alloc("tot", [128, NT, 8], F32)
            ds_all = A.alloc("ds_all", [128, NT, 8], F32)
            cdec = A.alloc("cdec", [128, NT, 8], F32)
            state = A.alloc("state", [128, 8, 64], F32)
            NB = 2
            xdt = [A.alloc("xdt%d" % i, [128, 8, 64], BF16) for i in range(NB)]
            xdtd = [A.alloc("xdtd%d" % i, [128, 8, 64], BF16) for i in range(NB)]
            dsk = [A.alloc("dsk%d" % i, [128, 512], F32) for i in range(NB)]
            Btok = [A.alloc("Btok%d" % i, [128, 128], BF16) for i in range(NB)]
            Gm = [A.alloc("Gm%d" % i, [128, 2, 128], BF16) for i in range(NB)]
            Ab = [A.alloc("Ab%d" % i, [128, 2, 8, 128], BF16) for i in range(NB)]
            A_hl = A.alloc("A_hl", [128, 2, NT, 8], BF16)
            A_res = A.alloc("A_res", [128, NT, 8], F32)
            Dm = [A.alloc("Dm%d" % i, [128, 8, 128], F32) for i in range(NB)]
            Lm = [A.alloc("Lm%d" % i, [128, 8, 128], BF16) for i in range(NB)]
            Eb = [A.alloc("Eb%d" % i, [128, 8, 128], BF16) for i in range(NB)]
            MT = [A.alloc("MT%d" % i, [128, 8, 128], BF16) for i in range(NB)]
            Cp = [A.alloc("Cp%d" % i, [128, 8, 128], BF16) for i in range(NB)]
            CTz = [A.alloc("CTz%d" % i, [128, 2, 128], BF16) for i in range(NB)]
            prevb = [A.alloc("prevb%d" % i, [128, 8, 64], BF16) for i in range(NB)]
            y1 = [A.alloc("y1_%d" % i, [128, 512], F32) for i in range(NB)]
            y2 = [A.alloc("y2_%d" % i, [128, 512], F32) for i in range(NB)]
            yo = [A.alloc("yo%d" % i, [128, 512], BF16) for i in range(NB)]
            junk = [A.alloc("junk%d" % i, [128, 512], BF16) for i in range(NB)]
            ms = [A.alloc("ms%d" % i, [128, 2], F32) for i in range(NB)]
            p3_tmp = ([A_hl, A_res, dtb, arep, dskip, nwr, dt_all, A_all, acum, tot, ds_all, cdec, state] + xdt + xdtd + dsk + Btok + Gm
                      + Ab + Dm + Lm + Eb + MT + Cp + CTz + prevb + y1 + y2 + yo + junk + ms)

            for i in range(NB):
                MSET(P, "pool", Cp[i][:], 0.0, [], [("Cp", i, 0), ("Cp", i, 1)])
                MSET(P, "pool", CTz[i][:], 0.0, [], [("CTz", i)])
            DMA(P, "sp", dtb[:], dtb_d, [], ["dtb"])
            DMA(P, "sp", arep[:], alog_d, [], ["arep"])
            DMA(P, "sp", dskip[:], dskip_d, [], ["dskip"])
            DMA(P, "sp", nwr[:], nw_d, [], ["nwr"])
            ACT(P, arep[:], arep[:], AF.Exp, ["arep"], ["arep"])
            TS(P, "dve", arep[:], arep[:], -1.0, None, ALU.mult, None, ["arep"], ["arep"])
            alldtr = [("dtr", t) for t in range(NT)]
            TT(P, "dve", dt_all[:], dtr[:], dtb[:].unsqueeze(1).to_broadcast([128, NT, 8]), ALU.add,
               alldtr + ["dtb"], ["dt_all"])
            ACT(P, dt_all[:], dt_all[:], AF.Exp, ["dt_all"], ["dt_all"])
            ACT(P, dt_all[:], dt_all[:], AF.Ln, ["dt_all"], ["dt_all"], bias=1.0)
            TT(P, "dve", A_all[:], dt_all[:], arep[:].unsqueeze(1).to_broadcast([128, NT, 8]), ALU.mult,
               ["dt_all", "arep"], ["A_all"])
            CP(P, "dve", A_hl[:, 0, :, :], A_all[:], ["A_all"], ["A_hl0"])
            TT(P, "dve", A_res[:], A_all[:], A_hl[:, 0, :, :], ALU.subtract, ["A_all", "A_hl0"], ["A_res"])
            CP(P, "dve", A_hl[:, 1, :, :], A_res[:], ["A_res"], ["A_hl1"])
            A2 = A_all[:].rearrange("p c h -> p (c h)")
            MM(P, ps[0][:, 0:128], tri_f[:], A2, True, True, ["tri_f", "A_all"], [PSR(0)])
            MM(P, ps[1][:, 0:128], ones_f[:], A2, True, True, ["ones_f", "A_all"], [PSR(1)])
            CP(P, "dve", acum[:].rearrange("p c h -> p (c h)"), ps[0][:, 0:128], [PSR(0)], ["acum"])
            CP(P, "dve", tot[:].rearrange("p c h -> p (c h)"), ps[1][:, 0:128], [PSR(1)], ["tot"])
            TT(P, "dve", ds_all[:], tot[:], acum[:], ALU.subtract, ["tot", "acum"], ["ds_all"])
            ACT(P, ds_all[:], ds_all[:], AF.Exp, ["ds_all"], ["ds_all"])
            ACT(P, cdec[:], tot[:], AF.Exp, ["tot"], ["cdec"])
            trb = Rot([0, 1])
            def ssd_s1(c):
                    i = c % NB
                    tok = slice(c * 128, (c + 1) * 128)
                    g4 = c // 4
                    xres = [("xbcT", ct, g4) for ct in range(6)]
                    CP(P, "dve", Ab[i][:], A_hl[:, :, c, :].unsqueeze(3).to_broadcast([128, 2, 8, 128]), ["A_hl0", "A_hl1"],
                       [("Ab", i)])
                    for h in range(8):
                        bb = 3 + h // 4
                        MM(P, ps[bb][:, (h % 4) * 128:(h % 4 + 1) * 128], Ab[i][:, 0, h, :], tri_b[:], True, False,
                           [("Ab", i), "tri_b"], [PSR(bb)], inc=False)
                        MM(P, ps[bb][:, (h % 4) * 128:(h % 4 + 1) * 128], Ab[i][:, 1, h, :], tri_b[:], False, True,
                           [("Ab", i), "tri_b"], [PSR(bb)], inc=(h % 4 == 3))
                    b = trb.next()
                    pv = psb(b)
                    for ct in range(5):
                        TR(P, pv[:, ct * 128:(ct + 1) * 128], xbcT[:, ct, tok], ident_b[:], xres + ["ident_b"], [PSR(b)],
                           inc=(ct == 4))
                    xs_ps = pv[:, 0:512].rearrange("p (h d) -> p h d", h=8)
                    TT(P, "dve", xdt[i][:], xs_ps, dt_all[:, c, :].unsqueeze(2).to_broadcast([128, 8, 64]), ALU.mult,
                       [PSR(b), "dt_all"], [("xdt", i)])
                    TT(P, "dve", dsk[i][:], pv[:, 0:512], dskip[:], ALU.mult, [PSR(b), "dskip"], [("dsk", i)])
                    CP(P, "act", Btok[i][:], pv[:, 512:640], [PSR(b)], [("Btok", i)])
                    TT(P, "dve", xdtd[i][:], xdt[i][:], ds_all[:, c, :].unsqueeze(2).to_broadcast([128, 8, 64]), ALU.mult,
                       [("xdt", i), "ds_all"], [("xdtd", i)])
                    for g in range(2):
                        CP(P, "pool", CTz[i][64 * g:64 * g + 64, g, :], xbcT[64 * g:64 * g + 64, 5, tok], xres, [("CTz", i)])
                    for g in range(2):
                        MM(P, ps[2][:, g * 128:(g + 1) * 128], xbcT[:, 4, tok], CTz[i][:, g, :],
                           True, True, xres + [("CTz", i)], [PSR(2)], inc=(g == 1))
                    TT(P, "dve", Gm[i][:], ps[2][:, 0:256].rearrange("p (g l) -> p g l", g=2),
                       tri_f[:].unsqueeze(1).to_broadcast([128, 2, 128]), ALU.mult, [PSR(2), "tri_f"], [("Gm", i)])
                    for g in range(2):
                        TT(P, "dve", Dm[i][:, 4 * g:4 * g + 4, :], ps[3 + g][:].rearrange("p (h l) -> p h l", h=4),
                           acum[:, c, 4 * g:4 * g + 4].unsqueeze(2).to_broadcast([128, 4, 128]), ALU.subtract,
                           [PSR(3 + g), "acum"], [("Dm", i)])
                    ACT(P, Dm[i][:], Dm[i][:], AF.Relu, [("Dm", i)], [("Dm", i)], scale=-1.0)
                    ACT(P, Lm[i][:], Dm[i][:], AF.Exp, [("Dm", i)], [("Lm", i)], scale=-1.0)
                    for g in range(2):
                        ACT(P, Eb[i][:, g * 4:(g + 1) * 4, :], ps[3 + g][:].rearrange("p (h l) -> p h l", h=4), AF.Exp,
                            [PSR(3 + g)], [("Eb", i, g)])
                    for g in range(2):
                        TT(P, "dve", MT[i][:, g * 4:(g + 1) * 4, :], Lm[i][:, g * 4:(g + 1) * 4, :],
                           Gm[i][:, g, :].unsqueeze(1).to_broadcast([128, 4, 128]), ALU.mult,
                           [("Lm", i), ("Gm", i)], [("MT", i, g)])
                        TT(P, "dve", Cp[i][64 * g:64 * g + 64, g * 4:(g + 1) * 4, :], Eb[i][64 * g:64 * g + 64, g * 4:(g + 1) * 4, :],
                           xbcT[64 * g:64 * g + 64, 5, tok].unsqueeze(1).to_broadcast([64, 4, 128]), ALU.mult,
                           [("Eb", i, g)] + xres, [("Cp", i, g)])

            def ssd_s2(c):
                    i = c % NB
                    tok = slice(c * 128, (c + 1) * 128)
                    g4 = c // 4
                    xres = [("xbcT", ct, g4) for ct in range(6)]
                    if c > 0:
                        CP(P, "pool", prevb[i][:], state[:], ["state"], [("prevb", i)])
                    for h in range(8):
                        g = h // 4
                        MM(P, ps[5][:, h * 64:(h + 1) * 64], MT[i][:, h, :], xdt[i][:, h, :], True, c == 0,
                           [("MT", i, g), ("xdt", i)], [PSR(5)], inc=(c == 0 and h == 7))
                        if c > 0:
                            MM(P, ps[5][:, h * 64:(h + 1) * 64], Cp[i][:, h, :],
                               prevb[i][:, h, :], False, True, [("Cp", i, g), ("prevb", i)], [PSR(5)],
                               inc=(h == 7))
                    if c < NT - 1:
                        for h in range(8):
                            MM(P, ps[6][:, h * 64:(h + 1) * 64], Btok[i][:], xdtd[i][:, h, :], True, True,
                               [("Btok", i), ("xdtd", i)], [PSR(6)], inc=(h == 7))
                        st2 = state[:].rearrange("p h d -> p (h d)")
                        if c == 0:
                            CP(P, "dve", st2, psf(6), [PSR(6)], ["state"])
                        else:
                            TT(P, "dve", state[:], state[:], cdec[:, c, :].unsqueeze(2).to_broadcast([128, 8, 64]), ALU.mult,
                               ["state", "cdec", ("prevb", i)], ["state"])
                            TT(P, "dve", st2, st2, psf(6), ALU.add, ["state", PSR(6)], ["state"])
                    TT(P, "dve", y1[i][:], psf(5), dsk[i][:], ALU.add, [PSR(5), ("dsk", i)], [("y1", i)])
                    TT(P, "pool", y2[i][:], y1[i][:], siluz[:, c, :], ALU.mult, [("y1", i), ("siluz", c)], [("y2", i)])
                    ACT(P, junk[i][:], y2[i][:], AF.Square, [("y2", i)], [("junk", i), ("ms", i)], scale=float(512 ** -0.5),
                        accum=ms[i][:, 0:1])
                    ACT(P, ms[i][:, 1:2], ms[i][:, 0:1], AF.Ln, [("ms", i)], [("ms2", i)], bias=LN_EPS)
                    ACT(P, ms[i][:, 1:2], ms[i][:, 1:2], AF.Exp, [("ms2", i)], [("ms2", i)], scale=-0.5)

            def ssd_s3(c):
                    i = c % NB
                    tok = slice(c * 128, (c + 1) * 128)
                    ACT(P, yo[i][:], y2[i][:], AF.Copy, [("y2", i), ("ms2", i)], [("yo", i)], scale=ms[i][:, 1:2])
                    pv7 = psb(7)
                    for k in range(4):
                        TR(P, pv7[:, k * 128:(k + 1) * 128], yo[i][:, k * 128:(k + 1) * 128], ident_b[:], [("yo", i), "ident_b"],
                           [PSR(7)], inc=(k == 3))
                    CP(P, "act", mixT[:, 4:8, tok], pv7[:, 0:512].rearrange("p (k t) -> p k t", k=4), [PSR(7)], [("mixT", "s", c)])


            ssd_s1(0)
            for c in range(NT):
                if c + 1 < NT:
                    ssd_s1(c + 1)
                if c > 0:
                    ssd_s3(c - 1)
                ssd_s2(c)
            ssd_s3(NT - 1)
            dump("ssdT", mixT[:, 4:8, :], [128, 4, S], [("mixT", "s", c) for c in range(NT)])
            P.barrier()
            A.free(*p3_tmp)
            A.free(xbcT, siluz, dtr)
            if stop_after == 3:
                A.free(mixT, qT, kT, iqT, ikT, Vext, iws)
                continue

            I4 = A.alloc("I4", [128, 4, S], F32)
            junkb = A.alloc("junkb", [128, S], BF16)
            selb = [A.alloc("selb%d" % i, [128, S], BF16) for i in range(2)]
            Rt = [A.alloc("Rt%d" % i, [128, 512], F32) for i in range(4)]
            maskT = [A.alloc("maskT%d" % i, [128, NT, 512], BF16) for i in range(2)]
            Pexp = [A.alloc("Pexp%d" % i, [128, 512], BF16) for i in range(3)]
            Pm = []
            Osb = [A.alloc("Osb%d" % i, [128, 512], F32) for i in range(3)]
            rec = []
            Sden = A.alloc("Sden", [128, 2, 128], F32)
            halfpow = A.alloc("halfpow", [128, NBIS + 2], F32)
            aw = A.alloc("aw", [128, 4, 4], F32)
            sg = A.alloc("sg", [128, 4, 4], F32)
            lo0 = A.alloc("lo0", [128, 4], F32)
            hi0 = A.alloc("hi0", [128, 4], F32)
            wd = A.alloc("wd", [128, 4, NBIS + 2], F32)
            mid = A.alloc("mid", [128, 4], F32)
            cnt = A.alloc("cnt", [128, 4], F32)
            sacc = A.alloc("sacc", [128, 2], F32)
            lhalf = A.alloc("lhalf", [128, 2], F32)
            junka = A.alloc("junka", [128, S], BF16)
            u2 = A.alloc("u2", [128, 4], F32)
            tq = A.alloc("tq", [128, 4], F32)
            thr = A.alloc("thr", [128, 4], F32)
            p2_tmp = maskT + [sacc, lhalf, junka, I4, junkb, Sden, halfpow, aw, sg, lo0, hi0, wd, mid, cnt, u2, tq, thr] + selb + Rt + Pexp + Pm + Osb + rec
            DMA(P, "sp", halfpow[:], halfpow_d, [], ["halfpow"])
            MSET(P, "pool", Sden[:], 0.0, [], ["Sden"])
            MSET(P, "pool", Sden[64:65, 0, :], 1.0, ["Sden"], ["Sden"])
            MSET(P, "pool", Sden[0:1, 1, :], 1.0, ["Sden"], ["Sden"])
            ibank = Rot([0, 1, 2])
            oi_box = [0]

            def gen_IB(g):
                mT = maskT[g % 2]
                nkb = g + 1
                Ls = []
                for b in range(4):
                    qi = 4 * g + b
                    L = (qi + 1) * 128
                    Ls.append(L)
                    qtok = slice(qi * 128, (qi + 1) * 128)
                    STT(P, "dve", aw[:, b, :], iws[:, qi, :], -1.0, iws[:, qi, :], ALU.mult, ALU.max, [], [("aw", b)])
                    TS(P, "dve", sg[:, b, :], iws[:, qi, :], 0.0, 2.0, ALU.is_ge, ALU.mult, [], [("sg", b)])
                    TS(P, "dve", sg[:, b, :], sg[:, b, :], -1.0, None, ALU.add, None, [("sg", b)], [("sg", b)])
                    for kb in range(nkb):
                        w = min(512, L - kb * 512)
                        ks = slice(kb * 512, kb * 512 + w)
                        for h in range(4):
                            bk = ibank.next()
                            MM(P, ps[bk][:, 0:w], iqT[:, h, qtok], ikT[:, ks], True, True, [], [PSR(bk)])
                            ACT(P, Rt[h][:, 0:w], ps[bk][:, 0:w], AF.Relu, [PSR(bk), ("aw", b)], [("Rt", h)],
                                scale=aw[:, b, h:h + 1])
                        TS(P, "dve", I4[:, b, ks], Rt[0][:, 0:w], sg[:, b, 0:1], None, ALU.mult, None,
                           [("Rt", 0), ("sg", b)], [("I4", b)])
                        for h in range(1, 4):
                            STT(P, "dve", I4[:, b, ks], Rt[h][:, 0:w], sg[:, b, h:h + 1], I4[:, b, ks], ALU.mult, ALU.add,
                                [("Rt", h), ("sg", b), ("I4", b)], [("I4", b)])
                        yield 3.0 * w / 512 + 0.5
                    P.op("dve", lambda e, o=lo0[:, b:b + 1], i_=I4[:, b, 0:L]: e.tensor_reduce(o, i_, axis=AX.X, op=ALU.min),
                         [("I4", b)], [("lo0", b)])
                    TT(P, "dve", I4[:, b, qi * 128:(qi + 1) * 128], I4[:, b, qi * 128:(qi + 1) * 128], cneg[:], ALU.add,
                       [("I4", b), ("lo0", b), "cneg"], [("I4", b)])
                    P.op("dve", lambda e, o=hi0[:, b:b + 1], i_=I4[:, b, 0:L]: e.tensor_reduce(o, i_, axis=AX.X, op=ALU.max),
                         [("I4", b)], [("hi0", b)])
                    yield 2.2 * L / 1024 + 0.6
                allb = [("lo0", b) for b in range(4)] + [("hi0", b) for b in range(4)]
                TT(P, "dve", tq[:], hi0[:], lo0[:], ALU.subtract, allb, ["tq"])
                TT(P, "dve", wd[:], tq[:].unsqueeze(2).to_broadcast([128, 4, NBIS + 2]),
                   halfpow[:].unsqueeze(1).to_broadcast([128, 4, NBIS + 2]), ALU.mult, ["tq", "halfpow"], ["wd"])
                TT(P, "dve", mid[:], lo0[:], wd[:, :, 0], ALU.add, allb + ["wd"], ["mid"])
                yield 1.0
                MSET(P, "pool", lhalf[:, 0:1], Ls[2] / 2.0, [], ["lhalf"])
                MSET(P, "pool", lhalf[:, 1:2], Ls[3] / 2.0, [], ["lhalf"])
                for k in range(NBIS):
                    for b in (2, 3):
                        ACT(P, junka[:, 0:Ls[b]], I4[:, b, 0:Ls[b]], AF.Sign, [("I4", b), "mid"], ["junka", ("sacc", b)],
                            bias=mid[:, b:b + 1], scale=-1.0, accum=sacc[:, b - 2:b - 1])
                    for b in (0, 1):
                        TS(P, "dve", junkb[:, 0:Ls[b]], I4[:, b, 0:Ls[b]], mid[:, b:b + 1], 0.0, ALU.is_ge, ALU.add,
                           [("I4", b), "mid"], ["junkb", ("cnt", b)], accum=cnt[:, b:b + 1])
                    STT(P, "dve", cnt[:, 2:4], sacc[:, 0:2], -0.5, lhalf[:, 0:2], ALU.mult, ALU.add,
                        [("sacc", 2), ("sacc", 3), "lhalf"], [("cnt", 2)])
                    TS(P, "dve", u2[:], cnt[:], 256.0, 2.0, ALU.is_ge, ALU.mult, [("cnt", 0), ("cnt", 1), ("cnt", 2)], ["u2"])
                    STT(P, "dve", tq[:], u2[:], -1.0, wd[:, :, k + 1], ALU.add, ALU.mult, ["u2", "wd"], ["tq"])
                    TT(P, "dve", mid[:], mid[:], tq[:], ALU.add, ["mid", "tq"], ["mid"])
                    yield (Ls[0] + Ls[1]) / 960.0 + 1.2
                TT(P, "dve", thr[:], mid[:], wd[:, :, NBIS - 2], ALU.subtract, ["mid", "wd"], ["thr"])
                for b in range(4):
                    qi = 4 * g + b
                    sb_ = selb[b % 2]
                    TS(P, "dve", sb_[:, 0:Ls[b]], I4[:, b, 0:Ls[b]], thr[:, b:b + 1], MASKB, ALU.is_lt, ALU.mult,
                       [("I4", b), "thr"], [("selb", b % 2)])
                    for c0 in range(0, qi + 1, 8):
                        n = min(8, qi + 1 - c0)
                        pv = psb(3)
                        for j in range(n):
                            TR(P, pv[:, j * 128:(j + 1) * 128], sb_[:, (c0 + j) * 128:(c0 + j + 1) * 128], ident_b[:],
                               [("selb", b % 2)], [PSR(3)], inc=(j == n - 1))
                        CP(P, "act", mT[:, c0:c0 + n, b * 128:(b + 1) * 128],
                           pv[:, 0:n * 128].rearrange("p (c q) -> p c q", c=n), [PSR(3)], [("maskT", g % 2, b)])
                    yield Ls[b] / 1024.0 + 1.0

            def gen_A(g):
                mT = maskT[g % 2]
                mres = [("maskT", g % 2, b) for b in range(4)]
                nkc = 4 * g + 4
                items = [(h, c) for h in range(8) for c in range(nkc)]

                def emit_S(idx):
                    h, c = items[idx]
                    cs = max(0, c - 4 * g) * 128
                    qs = slice(g * 512 + cs, g * 512 + 512)
                    bk = 4 + idx % 3
                    MM(P, ps[bk][:, cs:512], kT[:, c * 128:(c + 1) * 128], qT[:, h, qs], True, False, [], [PSR(bk)], inc=False)
                    MM(P, ps[bk][:, cs:512], ident_b[:], mT[:, c, cs:512], False, True, mres, [PSR(bk)])
                    ACT(P, Pexp[idx % 3][:, cs:512], ps[bk][:, cs:512], AF.Exp, [PSR(bk)], [("Pexp", idx % 3)], scale=0.125)

                def emit_PV(idx):
                    h, c = items[idx]
                    par = h % 2
                    rows = slice(64 * par, 64 * par + 64)
                    vcols = slice(0, 128) if par == 0 else slice(64, 192)
                    cs = max(0, c - 4 * g) * 128
                    MM(P, ps[7][:, cs:512], Vext[:, c, vcols], Pexp[idx % 3][:, cs:512], c == 0, c == nkc - 1,
                       [("Pexp", idx % 3)], [PSR(7)], inc=True)
                    if c == nkc - 1:
                        o_ = oi_box[0]
                        ob = Osb[o_ % 3]
                        ores = ("Osb", o_ % 3)
                        dr = slice(64, 65) if par == 0 else slice(0, 1)
                        CP(P, "act", ob[:], psf(7), [PSR(7)], [ores])
                        ACT(P, ob[dr, :], ob[dr, :], AF.Ln, [ores], [ores])
                        ACT(P, ob[dr, :], ob[dr, :], AF.Exp, [ores], [ores], scale=-1.0)

                        def fin1(ob=ob, ores=ores, par=par):
                            MM(P, psf(3), Sden[:, par, :], ob[:], True, True, [ores, "Sden"], [PSR(3)])

                        def fin2(ob=ob, ores=ores, rows=rows, h=h):
                            TT(P, "dve", mixT[rows, h // 2, g * 512:(g + 1) * 512], ob[rows, :], ps[3][rows, :], ALU.mult,
                               [ores, PSR(3)], [("mixT", "a", h, g)])
                        pending.append([2, fin1, fin2])
                        oi_box[0] += 1

                pending = []

                def run_pending(force=False):
                    for p_ in list(pending):
                        p_[0] -= 1
                        if p_[0] <= 0 or force:
                            p_[1]()
                            p_[2]()
                            pending.remove(p_)

                emit_S(0)
                emit_S(1)
                yield 1.4
                for idx in range(len(items)):
                    emit_PV(idx)
                    if idx + 2 < len(items):
                        emit_S(idx + 2)
                    run_pending()
                    yield 0.75
                run_pending(force=True)

            def timed_interleave(*gens):
                gens = [[0.0, g_] for g_ in gens]
                while gens:
                    gens.sort(key=lambda x: x[0])
                    cur = gens[0]
                    try:
                        cur[0] += next(cur[1])
                    except StopIteration:
                        gens.remove(cur)

            gorder = [3, 2, 1, 0]
            timed_interleave(gen_IB(gorder[0]))
            for gi, g in enumerate(gorder):
                if gi + 1 < 4:
                    timed_interleave(gen_A(g), gen_IB(gorder[gi + 1]))
                else:
                    timed_interleave(gen_A(g))
            dump("attT", mixT[:, 0:4, :], [128, 4, S], [("mixT", "a", h, g) for h in range(8) for g in range(4)])
            P.barrier()
            A.free(*p2_tmp)
            A.free(qT, kT, iqT, ikT, Vext, iws)
            if stop_after == 2:
                A.free(mixT)
                continue

            def ln_gen(r, st, junk_, gam, bet, hn, tag, add_eng="pool"):
                ACT(P, junk_[:], r[:], AF.Identity, [(tag, "r")], [(tag, "junk"), (tag, "st")], accum=st[:, 0:1])
                yield
                TS(P, "dve", st[:, 1:2], st[:, 0:1], -1.0 / D, None, ALU.mult, None, [(tag, "st")], [(tag, "st")])
                yield
                ACT(P, junk_[:], r[:], AF.Square, [(tag, "r"), (tag, "st")], [(tag, "junk"), (tag, "st")],
                    bias=st[:, 1:2], accum=st[:, 2:3])
                yield
                ACT(P, st[:, 3:4], st[:, 2:3], AF.Ln, [(tag, "st")], [(tag, "st")], bias=LN_EPS, scale=1.0 / D)
                yield
                ACT(P, st[:, 3:4], st[:, 3:4], AF.Exp, [(tag, "st")], [(tag, "st")], scale=-0.5)
                yield
                TT(P, "dve", st[:, 4:5], st[:, 1:2], st[:, 3:4], ALU.mult, [(tag, "st")], [(tag, "st")])
                yield
                ACT(P, hn[:], r[:], AF.Identity, [(tag, "r"), (tag, "st")], [(tag, "hn")], bias=st[:, 4:5], scale=st[:, 3:4])
                yield
                TT(P, "dve", hn[:], hn[:], gam[:], ALU.mult, [(tag, "hn"), "lng"], [(tag, "hn")])
                yield
                TT(P, add_eng, hn[:], hn[:], bet[:], ALU.add, [(tag, "hn"), "lnb"], [(tag, "hn")])
                yield

            def interleave(*gens):
                gens = list(gens)
                while gens:
                    for g_ in list(gens):
                        try:
                            next(g_)
                        except StopIteration:
                            gens.remove(g_)

            def layer_norm(r, st, junk_, gam, bet, hn, tag):
                interleave(ln_gen(r, st, junk_, gam, bet, hn, tag, add_eng="dve"))

            hT = A.alloc("hT", [128, 8, S], BF16)
            gates = A.alloc("gates", [128, NT, 16], F32)
            Wgu = [A.alloc("Wgu%d" % i, [128, 8, 512], BF16) for i in range(2)]
            wout = A.alloc("wout", [128, 8, D], BF16)
            lng = A.alloc("lng", [128, D], F32)
            lnb = A.alloc("lnb", [128, D], F32)
            xres = [A.alloc("xres%d" % i, [128, D], F32) for i in range(6)]
            rr = [A.alloc("rr%d" % i, [128, D], F32) for i in range(6)]
            hn = [A.alloc("hn%d" % i, [128, D], F32) for i in range(6)]
            lnj = [A.alloc("lnj%d" % i, [128, D], BF16) for i in range(6)]
            stt_ = [A.alloc("st%d" % i, [128, 8], F32) for i in range(6)]
            hT32 = []
            hlo = [A.alloc("hlo%d" % i, [128, 8, 128], BF16) for i in range(2)]
            wr_hl = A.alloc("wr_hl", [128, 2, 8, 20], BF16)
            wr_res = A.alloc("wr_res", [128, 8, 20], F32)
            wr = A.alloc("wr", [128, 8, 20], F32)
            nwc = A.alloc("nwc", [128, 4], F32)
            brp = A.alloc("brp", [128, 20], F32)
            lg = A.alloc("lg", [128, NT, 20], F32)
            rs = [A.alloc("rs%d" % i, [128, NT, 16], F32) for i in range(4)]
            rv = [A.alloc("rv%d" % i, [128, NT], F32) for i in range(8)]
            p4_tmp = [wout, lng, lnb, wr, nwc, brp, lg, wr_hl, wr_res] + hlo + xres + rr + hn + lnj + stt_ + rs + rv
            for c in range(8):
                DMA(P, "pool", wout[:, c, :], wout_d[c * 128:(c + 1) * 128, :], [], [("wout", c)])
            for e0 in range(2):
                DMA(P, "pool", Wgu[e0][:, :, 0:256], wg_d[e0].rearrange("(c p) f -> p c f", p=128), [], [("Wgu", e0)])
                DMA(P, "pool", Wgu[e0][:, :, 256:512], wu_d[e0].rearrange("(c p) f -> p c f", p=128), [], [("Wgu", e0)])
            DMA(P, "sp", lng[:], ln1g_d, [], ["lng"])
            DMA(P, "sp", lnb[:], ln1b_d, [], ["lnb"])
            DMA(P, "sp", nwc[:], nwc_d, [], ["nwc"])
            for k in range(4):
                TS(P, "dve", wout[:, 4 + k, :], wout[:, 4 + k, :], nwc[:, k:k + 1], None, ALU.mult, None,
                   [("wout", 4 + k), "nwc"], [("wout", 4 + k)])
            DMA(P, "sp", wr[:], wr_d.rearrange("(c p) f -> p c f", p=128), [], ["wr"])
            DMA(P, "sp", brp[:], br_d, [], ["brp"])
            CP(P, "dve", wr_hl[:, 0, :, :], wr[:], ["wr"], ["wr_h"])
            TT(P, "dve", wr_res[:], wr[:], wr_hl[:, 0, :, :], ALU.subtract, ["wr", "wr_h"], ["wr_res"])
            CP(P, "dve", wr_hl[:, 1, :, :], wr_res[:], ["wr_res"], ["wr_hl"])
            mb = Rot([0, 1, 2, 3])

            def mm_part(t):
                i = t % 6
                tok = slice(t * 128, (t + 1) * 128)
                tag = ("ln1", i)
                DMA(P, "sp", xres[i][:], x_d[seq, tok, :], [], [("xres", i)])
                for half in range(2):
                    b = mb.next()
                    hs = slice(half * 512, half * 512 + 512)
                    for c in range(8):
                        MM(P, psf(b), mixT[:, c, tok], wout[:, c, hs], c == 0, c == 7, [("wout", c)], [PSR(b)], inc=(c == 7))
                    STT(P, "dve", rr[i][:, hs], xres[i][:, hs], ALPHA, psf(b), ALU.mult, ALU.add,
                        [("xres", i), PSR(b)], [(tag, "r")])

            def post_gen(t):
                i = t % 6
                tok = slice(t * 128, (t + 1) * 128)
                tag = ("ln1", i)
                yield from ln_gen(rr[i], stt_[i], lnj[i], lng, lnb, hn[i], tag, add_eng="dve")
                DMA(P, "pool", hscr_d[seq, tok, :], hn[i][:], [(tag, "hn")], [])
                yield

            def tr_part(t):
                i = t % 6
                tok = slice(t * 128, (t + 1) * 128)
                tag = ("ln1", i)
                hres_ = [("hT32a", t % 2), ("hT32b", t % 2)]
                for c in range(8):
                    b = 4 + c // 4
                    TR(P, ps[b][:, (c % 4) * 128:(c % 4 + 1) * 128], hn[i][:, c * 128:(c + 1) * 128], ident_f[:],
                       [(tag, "hn")], [PSR(b)], inc=(c % 4 == 3))
                lo_ = hlo[t % 2]
                CP(P, "act", hT[:, 0:4, tok], ps[4][:].rearrange("p (c t) -> p c t", c=4), [PSR(4)], [("hT", t, 0)])
                CP(P, "dve", hT[:, 4:8, tok], ps[5][:].rearrange("p (c t) -> p c t", c=4), [PSR(5)], [("hT", t, 1)])
                TT(P, "dve", lo_[:, 0:4, :], ps[4][:].rearrange("p (c t) -> p c t", c=4), hT[:, 0:4, tok], ALU.subtract,
                   [PSR(4), ("hT", t, 0)], [hres_[0]])
                TT(P, "dve", lo_[:, 4:8, :], ps[5][:].rearrange("p (c t) -> p c t", c=4), hT[:, 4:8, tok], ALU.subtract,
                   [PSR(5), ("hT", t, 1)], [hres_[1]])
                rb = 6 + t % 2
                rres = hres_ + [("hT", t, 0), ("hT", t, 1), "wr_hl"]
                for c in range(8):
                    MM(P, ps[rb][:, 0:20], hT[:, c, tok], wr_hl[:, 0, c, :], c == 0, False, rres, [PSR(rb)], inc=False)
                    MM(P, ps[rb][:, 0:20], hT[:, c, tok], wr_hl[:, 1, c, :], False, False, rres, [PSR(rb)], inc=False)
                    MM(P, ps[rb][:, 0:20], lo_[:, c, :], wr_hl[:, 0, c, :], False, c == 7, rres, [PSR(rb)], inc=(c == 7))
                TT(P, "dve", lg[:, t, :], ps[rb][:, 0:20], brp[:], ALU.add, [PSR(rb), "brp"], [("lg", t)])

            mm_part(0)
            mm_part(1)
            mm_part(2)
            mm_part(3)
            interleave(post_gen(0), post_gen(1))
            for tp in range(0, NT, 2):
                if tp + 2 < NT:
                    interleave(post_gen(tp + 2), post_gen(tp + 3))
                if tp + 4 < NT:
                    mm_part(tp + 4)
                    mm_part(tp + 5)
                tr_part(tp)
                tr_part(tp + 1)
            dump("hT", hT[:], [128, 8, S], [("hT", t, k_) for t in range(NT) for k_ in range(2)])
            lgr = [("lg", t) for t in range(NT)]
            gl = lg[:, :, 0:4]
            gmax, gsum, gprob, m1, m2, dd, w1, w2 = [rv[i] for i in range(8)]
            ge, ohg, pen = rs[0][:, :, 0:4], rs[1][:, :, 0:4], rs[2][:, :, 0:4]

            def red(out, in_, op, r, w):
                P.op("dve", lambda e: e.tensor_reduce(out, in_, axis=AX.X, op=op), r, w)

            def bc(v, n):
                return v[:].unsqueeze(2).to_broadcast([128, NT, n])
            red(gmax[:], gl, ALU.max, lgr, ["gmax"])
            TT(P, "dve", ge, gl, bc(gmax, 4), ALU.subtract, lgr + ["gmax"], ["ge"])
            ACT(P, ge, ge, AF.Exp, ["ge"], ["ge"])
            red(gsum[:], ge, ALU.add, ["ge"], ["gsum"])
            P.op("dve", lambda e: e.reciprocal(gprob[:], gsum[:]), ["gsum"], ["gprob"])
            TT(P, "dve", ohg, gl, bc(gmax, 4), ALU.is_ge, lgr + ["gmax"], ["ohg"])
            TS(P, "dve", pen, ohg, -1.0, 1.0e4, ALU.add, ALU.mult, ["ohg"], ["pen"])
            mel, oh1, mel2, oh2 = rs[3], rs[0], rs[1], rs[2]
            for gq in range(4):
                TT(P, "dve", mel[:, :, gq * 4:(gq + 1) * 4], lg[:, :, 4 + gq * 4:8 + gq * 4],
                   pen[:, :, gq:gq + 1].to_broadcast([128, NT, 4]), ALU.add, lgr + ["pen"], ["mel"])
            red(m1[:], mel[:], ALU.max, ["mel"], ["m1"])
            TT(P, "dve", oh1[:], mel[:], bc(m1, 16), ALU.is_ge, ["mel", "m1", "ge"], ["oh1"])
            STT(P, "dve", mel2[:], oh1[:], -1.0e4, mel[:], ALU.mult, ALU.add, ["oh1", "mel", "ohg"], ["mel2"])
            red(m2[:], mel2[:], ALU.max, ["mel2"], ["m2"])
            TT(P, "dve", oh2[:], mel2[:], bc(m2, 16), ALU.is_ge, ["mel2", "m2", "pen"], ["oh2"])
            TT(P, "dve", dd[:], m2[:], m1[:], ALU.subtract, ["m1", "m2"], ["dd"])
            ACT(P, dd[:], dd[:], AF.Exp, ["dd"], ["dd"])
            TS(P, "dve", w1[:], dd[:], 1.0, None, ALU.add, None, ["dd"], ["w1"])
            P.op("dve", lambda e: e.reciprocal(w1[:], w1[:]), ["w1"], ["w1"])
            TT(P, "dve", w2[:], dd[:], w1[:], ALU.mult, ["dd", "w1"], ["w2"])
            TT(P, "dve", w1[:], w1[:], gprob[:], ALU.mult, ["w1", "gprob", "w2"], ["w1"])
            TT(P, "dve", w2[:], w2[:], gprob[:], ALU.mult, ["w2", "gprob"], ["w2"])
            TT(P, "dve", oh1[:], oh1[:], bc(w1, 16), ALU.mult, ["oh1", "w1", "mel2"], ["oh1"])
            TT(P, "dve", oh2[:], oh2[:], bc(w2, 16), ALU.mult, ["oh2", "w2"], ["oh2"])
            TT(P, "dve", gates[:], oh1[:], oh2[:], ALU.add, ["oh1", "oh2"], ["gates"])
            dump("gates", gates[:], [128, NT, 16], ["gates"])
            P.barrier()
            A.free(*p4_tmp)
            A.free(mixT)
            if stop_after == 4:
                A.free(hT, gates)
                continue

            selc = A.alloc("selc", [16, 16 * 128], F32)
            gT = A.alloc("gT", [16, 1024], F32)
            wdn = A.alloc("wdn", [128, 16, D], BF16)
            hid = A.alloc("hid", [128, 16, 1024], BF16)
            yacc = A.alloc("yacc", [128, 8, D], F32)
            sgt = [A.alloc("sgt%d" % i, [128, 512], BF16) for i in range(2)]
            ttm = [A.alloc("ttm%d" % i, [128, 512], BF16) for i in range(2)]
            lng = A.alloc("lng2", [128, D], F32)
            lnb = A.alloc("lnb2", [128, D], F32)
            hres = [A.alloc("hres%d" % i, [128, D], F32) for i in range(2)]
            r2 = [A.alloc("r2_%d" % i, [128, D], F32) for i in range(2)]
            on = [A.alloc("on%d" % i, [128, D], F32) for i in range(2)]
            lnj_one = A.alloc("lnj2", [128, D], BF16)
            lnj = [lnj_one, lnj_one]
            stt_ = [A.alloc("st2_%d" % i, [128, 8], F32) for i in range(2)]
            p5_tmp = [selc, gT, wdn, hid, yacc, lng, lnb, lnj_one] + Wgu + sgt + ttm + hres + r2 + on + stt_
            DMA(P, "sp", selc[:], sel_d, [], ["selc"])
            DMA(P, "sp", lng[:], ln2g_d, [], ["lng"])
            DMA(P, "sp", lnb[:], ln2b_d, [], ["lnb"])
            gub = Rot([2, 3, 4, 5])
            geb = Rot([0, 1])
            dnb = Rot([6, 7])
            wi = 0
            si = 0
            li = 0
            for hf in range(2):
                if hf == 1 and seq + 1 < nseq:
                    wtm_pref[0] = A.alloc("wtm", [128, 8, TMW], BF16)
                    for c in range(8):
                        DMA(P, "pool", wtm_pref[0][:, c, :], wtm_d[c * 128:(c + 1) * 128, :], [], [("wtm", c)])
                for j4 in range(2):
                    for k in range(4):
                        t = hf * 8 + j4 * 4 + k
                        TR(P, ps[j4][0:16, k * 128:(k + 1) * 128], gates[:, t, :], ident_f[:], ["gates"], [PSR(j4)], inc=(k == 3))
                    CP(P, "dve", gT[:, j4 * 512:(j4 + 1) * 512], ps[j4][0:16, :], [PSR(j4)], [("gT", j4)])
                for eh in range(2):
                    for el in range(8):
                        e = eh * 8 + el
                        W = Wgu[wi % 2]
                        wres = ("Wgu", wi % 2)
                        if not (hf == 0 and eh == 0 and el < 2):
                            DMA(P, "pool", W[:, :, 0:256], wg_d[e].rearrange("(c p) f -> p c f", p=128), [], [wres])
                            DMA(P, "pool", W[:, :, 256:512], wu_d[e].rearrange("(c p) f -> p c f", p=128), [], [wres])
                        DMA(P, "pool", wdn[:, 2 * el:2 * el + 2, :], wd_d[e].rearrange("(j p) d -> p j d", p=128), [],
                            [("wdn", el)])
                        wi += 1
                        for sub in range(2):
                            ts_ = slice(hf * 1024 + sub * 512, hf * 1024 + sub * 512 + 512)
                            loc = slice(sub * 512, sub * 512 + 512)
                            gb_ = geb.next()
                            MM(P, psf(gb_), selc[:, e * 128:(e + 1) * 128], gT[:, loc], True, True,
                               ["selc", ("gT", sub)], [PSR(gb_)])
                            for j in range(2):
                                bg, bu = gub.next(), gub.next()
                                for c in range(8):
                                    MM(P, psf(bg), W[:, c, j * 128:(j + 1) * 128], hT[:, c, ts_], c == 0, c == 7,
                                       [wres], [PSR(bg)], inc=(c == 7))
                                for c in range(8):
                                    MM(P, psf(bu), W[:, c, 256 + j * 128:256 + (j + 1) * 128], hT[:, c, ts_], c == 0, c == 7,
                                       [wres], [PSR(bu)], inc=(c == 7))
                                sg_, tm_ = sgt[si % 2], ttm[si % 2]
                                ACT(P, sg_[:], psf(bg), AF.Silu, [PSR(bg)], [("sgt", si % 2)])
                                TT(P, "dve", tm_[:], sg_[:], psf(bu), ALU.mult, [("sgt", si % 2), PSR(bu)], [("ttm", si % 2)])
                                TT(P, "dve", hid[:, el * 2 + j, loc], tm_[:], psf(gb_), ALU.mult,
                                   [("ttm", si % 2), PSR(gb_)], [("hid", el * 2 + j, sub)])
                                si += 1
                    hres_all = [("hid", k, sub) for k in range(16) for sub in range(2)] + [("wdn", el) for el in range(8)]
                    for lt in range(8):
                        t = hf * 8 + lt
                        tok = slice(t * 128, (t + 1) * 128)
                        i = li % 2
                        tag = ("ln2", i)
                        if eh == 1 and lt == 0:
                            for l2 in range(2):
                                t2 = hf * 8 + l2
                                DMA(P, "sp", hres[(li + l2) % 2][:], hscr_d[seq, t2 * 128:(t2 + 1) * 128, :], [],
                                    [("hres", (li + l2) % 2)])
                        for dh in range(2):
                            b = dnb.next()
                            hs = slice(dh * 512, dh * 512 + 512)
                            for k in range(16):
                                MM(P, psf(b), hid[:, k, lt * 128:(lt + 1) * 128], wdn[:, k, hs], k == 0, k == 15,
                                   hres_all, [PSR(b)], inc=(k == 15))
                            if eh == 0:
                                CP(P, "act", yacc[:, lt, hs], psf(b), [PSR(b)], [("yacc", lt)])
                            else:
                                TT(P, "dve", r2[i][:, hs], yacc[:, lt, hs], psf(b), ALU.add, [("yacc", lt), PSR(b)], [(tag, "r0")])
                                STT(P, "dve", r2[i][:, hs], hres[i][:, hs], ALPHA, r2[i][:, hs], ALU.mult, ALU.add,
                                    [("hres", i), (tag, "r0")], [(tag, "r")])
                        if eh == 1:
                            if lt + 2 < 8:
                                t2 = hf * 8 + lt + 2
                                DMA(P, "sp", hres[i][:], hscr_d[seq, t2 * 128:(t2 + 1) * 128, :], [], [("hres", i)])
                            layer_norm(r2[i], stt_[i], lnj[i], lng, lnb, on[i], tag)
                            DMA(P, "sp", out_d[seq, tok, :], on[i][:], [(tag, "hn")], [])
                            li += 1
            P.barrier()
            A.free(*p5_tmp)
            A.free(hT, gates)
    P.emit()
    return nc, dumps


def _in_map(inp, core, consts, wfm, wtm):
    m = dict(consts)
    m["x"] = np.ascontiguousarray(inp["x"][SEQ_PER_CORE * core:SEQ_PER_CORE * (core + 1)], dtype=np.float32)
    m["wfm"] = wfm
    m["wtm"] = wtm
    cw = inp["conv_w"][0]
    m["convw"] = np.ascontiguousarray(cw.reshape(4, 6, 128).transpose(2, 1, 0).reshape(128, 24))
    m["convb"] = np.ascontiguousarray(inp["conv_b"][0].reshape(6, 128).T)
    m["dtb"] = _rep(inp["dt_bias"][0])
    m["alog"] = _rep(inp["a_log"][0])
    m["dskip"] = _rep(np.repeat(inp["d_skip"][0], 64))
    m["nw"] = _rep(inp["ssd_norm_w"][0])
    m["nwc"] = np.ascontiguousarray(inp["ssd_norm_w"][0].reshape(4, 128).T)
    m["wout"] = np.ascontiguousarray(inp["w_out"][0])
    m["ln1g"] = _rep(inp["ln1_g"][0])
    m["ln1b"] = _rep(inp["ln1_b"][0])
    m["ln2g"] = _rep(inp["ln2_g"][0])
    m["ln2b"] = _rep(inp["ln2_b"][0])
    m["wr"] = np.ascontiguousarray(np.concatenate([inp["w_route_group"][0], inp["w_route_expert"][0]], 1))
    m["br"] = _rep(np.concatenate([inp["b_route_group"][0], inp["b_route_expert"][0]]))
    m["wg"] = np.ascontiguousarray(inp["w_gate"][0])
    m["wu"] = np.ascontiguousarray(inp["w_up"][0])
    m["wd"] = np.ascontiguousarray(inp["w_down"][0])
    return m


def kernel(**inputs):
    inp = {k: np.asarray(v, dtype=np.float32) for k, v in inputs.items()}
    wfm, wtm = _layout_w_in(inp["w_in"])
    consts = _consts()
    nc, _ = build()
    in_maps = [_in_map(inp, c, consts, wfm, wtm) for c in range(NCORES)]
    res = run_bass_kernel_spmd(nc, in_maps, core_ids=list(range(NCORES)))
    out = np.concatenate([np.asarray(res.results[c]["out"], dtype=np.float32) for c in range(NCORES)], axis=0)
    return out
```

```python
import numpy as np
import concourse.bass as bass
import concourse.mybir as mybir
from concourse.bass_utils import run_bass_kernel_spmd

F32 = mybir.dt.float32
BF16 = mybir.dt.bfloat16
ALU = mybir.AluOpType
AF = mybir.ActivationFunctionType
AX = mybir.AxisListType

NCORES = 8
S = 2048
D = 1024
NT = S // 128
SEQ_PER_CORE = 2
ALPHA = 2.0 ** 0.25
LN_EPS = 1e-5
IDX_SCALE = (4 ** -0.5) * (64 ** -0.5)
NF = 22
TMW = 588
NBIS = 14
NEG = -1.0e30
MASKB = -262144.0


class Prog:
    CE = ("pe", "act", "dve", "pool")

    def __init__(self, nc, kdma=12):
        self.nc = nc
        self.ops = {e: [] for e in ("pe", "act", "dve", "pool", "sp")}
        self.cnt = {e: 0 for e in self.CE}
        self.last_w = {}
        self.readers = {}
        self.seen = {e: {} for e in self.ops}
        self.kdma = kdma
        self.ring = {"sp": [0] * kdma, "pool": [0] * kdma}
        self.ring_next = {"sp": 0, "pool": 0}
        self.nops = 0
        self.floor = {e: {} for e in self.ops}
        self.ps_last = {}

    def barrier(self):
        cur = {e: self.cnt[e] for e in self.CE if self.cnt[e] > 0}
        for q in ("sp", "pool"):
            for k in range(self.kdma):
                if self.ring[q][k] > 0:
                    cur["d%s%d" % (q, k)] = 16 * self.ring[q][k]
        for e in self.floor:
            self.floor[e] = dict(cur)

    def _deps(self, reads, writes):
        deps = {}

        def add(tok):
            if tok is None:
                return
            k, v = tok
            if deps.get(k, 0) < v:
                deps[k] = v
        for r in reads:
            add(self.last_w.get(r))
        for w in writes:
            add(self.last_w.get(w))
            for k, v in self.readers.get(w, {}).items():
                add((k, v))
        return deps

    def _commit(self, tok, reads, writes):
        for r in reads:
            d = self.readers.setdefault(r, {})
            if d.get(tok[0], 0) < tok[1]:
                d[tok[0]] = tok[1]
        for w in writes:
            self.last_w[w] = tok
            self.readers[w] = {}

    def _waits(self, eng, deps):
        waits = []
        fl = self.floor[eng]
        if fl:
            for k, v in fl.items():
                if deps.get(k, 0) < v:
                    deps[k] = v
            self.floor[eng] = {}
        for k, v in deps.items():
            if k == "pe" and eng == "pe":
                continue
            if self.seen[eng].get(k, 0) >= v:
                continue
            self.seen[eng][k] = v
            waits.append((k, v))
        return waits

    def op(self, eng, fn, reads=(), writes=(), inc=True):
        assert eng in self.CE
        if eng != "pe":
            assert inc
        deps = self._deps(reads, writes)
        idx = self.cnt[eng] + 1
        if inc:
            self.cnt[eng] = idx
        tok = (eng, idx)
        for r in reads:
            if isinstance(r, tuple) and r[0] == "ps":
                prev = self.ps_last.get(r[1])
                if prev is not None and prev[0] != eng and deps.get(prev[0], 0) < prev[1]:
                    deps[prev[0]] = prev[1]
                self.ps_last[r[1]] = tok
        waits = self._waits(eng, deps)
        self.ops[eng].append((waits, fn, (eng, 1) if inc else None))
        self._commit(tok, reads, writes)
        self.nops += 1
        return tok

    def dma(self, q, fn, reads=(), writes=()):
        deps = self._deps(reads, writes)
        k = self.ring_next[q] % self.kdma
        self.ring_next[q] += 1
        key = "d%s%d" % (q, k)
        if self.ring[q][k] > 0:
            v = 16 * self.ring[q][k]
            if deps.get(key, 0) < v:
                deps[key] = v
        waits = self._waits(q, deps)
        self.ring[q][k] += 1
        tok = (key, 16 * self.ring[q][k])
        self.ops[q].append((waits, fn, (key, 16)))
        self._commit(tok, reads, writes)
        self.nops += 1
        return tok

    def emit(self):
        nc = self.nc
        names = list(self.CE) + ["d%s%d" % (q, k) for q in ("sp", "pool") for k in range(self.kdma)]
        fin = []
        for q in ("sp", "pool"):
            for k in range(self.kdma):
                if self.ring[q][k] > 0:
                    fin.append(("d%s%d" % (q, k), 16 * self.ring[q][k]))
        for e in self.CE:
            if self.cnt[e] > 0:
                fin.append((e, self.cnt[e]))
        ops = self.ops
        import contextlib
        with contextlib.ExitStack() as st:
            sems = {n: st.enter_context(nc.semaphore("s_" + n)) for n in names}
            block = st.enter_context(nc.Block())

            def replay(eng_name):
                def run(e):
                    for waits, fn, inc in ops[eng_name]:
                        for k, v in waits:
                            e.wait_ge(sems[k], v)
                        ins = fn(e)
                        if inc is not None:
                            ins.then_inc(sems[inc[0]], inc[1])
                    if eng_name == "sp":
                        for k, v in fin:
                            e.wait_ge(sems[k], v)
                return run
            block.sync(replay("sp"))
            block.tensor(replay("pe"))
            block.scalar(replay("act"))
            block.vector(replay("dve"))
            block.gpsimd(replay("pool"))


def _rot(cols):
    return np.concatenate([cols[32:], cols[:32]])


def _layout_w_in(w_in):
    w = w_in[0]
    oq, ok, ov, oiq, oik, oiw, oz, oxbc, odt = 0, 512, 576, 640, 896, 960, 964, 1476, 2244
    tiles = []
    qh = [np.arange(oq + 64 * h, oq + 64 * (h + 1)) for h in range(8)]
    for p in range(4):
        tiles.append(np.concatenate([qh[2 * p], qh[2 * p + 1]]))
    for p in range(4):
        tiles.append(np.concatenate([_rot(qh[2 * p]), _rot(qh[2 * p + 1])]))
    kc = np.arange(ok, ok + 64)
    tiles.append(np.concatenate([kc, kc]))
    tiles.append(np.concatenate([_rot(kc), _rot(kc)]))
    ih = [np.arange(oiq + 64 * h, oiq + 64 * (h + 1)) for h in range(4)]
    for p in range(2):
        tiles.append(np.concatenate([ih[2 * p], ih[2 * p + 1]]))
    for p in range(2):
        tiles.append(np.concatenate([_rot(ih[2 * p]), _rot(ih[2 * p + 1])]))
    ikc = np.arange(oik, oik + 64)
    tiles.append(np.concatenate([ikc, ikc]))
    tiles.append(np.concatenate([_rot(ikc), _rot(ikc)]))
    for t in range(6):
        tiles.append(np.arange(oxbc + 128 * t, oxbc + 128 * (t + 1)))
    fm_cols = np.concatenate(tiles)
    assert fm_cols.shape[0] == NF * 128
    tm_cols = np.concatenate([np.arange(oz, oz + 512), np.arange(ov, ov + 64), np.arange(oiw, oiw + 4),
                              np.arange(odt, odt + 8)])
    assert tm_cols.shape[0] == TMW
    return np.ascontiguousarray(w[:, fm_cols]), np.ascontiguousarray(w[:, tm_cols])


def _consts():
    c = {}
    c["ident"] = np.eye(128, dtype=np.float32)
    s_ = np.arange(128)
    c["tri"] = (s_[:, None] <= s_[None, :]).astype(np.float32)
    c["cneg"] = np.where(s_[None, :] <= s_[:, None], 0.0, NEG).astype(np.float32)
    inv = 10000.0 ** (-np.arange(0, 64, 2, dtype=np.float32) / 64.0)
    ang = np.arange(S, dtype=np.float32)[:, None] * inv[None, :]
    cos, sin = np.cos(ang).astype(np.float32), np.sin(ang).astype(np.float32)
    cosF = np.concatenate([cos, cos], 1).T
    sinS = np.concatenate([-sin, sin], 1).T
    c["cosF"] = np.ascontiguousarray(np.concatenate([cosF, cosF], 0))
    c["sinS"] = np.ascontiguousarray(np.concatenate([sinS, sinS], 0))
    sel = np.zeros((16, 16, 128), np.float32)
    for e in range(16):
        sel[e, e, :] = 1.0
    c["sel"] = sel.reshape(16, 16 * 128)
    c["halfpow"] = np.broadcast_to((0.5 ** np.arange(1, NBIS + 3, dtype=np.float32))[None, :], (128, NBIS + 2)).copy()
    return c


def _rep(v, n=128):
    return np.ascontiguousarray(np.broadcast_to(np.asarray(v, np.float32).reshape(1, -1), (n, np.asarray(v).size)))


def MM(P, out, lhsT, rhs, start, stop, r, w, inc=True):
    return P.op("pe", lambda e: e.matmul(out, lhsT=lhsT, rhs=rhs, start=start, stop=stop), r, w, inc)


def TR(P, out, in_, ident, r, w, inc=True):
    return P.op("pe", lambda e: e.transpose(out, in_, ident), r, w, inc)


def ACT(P, out, in_, func, r, w, bias=0.0, scale=1.0, accum=None):
    if accum is None:
        return P.op("act", lambda e: e.activation(out=out, in_=in_, func=func, bias=bias, scale=scale), r, w)
    return P.op("act", lambda e: e.activation(out=out, in_=in_, func=func, bias=bias, scale=scale, accum_out=accum), r, w)


def TT(P, eng, out, a, b, op, r, w):
    return P.op(eng, lambda e: e.tensor_tensor(out, a, b, op), r, w)


def TS(P, eng, out, a, s1, s2, op0, op1, r, w, accum=None):
    if op1 is None:
        return P.op(eng, lambda e: e.tensor_scalar(out, a, s1, None, op0=op0), r, w)
    if accum is None:
        return P.op(eng, lambda e: e.tensor_scalar(out, a, s1, s2, op0=op0, op1=op1), r, w)
    return P.op(eng, lambda e: e.tensor_scalar(out, a, s1, s2, op0=op0, op1=op1, accum_out=accum), r, w)


def STT(P, eng, out, a, scalar, b, op0, op1, r, w):
    return P.op(eng, lambda e: e.scalar_tensor_tensor(out, a, scalar, b, op0=op0, op1=op1), r, w)


def CP(P, eng, out, in_, r, w):
    if eng == "act":
        return P.op("act", lambda e: e.copy(out, in_), r, w)
    return P.op(eng, lambda e: e.tensor_copy(out, in_), r, w)


def MSET(P, eng, ap, val, r, w):
    return P.op(eng, lambda e: e.memset(ap, val), r, w)


def DMA(P, q, out, in_, r, w):
    return P.dma(q, lambda e: e.dma_start(out=out, in_=in_), r, w)


class Rot:
    def __init__(self, items):
        self.items = list(items)
        self.i = 0

    def next(self):
        v = self.items[self.i % len(self.items)]
        self.i += 1
        return v


class Arena:
    def __init__(self, nc, lo=16512, hi=229344):
        self.nc = nc
        self.free_list = [(lo, hi)]
        self.live = {}
        self.uid = 0
        self.cache = {}

    def alloc(self, name, shape, dt):
        n = 1
        for d in shape[1:]:
            n *= d
        size = n * (4 if dt == F32 else 2)
        size = (size + 31) // 32 * 32
        for i, (a, b) in enumerate(self.free_list):
            if b - a >= size:
                self.free_list[i] = (a + size, b)
                if a + size == b:
                    self.free_list.pop(i)
                key = (name, tuple(shape), str(dt), a)
                h = self.cache.get(key)
                if h is None:
                    self.uid += 1
                    h = self.nc.alloc_sbuf_tensor_at("sb%d_%s" % (self.uid, name), list(shape), dt, offset=a)
                    self.cache[key] = h
                self.live[id(h)] = (a, size, h)
                return h
        raise RuntimeError("arena out of SBUF for %s (%d bytes); free=%s" % (name, size, self.free_list))

    def free(self, *hs):
        for h in hs:
            a, size, _ = self.live.pop(id(h))
            self.free_list.append((a, a + size))
        self.free_list.sort()
        merged = []
        for a, b in self.free_list:
            if merged and merged[-1][1] == a:
                merged[-1] = (merged[-1][0], b)
            else:
                merged.append((a, b))
        self.free_list = merged


def build(dbg=(), nseq=SEQ_PER_CORE, stop_after=None):
    nc = bass.Bass("TRN2", target_bir_lowering=False)
    P = Prog(nc)
    A = Arena(nc)
    dbg = set(dbg)
    dumps = {}

    def din(name, shape):
        return nc.dram_tensor(name, list(shape), F32, kind="ExternalInput").ap()

    x_d = din("x", [SEQ_PER_CORE, S, D])
    wfm_d = din("wfm", [D, NF * 128])
    wtm_d = din("wtm", [D, TMW])
    ident_d = din("ident", [128, 128])
    tri_d = din("tri", [128, 128])
    cneg_d = din("cneg", [128, 128])
    cosF_d = din("cosF", [128, S])
    sinS_d = din("sinS", [128, S])
    sel_d = din("sel", [16, 16 * 128])
    halfpow_d = din("halfpow", [128, NBIS + 2])
    convw_d = din("convw", [128, 24])
    convb_d = din("convb", [128, 6])
    dtb_d = din("dtb", [128, 8])
    alog_d = din("alog", [128, 8])
    dskip_d = din("dskip", [128, 512])
    nw_d = din("nw", [128, 512])
    nwc_d = din("nwc", [128, 4])
    wout_d = din("wout", [D, D])
    ln1g_d = din("ln1g", [128, D])
    ln1b_d = din("ln1b", [128, D])
    ln2g_d = din("ln2g", [128, D])
    ln2b_d = din("ln2b", [128, D])
    wr_d = din("wr", [D, 20])
    br_d = din("br", [128, 20])
    wg_d = din("wg", [16, D, 256])
    wu_d = din("wu", [16, D, 256])
    wd_d = din("wd", [16, 256, D])
    out_d = nc.dram_tensor("out", [SEQ_PER_CORE, S, D], F32, kind="ExternalOutput").ap()
    hscr_d = nc.dram_tensor("hscr", [SEQ_PER_CORE, S, D], F32, kind="Internal").ap()

    import contextlib
    with contextlib.ExitStack() as es:
        ps = [es.enter_context(nc.psum_tensor("ps%d" % i, [128, 512], F32)) for i in range(8)]

        def psf(b):
            return ps[b][:]

        def psb(b):
            return ps[b][:].bitcast(BF16)

        def PSR(b):
            return ("ps", b)

        def dump(name, ap, shape, reads, dt=None):
            if name not in dbg:
                return
            t = nc.dram_tensor("dbg_" + name, list(shape), dt or ap.dtype, kind="ExternalOutput").ap()
            dumps[name] = t
            DMA(P, "sp", t, ap, reads, [])

        ident_f = A.alloc("ident_f", [128, 128], F32)
        ident_b = A.alloc("ident_b", [128, 128], BF16)
        tri_f = A.alloc("tri_f", [128, 128], F32)
        tri_b = A.alloc("tri_b", [128, 128], BF16)
        cneg = A.alloc("cneg", [128, 128], F32)
        ones_f = A.alloc("ones_f", [128, 128], F32)
        DMA(P, "sp", ident_f[:], ident_d, [], ["ident_f"])
        DMA(P, "pool", ident_b[:], ident_d, [], ["ident_b"])
        DMA(P, "sp", tri_f[:], tri_d, [], ["tri_f"])
        DMA(P, "pool", tri_b[:], tri_d, [], ["tri_b"])
        DMA(P, "sp", cneg[:], cneg_d, [], ["cneg"])
        MSET(P, "dve", ones_f[:], 1.0, [], ["ones_f"])

        wtm_pref = [None]
        for seq in range(nseq):
            qT = A.alloc("qz", [128, 8, S], BF16)
            kT = A.alloc("kT", [128, S], BF16)
            iqT = A.alloc("iqz", [128, 4, S], BF16)
            ikT = A.alloc("ikT", [128, S], BF16)
            Vext = A.alloc("Vext", [128, NT, 192], BF16)
            iws = A.alloc("iws", [128, NT, 4], F32)
            xbcT = A.alloc("xbcT", [128, 6, S], BF16)
            siluz = A.alloc("siluz", [128, NT, 512], BF16)
            dtr = A.alloc("dtr", [128, NT, 8], F32)

            xT = A.alloc("xT", [128, 8, S], BF16)
            if wtm_pref[0] is not None:
                wtm = wtm_pref[0]
                wtm_pref[0] = None
            else:
                wtm = A.alloc("wtm", [128, 8, TMW], BF16)
                for c in range(8):
                    DMA(P, "pool", wtm[:, c, :], wtm_d[c * 128:(c + 1) * 128, :], [], [("wtm", c)])
            xb = [A.alloc("xb%d" % i, [128, D], BF16) for i in range(4)]
            MSET(P, "pool", qT[:], 0.0, [], ["qz0"])
            MSET(P, "pool", iqT[:], 0.0, [], ["iqz0"])
            MSET(P, "pool", Vext[:, :, 64:128], 0.0, [], [("Vext0",)])
            MSET(P, "pool", Vext[:, :, 64:65], 1.0, [("Vext0",)], [("Vext0",)])
            tr_banks = Rot([0, 1])
            fm_banks = Rot([4, 5, 6, 7])
            cp_eng = Rot(["act", "dve"])
            def a_dma(t):
                tok = slice(t * 128, (t + 1) * 128)
                DMA(P, "pool", xb[t % 4][:], x_d[seq, tok, :], [], [("xb", t % 4)])

            def a_tr(t):
                tok = slice(t * 128, (t + 1) * 128)
                xbuf = xb[t % 4]
                b = tr_banks.next()
                pv = psb(b)
                for c in range(8):
                    TR(P, pv[:, c * 128:(c + 1) * 128], xbuf[:, c * 128:(c + 1) * 128], ident_b[:],
                       [("xb", t % 4), "ident_b"], [PSR(b)], inc=(c == 7))
                CP(P, cp_eng.next(), xT[:, :, tok], pv.rearrange("p (c t) -> p c t", c=8),
                   [PSR(b)], [("xT", t)])

            def a_mm(t):
                tok = slice(t * 128, (t + 1) * 128)
                bz, bv = (2, 3) if t % 2 == 0 else (4, 5)
                for c in range(8):
                    MM(P, psf(bz), xT[:, c, tok], wtm[:, c, 0:512], c == 0, c == 7,
                       [("xT", t), ("wtm", c)], [PSR(bz)], inc=(c == 7))
                for c in range(8):
                    MM(P, ps[bv][:, 0:76], xT[:, c, tok], wtm[:, c, 512:588], c == 0, c == 7,
                       [("xT", t), ("wtm", c)], [PSR(bv)], inc=(c == 7))
                ACT(P, siluz[:, t, :], psf(bz), AF.Silu, [PSR(bz)], [("siluz", t)])
                CP(P, "dve", Vext[:, t, 0:64], ps[bv][:, 0:64], [PSR(bv)], [("Vext", t)])
                CP(P, "dve", Vext[:, t, 128:192], ps[bv][:, 0:64], [PSR(bv)], [("Vext", t)])
                TS(P, "dve", iws[:, t, :], ps[bv][:, 64:68], IDX_SCALE, None, ALU.mult, None,
                   [PSR(bv)], [("iws", t)])
                CP(P, "dve", dtr[:, t, :], ps[bv][:, 68:76], [PSR(bv)], [("dtr", t)])

            for t in range(3):
                a_dma(t)
            a_tr(0)
            for t in range(NT):
                if t + 3 < NT:
                    a_dma(t + 3)
                if t + 1 < NT:
                    a_tr(t + 1)
                a_mm(t)

            P.barrier()
            A.free(wtm, *xb)
            cosF = A.alloc("cosF", [128, S], F32)
            sinS = A.alloc("sinS", [128, S], F32)
            convw = A.alloc("convw", [128, 24], F32)
            convb = A.alloc("convb", [128, 6], F32)
            diagw = A.alloc("diagw", [128, 24, 128], BF16)
            uT = A.alloc("uT", [128, 6, S + 3], BF16)
            rt1 = [A.alloc("rt1_%d" % i, [128, 512], F32) for i in range(2)]
            rt2 = [A.alloc("rt2_%d" % i, [128, 512], F32) for i in range(2)]
            wsl = [A.alloc("wsl%d" % i, [128, 8, 128], BF16) for i in range(4)]
            p1_tmp = [xT, cosF, sinS, convw, convb, diagw, uT] + rt1 + rt2 + wsl
            DMA(P, "sp", cosF[:], cosF_d, [], ["cosF"])
            DMA(P, "sp", sinS[:], sinS_d, [], ["sinS"])
            DMA(P, "sp", convw[:], convw_d, [], ["convw"])
            DMA(P, "sp", convb[:], convb_d, [], ["convb"])
            for i in range(24):
                TS(P, "dve", diagw[:, i, :], ident_f[:], convw[:, i:i + 1], None, ALU.mult, None,
                   ["ident_f", "convw"], [("diagw", i)])
            MSET(P, "pool", uT[:, :, 0:3], 0.0, [], [("uT", -1)])
            wsi = [0]

            worder = [0, 4, 1, 5, 2, 6, 3, 7, 8, 9, 10, 12, 11, 13, 14, 15, 16, 17, 18, 19, 20, 21]
            wslot = {}

            def prefetch(n):
                for _ in range(n):
                    if wsi[0] >= len(worder):
                        return
                    f = worder[wsi[0]]
                    k = wsi[0] % 4
                    wsi[0] += 1
                    wslot[f] = k
                    DMA(P, "pool", wsl[k][:], wfm_d[:, f * 128:(f + 1) * 128].rearrange("(c p) f -> p c f", p=128),
                        [], [("wsl", k)])

            def load_w(f):
                return wslot[f]

            prefetch(4)

            def fm_proj(k, tg, b):
                tokg = slice(tg * 512, tg * 512 + 512)
                for c in range(8):
                    MM(P, psf(b), wsl[k][:, c, :], xT[:, c, tokg], c == 0, c == 7,
                       [("xT", tg * 4 + i) for i in range(4)] + [("wsl", k)], [PSR(b)], inc=(c == 7))

            pairs = [(0, 4, (qT, 0), "qT0"), (1, 5, (qT, 1), "qT1"), (2, 6, (qT, 2), "qT2"), (3, 7, (qT, 3), "qT3"),
                     (8, 9, kT, "kT"), (10, 12, (iqT, 0), "iqT0"), (11, 13, (iqT, 1), "iqT1"), (14, 15, ikT, "ikT")]
            rti = 0
            for fa, fr, dstf, dname in pairs:
                ka, kr = load_w(fa), load_w(fr)
                for tg in range(4):
                    tokg = slice(tg * 512, tg * 512 + 512)
                    ba, br = fm_banks.next(), fm_banks.next()
                    fm_proj(ka, tg, ba)
                    fm_proj(kr, tg, br)
                    r1, r2 = rt1[rti % 2], rt2[rti % 2]
                    TT(P, "dve", r1[:], psf(ba), cosF[:, tokg], ALU.mult, [PSR(ba), "cosF"], [("rt1", rti % 2)])
                    TT(P, "dve", r2[:], psf(br), sinS[:, tokg], ALU.mult, [PSR(br), "sinS"], [("rt2", rti % 2)])
                    rr_ = [("rt1", rti % 2), ("rt2", rti % 2), "qz0", "iqz0"]
                    if isinstance(dstf, tuple):
                        dt_, pp = dstf
                        TT(P, "pool", dt_[0:64, 2 * pp, tokg], r1[0:64, :], r2[0:64, :], ALU.add, rr_, [(dname, tg, 0)])
                        TT(P, "pool", dt_[64:128, 2 * pp + 1, tokg], r1[64:128, :], r2[64:128, :], ALU.add, rr_,
                           [(dname, tg, 1)])
                    else:
                        TT(P, "pool", dstf[:, tokg], r1[:], r2[:], ALU.add, rr_, [(dname, tg)])
                    rti += 1
                prefetch(2)
            for ct in range(6):
                k = load_w(16 + ct)
                for tg in range(4):
                    t0 = tg * 512
                    b = fm_banks.next()
                    fm_proj(k, tg, b)
                    CP(P, "act", uT[:, ct, 3 + t0:3 + t0 + 512], psf(b), [PSR(b)], [("uT", ct, tg)])
                prefetch(1)
                for tg in range(4):
                    t0 = tg * 512
                    b = fm_banks.next()
                    rr = [("uT", ct, tg), ("uT", -1)] + ([("uT", ct, tg - 1)] if tg > 0 else [])
                    for j in range(4):
                        MM(P, psf(b), diagw[:, ct * 4 + j, :], uT[:, ct, t0 + j:t0 + j + 512], j == 0, j == 3,
                           rr + [("diagw", ct * 4 + j)], [PSR(b)], inc=(j == 3))
                    ACT(P, xbcT[:, ct, t0:t0 + 512], psf(b), AF.Silu, [PSR(b), "convb"], [("xbcT", ct, tg)],
                        bias=convb[:, ct:ct + 1])
            g4 = range(4)
            P.barrier()
            dump("qz", qT[:], [128, 8, S], [])
            dump("iqz", iqT[:], [128, 4, S], [])
            A.free(*p1_tmp)
            if stop_after == 1:
                A.free(qT, kT, iqT, ikT, Vext, iws, xbcT, siluz, dtr)
                continue

            mixT = A.alloc("mixT", [128, 8, S], BF16)
            dtb = A.alloc("dtb", [128, 8], F32)
            arep = A.alloc("arep", [128, 8], F32)
            dskip = A.alloc("dskip", [128, 512], F32)
            nwr = A.alloc("nwr", [128, 512], F32)
            dt_all = A.alloc("dt_all", [128, NT, 8], F32)
            A_all = A.alloc("A_all", [128, NT, 8], F32)
            acum = A.alloc("acum", [128, NT, 8], F32)
            tot = A.alloc("tot", [128, NT, 8], F32)
            ds_all = A.alloc("ds_all", [128, NT, 8], F32)
            cdec = A.alloc("cdec", [128, NT, 8], F32)
            state = A.alloc("state", [128, 8, 64], F32)
            NB = 2
            xdt = [A.alloc("xdt%d" % i, [128, 8, 64], BF16) for i in range(NB)]
            xdtd = [A.alloc("xdtd%d" % i, [128, 8, 64], BF16) for i in range(NB)]
            dsk = [A.alloc("dsk%d" % i, [128, 512], F32) for i in range(NB)]
            Btok = [A.alloc("Btok%d" % i, [128, 128], BF16) for i in range(NB)]
            Gm = [A.alloc("Gm%d" % i, [128, 2, 128], BF16) for i in range(NB)]
            Ab = [A.alloc("Ab%d" % i, [128, 2, 8, 128], BF16) for i in range(NB)]
            A_hl = A.alloc("A_hl", [128, 2, NT, 8], BF16)
            A_res = A.alloc("A_res", [128, NT, 8], F32)
            Dm = [A.alloc("Dm%d" % i, [128, 8, 128], F32) for i in range(NB)]
            Lm = [A.alloc("Lm%d" % i, [128, 8, 128], BF16) for i in range(NB)]
            Eb = [A.alloc("Eb%d" % i, [128, 8, 128], BF16) for i in range(NB)]
            MT = [A.alloc("MT%d" % i, [128, 8, 128], BF16) for i in range(NB)]
            Cp = [A.alloc("Cp%d" % i, [128, 8, 128], BF16) for i in range(NB)]
            CTz = [A.alloc("CTz%d" % i, [128, 2, 128], BF16) for i in range(NB)]
            prevb = [A.alloc("prevb%d" % i, [128, 8, 64], BF16) for i in range(NB)]
            y1 = [A.alloc("y1_%d" % i, [128, 512], F32) for i in range(NB)]
            y2 = [A.alloc("y2_%d" % i, [128, 512], F32) for i in range(NB)]
            yo = [A.alloc("yo%d" % i, [128, 512], BF16) for i in range(NB)]
            junk = [A.alloc("junk%d" % i, [128, 512], BF16) for i in range(NB)]
            ms = [A.alloc("ms%d" % i, [128, 2], F32) for i in range(NB)]
            p3_tmp = ([A_hl, A_res, dtb, arep, dskip, nwr, dt_all, A_all, acum, tot, ds_all, cdec, state] + xdt + xdtd + dsk + Btok + Gm
                      + Ab + Dm + Lm + Eb + MT + Cp + CTz + prevb + y1 + y2 + yo + junk + ms)

            for i in range(NB):
                MSET(P, "pool", Cp[i][:], 0.0, [], [("Cp", i, 0), ("Cp", i, 1)])
                MSET(P, "pool", CTz[i][:], 0.0, [], [("CTz", i)])
            DMA(P, "sp", dtb[:], dtb_d, [], ["dtb"])
            DMA(P, "sp", arep[:], alog_d, [], ["arep"])
            DMA(P, "sp", dskip[:], dskip_d, [], ["dskip"])
            DMA(P, "sp", nwr[:], nw_d, [], ["nwr"])
            ACT(P, arep[:], arep[:], AF.Exp, ["arep"], ["arep"])
            TS(P, "dve", arep[:], arep[:], -1.0, None, ALU.mult, None, ["arep"], ["arep"])
            alldtr = [("dtr", t) for t in range(NT)]
            TT(P, "dve", dt_all[:], dtr[:], dtb[:].unsqueeze(1).to_broadcast([128, NT, 8]), ALU.add,
               alldtr + ["dtb"], ["dt_all"])
            ACT(P, dt_all[:], dt_all[:], AF.Exp, ["dt_all"], ["dt_all"])
            ACT(P, dt_all[:], dt_all[:], AF.Ln, ["dt_all"], ["dt_all"], bias=1.0)
            TT(P, "dve", A_all[:], dt_all[:], arep[:].unsqueeze(1).to_broadcast([128, NT, 8]), ALU.mult,
               ["dt_all", "arep"], ["A_all"])
            CP(P, "dve", A_hl[:, 0, :, :], A_all[:], ["A_all"], ["A_hl0"])
            TT(P, "dve", A_res[:], A_all[:], A_hl[:, 0, :, :], ALU.subtract, ["A_all", "A_hl0"], ["A_res"])
            CP(P, "dve", A_hl[:, 1, :, :], A_res[:], ["A_res"], ["A_hl1"])
            A2 = A_all[:].rearrange("p c h -> p (c h)")
            MM(P, ps[0][:, 0:128], tri_f[:], A2, True, True, ["tri_f", "A_all"], [PSR(0)])
            MM(P, ps[1][:, 0:128], ones_f[:], A2, True, True, ["ones_f", "A_all"], [PSR(1)])
            CP(P, "dve", acum[:].rearrange("p c h -> p (c h)"), ps[0][:, 0:128], [PSR(0)], ["acum"])
            CP(P, "dve", tot[:].rearrange("p c h -> p (c h)"), ps[1][:, 0:128], [PSR(1)], ["tot"])
            TT(P, "dve", ds_all[:], tot[:], acum[:], ALU.subtract, ["tot", "acum"], ["ds_all"])
            ACT(P, ds_all[:], ds_all[:], AF.Exp, ["ds_all"], ["ds_all"])
            ACT(P, cdec[:], tot[:], AF.Exp, ["tot"], ["cdec"])
            trb = Rot([0, 1])
            def ssd_s1(c):
                    i = c % NB
                    tok = slice(c * 128, (c + 1) * 128)
                    g4 = c // 4
                    xres = [("xbcT", ct, g4) for ct in range(6)]
                    CP(P, "dve", Ab[i][:], A_hl[:, :, c, :].unsqueeze(3).to_broadcast([128, 2, 8, 128]), ["A_hl0", "A_hl1"],
                       [("Ab", i)])
                    for h in range(8):
                        bb = 3 + h // 4
                        MM(P, ps[bb][:, (h % 4) * 128:(h % 4 + 1) * 128], Ab[i][:, 0, h, :], tri_b[:], True, False,
                           [("Ab", i), "tri_b"], [PSR(bb)], inc=False)
                        MM(P, ps[bb][:, (h % 4) * 128:(h % 4 + 1) * 128], Ab[i][:, 1, h, :], tri_b[:], False, True,
                           [("Ab", i), "tri_b"], [PSR(bb)], inc=(h % 4 == 3))
                    b = trb.next()
                    pv = psb(b)
                    for ct in range(5):
                        TR(P, pv[:, ct * 128:(ct + 1) * 128], xbcT[:, ct, tok], ident_b[:], xres + ["ident_b"], [PSR(b)],
                           inc=(ct == 4))
                    xs_ps = pv[:, 0:512].rearrange("p (h d) -> p h d", h=8)
                    TT(P, "dve", xdt[i][:], xs_ps, dt_all[:, c, :].unsqueeze(2).to_broadcast([128, 8, 64]), ALU.mult,
                       [PSR(b), "dt_all"], [("xdt", i)])
                    TT(P, "dve", dsk[i][:], pv[:, 0:512], dskip[:], ALU.mult, [PSR(b), "dskip"], [("dsk", i)])
                    CP(P, "act", Btok[i][:], pv[:, 512:640], [PSR(b)], [("Btok", i)])
                    TT(P, "dve", xdtd[i][:], xdt[i][:], ds_all[:, c, :].unsqueeze(2).to_broadcast([128, 8, 64]), ALU.mult,
                       [("xdt", i), "ds_all"], [("xdtd", i)])
                    for g in range(2):
                        CP(P, "pool", CTz[i][64 * g:64 * g + 64, g, :], xbcT[64 * g:64 * g + 64, 5, tok], xres, [("CTz", i)])
                    for g in range(2):
                        MM(P, ps[2][:, g * 128:(g + 1) * 128], xbcT[:, 4, tok], CTz[i][:, g, :],
                           True, True, xres + [("CTz", i)], [PSR(2)], inc=(g == 1))
                    TT(P, "dve", Gm[i][:], ps[2][:, 0:256].rearrange("p (g l) -> p g l", g=2),
                       tri_f[:].unsqueeze(1).to_broadcast([128, 2, 128]), ALU.mult, [PSR(2), "tri_f"], [("Gm", i)])
                    for g in range(2):
                        TT(P, "dve", Dm[i][:, 4 * g:4 * g + 4, :], ps[3 + g][:].rearrange("p (h l) -> p h l", h=4),
                           acum[:, c, 4 * g:4 * g + 4].unsqueeze(2).to_broadcast([128, 4, 128]), ALU.subtract,
                           [PSR(3 + g), "acum"], [("Dm", i)])
                    ACT(P, Dm[i][:], Dm[i][:], AF.Relu, [("Dm", i)], [("Dm", i)], scale=-1.0)
                    ACT(P, Lm[i][:], Dm[i][:], AF.Exp, [("Dm", i)], [("Lm", i)], scale=-1.0)
                    for g in range(2):
                        ACT(P, Eb[i][:, g * 4:(g + 1) * 4, :], ps[3 + g][:].rearrange("p (h l) -> p h l", h=4), AF.Exp,
                            [PSR(3 + g)], [("Eb", i, g)])
                    for g in range(2):
                        TT(P, "dve", MT[i][:, g * 4:(g + 1) * 4, :], Lm[i][:, g * 4:(g + 1) * 4, :],
                           Gm[i][:, g, :].unsqueeze(1).to_broadcast([128, 4, 128]), ALU.mult,
                           [("Lm", i), ("Gm", i)], [("MT", i, g)])
                        TT(P, "dve", Cp[i][64 * g:64 * g + 64, g * 4:(g + 1) * 4, :], Eb[i][64 * g:64 * g + 64, g * 4:(g + 1) * 4, :],
                           xbcT[64 * g:64 * g + 64, 5, tok].unsqueeze(1).to_broadcast([64, 4, 128]), ALU.mult,
                           [("Eb", i, g)] + xres, [("Cp", i, g)])

            def ssd_s2(c):
                    i = c % NB
                    tok = slice(c * 128, (c + 1) * 128)
                    g4 = c // 4
                    xres = [("xbcT", ct, g4) for ct in range(6)]
                    if c > 0:
                        CP(P, "pool", prevb[i][:], state[:], ["state"], [("prevb", i)])
                    for h in range(8):
                        g = h // 4
                        MM(P, ps[5][:, h * 64:(h + 1) * 64], MT[i][:, h, :], xdt[i][:, h, :], True, c == 0,
                           [("MT", i, g), ("xdt", i)], [PSR(5)], inc=(c == 0 and h == 7))
                        if c > 0:
                            MM(P, ps[5][:, h * 64:(h + 1) * 64], Cp[i][:, h, :],
                               prevb[i][:, h, :], False, True, [("Cp", i, g), ("prevb", i)], [PSR(5)],
                               inc=(h == 7))
                    if c < NT - 1:
                        for h in range(8):
                            MM(P, ps[6][:, h * 64:(h + 1) * 64], Btok[i][:], xdtd[i][:, h, :], True, True,
                               [("Btok", i), ("xdtd", i)], [PSR(6)], inc=(h == 7))
                        st2 = state[:].rearrange("p h d -> p (h d)")
                        if c == 0:
                            CP(P, "dve", st2, psf(6), [PSR(6)], ["state"])
                        else:
                            TT(P, "dve", state[:], state[:], cdec[:, c, :].unsqueeze(2).to_broadcast([128, 8, 64]), ALU.mult,
                               ["state", "cdec", ("prevb", i)], ["state"])
                            TT(P, "dve", st2, st2, psf(6), ALU.add, ["state", PSR(6)], ["state"])
                    TT(P, "dve", y1[i][:], psf(5), dsk[i][:], ALU.add, [PSR(5), ("dsk", i)], [("y1", i)])
                    TT(P, "pool", y2[i][:], y1[i][:], siluz[:, c, :], ALU.mult, [("y1", i), ("siluz", c)], [("y2", i)])
                    ACT(P, junk[i][:], y2[i][:], AF.Square, [("y2", i)], [("junk", i), ("ms", i)], scale=float(512 ** -0.5),
                        accum=ms[i][:, 0:1])
                    ACT(P, ms[i][:, 1:2], ms[i][:, 0:1], AF.Ln, [("ms", i)], [("ms2", i)], bias=LN_EPS)
                    ACT(P, ms[i][:, 1:2], ms[i][:, 1:2], AF.Exp, [("ms2", i)], [("ms2", i)], scale=-0.5)

            def ssd_s3(c):
                    i = c % NB
                    tok = slice(c * 128, (c + 1) * 128)
                    ACT(P, yo[i][:], y2[i][:], AF.Copy, [("y2", i), ("ms2", i)], [("yo", i)], scale=ms[i][:, 1:2])
                    pv7 = psb(7)
                    for k in range(4):
                        TR(P, pv7[:, k * 128:(k + 1) * 128], yo[i][:, k * 128:(k + 1) * 128], ident_b[:], [("yo", i), "ident_b"],
                           [PSR(7)], inc=(k == 3))
                    CP(P, "act", mixT[:, 4:8, tok], pv7[:, 0:512].rearrange("p (k t) -> p k t", k=4), [PSR(7)], [("mixT", "s", c)])


            ssd_s1(0)
            for c in range(NT):
                if c + 1 < NT:
                    ssd_s1(c + 1)
                if c > 0:
                    ssd_s3(c - 1)
                ssd_s2(c)
            ssd_s3(NT - 1)
            dump("ssdT", mixT[:, 4:8, :], [128, 4, S], [("mixT", "s", c) for c in range(NT)])
            P.barrier()
            A.free(*p3_tmp)
            A.free(xbcT, siluz, dtr)
            if stop_after == 3:
                A.free(mixT, qT, kT, iqT, ikT, Vext, iws)
                continue

            I4 = A.alloc("I4", [128, 4, S], F32)
            junkb = A.alloc("junkb", [128, S], BF16)
            selb = [A.alloc("selb%d" % i, [128, S], BF16) for i in range(2)]
            Rt = [A.alloc("Rt%d" % i, [128, 512], F32) for i in range(4)]
            maskT = [A.alloc("maskT%d" % i, [128, NT, 512], BF16) for i in range(2)]
            Pexp = [A.alloc("Pexp%d" % i, [128, 512], BF16) for i in range(3)]
            Pm = []
            Osb = [A.alloc("Osb%d" % i, [128, 512], F32) for i in range(3)]
            rec = []
            Sden = A.alloc("Sden", [128, 2, 128], F32)
            halfpow = A.alloc("halfpow", [128, NBIS + 2], F32)
            aw = A.alloc("aw", [128, 4, 4], F32)
            sg = A.alloc("sg", [128, 4, 4], F32)
            lo0 = A.alloc("lo0", [128, 4], F32)
            hi0 = A.alloc("hi0", [128, 4], F32)
            wd = A.alloc("wd", [128, 4, NBIS + 2], F32)
            mid = A.alloc("mid", [128, 4], F32)
            cnt = A.alloc("cnt", [128, 4], F32)
            sacc = A.alloc("sacc", [128, 2], F32)
            lhalf = A.alloc("lhalf", [128, 2], F32)
            junka = A.alloc("junka", [128, S], BF16)
            u2 = A.alloc("u2", [128, 4], F32)
            tq = A.alloc("tq", [128, 4], F32)
            thr = A.alloc("thr", [128, 4], F32)
            p2_tmp = maskT + [sacc, lhalf, junka, I4, junkb, Sden, halfpow, aw, sg, lo0, hi0, wd, mid, cnt, u2, tq, thr] + selb + Rt + Pexp + Pm + Osb + rec
            DMA(P, "sp", halfpow[:], halfpow_d, [], ["halfpow"])
            MSET(P, "pool", Sden[:], 0.0, [], ["Sden"])
            MSET(P, "pool", Sden[64:65, 0, :], 1.0, ["Sden"], ["Sden"])
            MSET(P, "pool", Sden[0:1, 1, :], 1.0, ["Sden"], ["Sden"])
            ibank = Rot([0, 1, 2])
            oi_box = [0]

            def gen_IB(g):
                mT = maskT[g % 2]
                nkb = g + 1
                Ls = []
                for b in range(4):
                    qi = 4 * g + b
                    L = (qi + 1) * 128
                    Ls.append(L)
                    qtok = slice(qi * 128, (qi + 1) * 128)
                    for kb in range(nkb):
                        w = min(512, L - kb * 512)
                        ks = slice(kb * 512, kb * 512 + w)
                        for h in range(4):
                            bk = ibank.next()
                            MM(P, ps[bk][:, 0:w], iqT[:, h, qtok], ikT[:, ks], True, True, [], [PSR(bk)])
                            ACT(P, Rt[h][:, 0:w], ps[bk][:, 0:w], AF.Relu, [PSR(bk)], [("Rt", h)])
                        TS(P, "dve", I4[:, b, ks], Rt[0][:, 0:w], iws[:, qi, 0:1], None, ALU.mult, None,
                           [("Rt", 0)], [("I4", b)])
                        for h in range(1, 4):
                            STT(P, "dve", I4[:, b, ks], Rt[h][:, 0:w], iws[:, qi, h:h + 1], I4[:, b, ks], ALU.mult, ALU.add,
                                [("Rt", h), ("I4", b)], [("I4", b)])
                        yield 3.0 * w / 512 + 0.5
                    P.op("dve", lambda e, o=lo0[:, b:b + 1], i_=I4[:, b, 0:L]: e.tensor_reduce(o, i_, axis=AX.X, op=ALU.min),
                         [("I4", b)], [("lo0", b)])
                    TT(P, "dve", I4[:, b, qi * 128:(qi + 1) * 128], I4[:, b, qi * 128:(qi + 1) * 128], cneg[:], ALU.add,
                       [("I4", b), ("lo0", b), "cneg"], [("I4", b)])
                    P.op("dve", lambda e, o=hi0[:, b:b + 1], i_=I4[:, b, 0:L]: e.tensor_reduce(o, i_, axis=AX.X, op=ALU.max),
                         [("I4", b)], [("hi0", b)])
                    yield 2.2 * L / 1024 + 0.6
                allb = [("lo0", b) for b in range(4)] + [("hi0", b) for b in range(4)]
                TT(P, "dve", tq[:], hi0[:], lo0[:], ALU.subtract, allb, ["tq"])
                TT(P, "dve", wd[:], tq[:].unsqueeze(2).to_broadcast([128, 4, NBIS + 2]),
                   halfpow[:].unsqueeze(1).to_broadcast([128, 4, NBIS + 2]), ALU.mult, ["tq", "halfpow"], ["wd"])
                TT(P, "dve", mid[:], lo0[:], wd[:, :, 0], ALU.add, allb + ["wd"], ["mid"])
                yield 1.0
                MSET(P, "pool", lhalf[:, 0:1], Ls[2] / 2.0, [], ["lhalf"])
                MSET(P, "pool", lhalf[:, 1:2], Ls[3] / 2.0, [], ["lhalf"])
                for k in range(NBIS):
                    for b in (2, 3):
                        ACT(P, junka[:, 0:Ls[b]], I4[:, b, 0:Ls[b]], AF.Sign, [("I4", b), "mid"], ["junka", ("sacc", b)],
                            bias=mid[:, b:b + 1], scale=-1.0, accum=sacc[:, b - 2:b - 1])
                    for b in (0, 1):
                        TS(P, "dve", junkb[:, 0:Ls[b]], I4[:, b, 0:Ls[b]], mid[:, b:b + 1], 0.0, ALU.is_ge, ALU.add,
                           [("I4", b), "mid"], ["junkb", ("cnt", b)], accum=cnt[:, b:b + 1])
                    STT(P, "dve", cnt[:, 2:4], sacc[:, 0:2], -0.5, lhalf[:, 0:2], ALU.mult, ALU.add,
                        [("sacc", 2), ("sacc", 3), "lhalf"], [("cnt", 2)])
                    TS(P, "dve", u2[:], cnt[:], 256.0, 2.0, ALU.is_ge, ALU.mult, [("cnt", 0), ("cnt", 1), ("cnt", 2)], ["u2"])
                    STT(P, "dve", tq[:], u2[:], -1.0, wd[:, :, k + 1], ALU.add, ALU.mult, ["u2", "wd"], ["tq"])
                    TT(P, "dve", mid[:], mid[:], tq[:], ALU.add, ["mid", "tq"], ["mid"])
                    yield (Ls[0] + Ls[1]) / 960.0 + 1.2
                TT(P, "dve", thr[:], mid[:], wd[:, :, NBIS - 2], ALU.subtract, ["mid", "wd"], ["thr"])
                for b in range(4):
                    qi = 4 * g + b
                    sb_ = selb[b % 2]
                    TS(P, "dve", sb_[:, 0:Ls[b]], I4[:, b, 0:Ls[b]], thr[:, b:b + 1], MASKB, ALU.is_lt, ALU.mult,
                       [("I4", b), "thr"], [("selb", b % 2)])
                    for c0 in range(0, qi + 1, 8):
                        n = min(8, qi + 1 - c0)
                        pv = psb(3)
                        for j in range(n):
                            TR(P, pv[:, j * 128:(j + 1) * 128], sb_[:, (c0 + j) * 128:(c0 + j + 1) * 128], ident_b[:],
                               [("selb", b % 2)], [PSR(3)], inc=(j == n - 1))
                        CP(P, "act", mT[:, c0:c0 + n, b * 128:(b + 1) * 128],
                           pv[:, 0:n * 128].rearrange("p (c q) -> p c q", c=n), [PSR(3)], [("maskT", g % 2, b)])
                    yield Ls[b] / 1024.0 + 1.0

            def gen_A(g):
                mT = maskT[g % 2]
                mres = [("maskT", g % 2, b) for b in range(4)]
                nkc = 4 * g + 4
                items = [(h, c) for h in range(8) for c in range(nkc)]

                def emit_S(idx):
                    h, c = items[idx]
                    cs = max(0, c - 4 * g) * 128
                    qs = slice(g * 512 + cs, g * 512 + 512)
                    bk = 4 + idx % 3
                    MM(P, ps[bk][:, cs:512], kT[:, c * 128:(c + 1) * 128], qT[:, h, qs], True, False, [], [PSR(bk)], inc=False)
                    MM(P, ps[bk][:, cs:512], ident_b[:], mT[:, c, cs:512], False, True, mres, [PSR(bk)])
                    ACT(P, Pexp[idx % 3][:, cs:512], ps[bk][:, cs:512], AF.Exp, [PSR(bk)], [("Pexp", idx % 3)], scale=0.125)

                def emit_PV(idx):
                    h, c = items[idx]
                    par = h % 2
                    rows = slice(64 * par, 64 * par + 64)
                    vcols = slice(0, 128) if par == 0 else slice(64, 192)
                    cs = max(0, c - 4 * g) * 128
                    MM(P, ps[7][:, cs:512], Vext[:, c, vcols], Pexp[idx % 3][:, cs:512], c == 0, c == nkc - 1,
                       [("Pexp", idx % 3)], [PSR(7)], inc=True)
                    if c == nkc - 1:
                        o_ = oi_box[0]
                        ob = Osb[o_ % 3]
                        ores = ("Osb", o_ % 3)
                        dr = slice(64, 65) if par == 0 else slice(0, 1)
                        CP(P, "act", ob[:], psf(7), [PSR(7)], [ores])
                        ACT(P, ob[dr, :], ob[dr, :], AF.Ln, [ores], [ores])
                        ACT(P, ob[dr, :], ob[dr, :], AF.Exp, [ores], [ores], scale=-1.0)

                        def fin1(ob=ob, ores=ores, par=par):
                            MM(P, psf(3), Sden[:, par, :], ob[:], True, True, [ores, "Sden"], [PSR(3)])

                        def fin2(ob=ob, ores=ores, rows=rows, h=h):
                            TT(P, "dve", mixT[rows, h // 2, g * 512:(g + 1) * 512], ob[rows, :], ps[3][rows, :], ALU.mult,
                               [ores, PSR(3)], [("mixT", "a", h, g)])
                        pending.append([2, fin1, fin2])
                        oi_box[0] += 1

                pending = []

                def run_pending(force=False):
                    for p_ in list(pending):
                        p_[0] -= 1
                        if p_[0] <= 0 or force:
                            p_[1]()
                            p_[2]()
                            pending.remove(p_)

                emit_S(0)
                emit_S(1)
                yield 1.4
                for idx in range(len(items)):
                    emit_PV(idx)
                    if idx + 2 < len(items):
                        emit_S(idx + 2)
                    run_pending()
                    yield 0.75
                run_pending(force=True)

            def timed_interleave(*gens):
                gens = [[0.0, g_] for g_ in gens]
                while gens:
                    gens.sort(key=lambda x: x[0])
                    cur = gens[0]
                    try:
                        cur[0] += next(cur[1])
                    except StopIteration:
                        gens.remove(cur)

            gorder = [3, 2, 1, 0]
            timed_interleave(gen_IB(gorder[0]))
            for gi, g in enumerate(gorder):
                if gi + 1 < 4:
                    timed_interleave(gen_A(g), gen_IB(gorder[gi + 1]))
                else:
                    timed_interleave(gen_A(g))
            dump("attT", mixT[:, 0:4, :], [128, 4, S], [("mixT", "a", h, g) for h in range(8) for g in range(4)])
            P.barrier()
            A.free(*p2_tmp)
            A.free(qT, kT, iqT, ikT, Vext, iws)
            if stop_after == 2:
                A.free(mixT)
                continue

            def ln_gen(r, st, junk_, gam, bet, hn, tag, add_eng="pool"):
                ACT(P, junk_[:], r[:], AF.Identity, [(tag, "r")], [(tag, "junk"), (tag, "st")], accum=st[:, 0:1])
                yield
                TS(P, "dve", st[:, 1:2], st[:, 0:1], -1.0 / D, None, ALU.mult, None, [(tag, "st")], [(tag, "st")])
                yield
                ACT(P, junk_[:], r[:], AF.Square, [(tag, "r"), (tag, "st")], [(tag, "junk"), (tag, "st")],
                    bias=st[:, 1:2], accum=st[:, 2:3])
                yield
                ACT(P, st[:, 3:4], st[:, 2:3], AF.Ln, [(tag, "st")], [(tag, "st")], bias=LN_EPS, scale=1.0 / D)
                yield
                ACT(P, st[:, 3:4], st[:, 3:4], AF.Exp, [(tag, "st")], [(tag, "st")], scale=-0.5)
                yield
                TT(P, "dve", st[:, 4:5], st[:, 1:2], st[:, 3:4], ALU.mult, [(tag, "st")], [(tag, "st")])
                yield
                ACT(P, hn[:], r[:], AF.Identity, [(tag, "r"), (tag, "st")], [(tag, "hn")], bias=st[:, 4:5], scale=st[:, 3:4])
                yield
                TT(P, "dve", hn[:], hn[:], gam[:], ALU.mult, [(tag, "hn"), "lng"], [(tag, "hn")])
                yield
                TT(P, add_eng, hn[:], hn[:], bet[:], ALU.add, [(tag, "hn"), "lnb"], [(tag, "hn")])
                yield

            def interleave(*gens):
                gens = list(gens)
                while gens:
                    for g_ in list(gens):
                        try:
                            next(g_)
                        except StopIteration:
                            gens.remove(g_)

            def layer_norm(r, st, junk_, gam, bet, hn, tag):
                interleave(ln_gen(r, st, junk_, gam, bet, hn, tag, add_eng="dve"))

            hT = A.alloc("hT", [128, 8, S], BF16)
            gates = A.alloc("gates", [128, NT, 16], F32)
            Wgu = [A.alloc("Wgu%d" % i, [128, 8, 512], BF16) for i in range(2)]
            wout = A.alloc("wout", [128, 8, D], BF16)
            lng = A.alloc("lng", [128, D], F32)
            lnb = A.alloc("lnb", [128, D], F32)
            xres = [A.alloc("xres%d" % i, [128, D], F32) for i in range(6)]
            rr = [A.alloc("rr%d" % i, [128, D], F32) for i in range(6)]
            hn = [A.alloc("hn%d" % i, [128, D], F32) for i in range(6)]
            lnj = [A.alloc("lnj%d" % i, [128, D], BF16) for i in range(6)]
            stt_ = [A.alloc("st%d" % i, [128, 8], F32) for i in range(6)]
            hT32 = []
            hlo = [A.alloc("hlo%d" % i, [128, 8, 128], BF16) for i in range(2)]
            wr_hl = A.alloc("wr_hl", [128, 2, 8, 20], BF16)
            wr_res = A.alloc("wr_res", [128, 8, 20], F32)
            wr = A.alloc("wr", [128, 8, 20], F32)
            nwc = A.alloc("nwc", [128, 4], F32)
            brp = A.alloc("brp", [128, 20], F32)
            lg = A.alloc("lg", [128, NT, 20], F32)
            rs = [A.alloc("rs%d" % i, [128, NT, 16], F32) for i in range(4)]
            rv = [A.alloc("rv%d" % i, [128, NT], F32) for i in range(8)]
            p4_tmp = [wout, lng, lnb, wr, nwc, brp, lg, wr_hl, wr_res] + hlo + xres + rr + hn + lnj + stt_ + rs + rv
            for c in range(8):
                DMA(P, "pool", wout[:, c, :], wout_d[c * 128:(c + 1) * 128, :], [], [("wout", c)])
            for e0 in range(2):
                DMA(P, "pool", Wgu[e0][:, :, 0:256], wg_d[e0].rearrange("(c p) f -> p c f", p=128), [], [("Wgu", e0)])
                DMA(P, "pool", Wgu[e0][:, :, 256:512], wu_d[e0].rearrange("(c p) f -> p c f", p=128), [], [("Wgu", e0)])
            DMA(P, "sp", lng[:], ln1g_d, [], ["lng"])
            DMA(P, "sp", lnb[:], ln1b_d, [], ["lnb"])
            DMA(P, "sp", nwc[:], nwc_d, [], ["nwc"])
            for k in range(4):
                TS(P, "dve", wout[:, 4 + k, :], wout[:, 4 + k, :], nwc[:, k:k + 1], None, ALU.mult, None,
                   [("wout", 4 + k), "nwc"], [("wout", 4 + k)])
            DMA(P, "sp", wr[:], wr_d.rearrange("(c p) f -> p c f", p=128), [], ["wr"])
            DMA(P, "sp", brp[:], br_d, [], ["brp"])
            CP(P, "dve", wr_hl[:, 0, :, :], wr[:], ["wr"], ["wr_h"])
            TT(P, "dve", wr_res[:], wr[:], wr_hl[:, 0, :, :], ALU.subtract, ["wr", "wr_h"], ["wr_res"])
            CP(P, "dve", wr_hl[:, 1, :, :], wr_res[:], ["wr_res"], ["wr_hl"])
            mb = Rot([0, 1, 2, 3])

            def mm_part(t):
                i = t % 6
                tok = slice(t * 128, (t + 1) * 128)
                tag = ("ln1", i)
                DMA(P, "sp", xres[i][:], x_d[seq, tok, :], [], [("xres", i)])
                for half in range(2):
                    b = mb.next()
                    hs = slice(half * 512, half * 512 + 512)
                    for c in range(8):
                        MM(P, psf(b), mixT[:, c, tok], wout[:, c, hs], c == 0, c == 7, [("wout", c)], [PSR(b)], inc=(c == 7))
                    STT(P, "dve", rr[i][:, hs], xres[i][:, hs], ALPHA, psf(b), ALU.mult, ALU.add,
                        [("xres", i), PSR(b)], [(tag, "r")])

            def post_gen(t):
                i = t % 6
                tok = slice(t * 128, (t + 1) * 128)
                tag = ("ln1", i)
                yield from ln_gen(rr[i], stt_[i], lnj[i], lng, lnb, hn[i], tag, add_eng="dve")
                DMA(P, "pool", hscr_d[seq, tok, :], hn[i][:], [(tag, "hn")], [])
                yield

            def tr_part(t):
                i = t % 6
                tok = slice(t * 128, (t + 1) * 128)
                tag = ("ln1", i)
                hres_ = [("hT32a", t % 2), ("hT32b", t % 2)]
                for c in range(8):
                    b = 4 + c // 4
                    TR(P, ps[b][:, (c % 4) * 128:(c % 4 + 1) * 128], hn[i][:, c * 128:(c + 1) * 128], ident_f[:],
                       [(tag, "hn")], [PSR(b)], inc=(c % 4 == 3))
                lo_ = hlo[t % 2]
                CP(P, "act", hT[:, 0:4, tok], ps[4][:].rearrange("p (c t) -> p c t", c=4), [PSR(4)], [("hT", t, 0)])
                CP(P, "dve", hT[:, 4:8, tok], ps[5][:].rearrange("p (c t) -> p c t", c=4), [PSR(5)], [("hT", t, 1)])
                TT(P, "dve", lo_[:, 0:4, :], ps[4][:].rearrange("p (c t) -> p c t", c=4), hT[:, 0:4, tok], ALU.subtract,
                   [PSR(4), ("hT", t, 0)], [hres_[0]])
                TT(P, "dve", lo_[:, 4:8, :], ps[5][:].rearrange("p (c t) -> p c t", c=4), hT[:, 4:8, tok], ALU.subtract,
                   [PSR(5), ("hT", t, 1)], [hres_[1]])
                rb = 6 + t % 2
                rres = hres_ + [("hT", t, 0), ("hT", t, 1), "wr_hl"]
                for c in range(8):
                    MM(P, ps[rb][:, 0:20], hT[:, c, tok], wr_hl[:, 0, c, :], c == 0, False, rres, [PSR(rb)], inc=False)
                    MM(P, ps[rb][:, 0:20], hT[:, c, tok], wr_hl[:, 1, c, :], False, False, rres, [PSR(rb)], inc=False)
                    MM(P, ps[rb][:, 0:20], lo_[:, c, :], wr_hl[:, 0, c, :], False, c == 7, rres, [PSR(rb)], inc=(c == 7))
                TT(P, "dve", lg[:, t, :], ps[rb][:, 0:20], brp[:], ALU.add, [PSR(rb), "brp"], [("lg", t)])

            mm_part(0)
            mm_part(1)
            mm_part(2)
            mm_part(3)
            interleave(post_gen(0), post_gen(1))
            for tp in range(0, NT, 2):
                if tp + 2 < NT:
                    interleave(post_gen(tp + 2), post_gen(tp + 3))
                if tp + 4 < NT:
                    mm_part(tp + 4)
                    mm_part(tp + 5)
                tr_part(tp)
                tr_part(tp + 1)
            dump("hT", hT[:], [128, 8, S], [("hT", t, k_) for t in range(NT) for k_ in range(2)])
            lgr = [("lg", t) for t in range(NT)]
            gl = lg[:, :, 0:4]
            gmax, gsum, gprob, m1, m2, dd, w1, w2 = [rv[i] for i in range(8)]
            ge, ohg, pen = rs[0][:, :, 0:4], rs[1][:, :, 0:4], rs[2][:, :, 0:4]

            def red(out, in_, op, r, w):
                P.op("dve", lambda e: e.tensor_reduce(out, in_, axis=AX.X, op=op), r, w)

            def bc(v, n):
                return v[:].unsqueeze(2).to_broadcast([128, NT, n])
            red(gmax[:], gl, ALU.max, lgr, ["gmax"])
            TT(P, "dve", ge, gl, bc(gmax, 4), ALU.subtract, lgr + ["gmax"], ["ge"])
            ACT(P, ge, ge, AF.Exp, ["ge"], ["ge"])
            red(gsum[:], ge, ALU.add, ["ge"], ["gsum"])
            P.op("dve", lambda e: e.reciprocal(gprob[:], gsum[:]), ["gsum"], ["gprob"])
            TT(P, "dve", ohg, gl, bc(gmax, 4), ALU.is_ge, lgr + ["gmax"], ["ohg"])
            TS(P, "dve", pen, ohg, -1.0, 1.0e4, ALU.add, ALU.mult, ["ohg"], ["pen"])
            mel, oh1, mel2, oh2 = rs[3], rs[0], rs[1], rs[2]
            for gq in range(4):
                TT(P, "dve", mel[:, :, gq * 4:(gq + 1) * 4], lg[:, :, 4 + gq * 4:8 + gq * 4],
                   pen[:, :, gq:gq + 1].to_broadcast([128, NT, 4]), ALU.add, lgr + ["pen"], ["mel"])
            red(m1[:], mel[:], ALU.max, ["mel"], ["m1"])
            TT(P, "dve", oh1[:], mel[:], bc(m1, 16), ALU.is_ge, ["mel", "m1", "ge"], ["oh1"])
            STT(P, "dve", mel2[:], oh1[:], -1.0e4, mel[:], ALU.mult, ALU.add, ["oh1", "mel", "ohg"], ["mel2"])
            red(m2[:], mel2[:], ALU.max, ["mel2"], ["m2"])
            TT(P, "dve", oh2[:], mel2[:], bc(m2, 16), ALU.is_ge, ["mel2", "m2", "pen"], ["oh2"])
            TT(P, "dve", dd[:], m2[:], m1[:], ALU.subtract, ["m1", "m2"], ["dd"])
            ACT(P, dd[:], dd[:], AF.Exp, ["dd"], ["dd"])
            TS(P, "dve", w1[:], dd[:], 1.0, None, ALU.add, None, ["dd"], ["w1"])
            P.op("dve", lambda e: e.reciprocal(w1[:], w1[:]), ["w1"], ["w1"])
            TT(P, "dve", w2[:], dd[:], w1[:], ALU.mult, ["dd", "w1"], ["w2"])
            TT(P, "dve", w1[:], w1[:], gprob[:], ALU.mult, ["w1", "gprob", "w2"], ["w1"])
            TT(P, "dve", w2[:], w2[:], gprob[:], ALU.mult, ["w2", "gprob"], ["w2"])
            TT(P, "dve", oh1[:], oh1[:], bc(w1, 16), ALU.mult, ["oh1", "w1", "mel2"], ["oh1"])
            TT(P, "dve", oh2[:], oh2[:], bc(w2, 16), ALU.mult, ["oh2", "w2"], ["oh2"])
            TT(P, "dve", gates[:], oh1[:], oh2[:], ALU.add, ["oh1", "oh2"], ["gates"])
            dump("gates", gates[:], [128, NT, 16], ["gates"])
            P.barrier()
            A.free(*p4_tmp)
            A.free(mixT)
            if stop_after == 4:
                A.free(hT, gates)
                continue

            selc = A.alloc("selc", [16, 16 * 128], F32)
            gT = A.alloc("gT", [16, 1024], F32)
            wdn = A.alloc("wdn", [128, 16, D], BF16)
            hid = A.alloc("hid", [128, 16, 1024], BF16)
            yacc = A.alloc("yacc", [128, 8, D], F32)
            sgt = [A.alloc("sgt%d" % i, [128, 512], BF16) for i in range(2)]
            ttm = [A.alloc("ttm%d" % i, [128, 512], BF16) for i in range(2)]
            lng = A.alloc("lng2", [128, D], F32)
            lnb = A.alloc("lnb2", [128, D], F32)
            hres = [A.alloc("hres%d" % i, [128, D], F32) for i in range(2)]
            r2 = [A.alloc("r2_%d" % i, [128, D], F32) for i in range(2)]
            on = [A.alloc("on%d" % i, [128, D], F32) for i in range(2)]
            lnj = [A.alloc("lnj2_%d" % i, [128, D], BF16) for i in range(2)]
            stt_ = [A.alloc("st2_%d" % i, [128, 8], F32) for i in range(2)]
            p5_tmp = [selc, gT, wdn, hid, yacc, lng, lnb] + Wgu + sgt + ttm + hres + r2 + on + lnj + stt_
            DMA(P, "sp", selc[:], sel_d, [], ["selc"])
            DMA(P, "sp", lng[:], ln2g_d, [], ["lng"])
            DMA(P, "sp", lnb[:], ln2b_d, [], ["lnb"])
            gub = Rot([2, 3, 4, 5])
            geb = Rot([0, 1])
            dnb = Rot([6, 7])
            wi = 0
            si = 0
            li = 0
            for hf in range(2):
                for j4 in range(2):
                    for k in range(4):
                        t = hf * 8 + j4 * 4 + k
                        TR(P, ps[j4][0:16, k * 128:(k + 1) * 128], gates[:, t, :], ident_f[:], ["gates"], [PSR(j4)], inc=(k == 3))
                    CP(P, "dve", gT[:, j4 * 512:(j4 + 1) * 512], ps[j4][0:16, :], [PSR(j4)], [("gT", j4)])
                for eh in range(2):
                    for el in range(8):
                        e = eh * 8 + el
                        W = Wgu[wi % 2]
                        wres = ("Wgu", wi % 2)
                        if not (hf == 0 and eh == 0 and el < 2):
                            DMA(P, "pool", W[:, :, 0:256], wg_d[e].rearrange("(c p) f -> p c f", p=128), [], [wres])
                            DMA(P, "pool", W[:, :, 256:512], wu_d[e].rearrange("(c p) f -> p c f", p=128), [], [wres])
                        DMA(P, "pool", wdn[:, 2 * el:2 * el + 2, :], wd_d[e].rearrange("(j p) d -> p j d", p=128), [],
                            [("wdn", el)])
                        wi += 1
                        for sub in range(2):
                            ts_ = slice(hf * 1024 + sub * 512, hf * 1024 + sub * 512 + 512)
                            loc = slice(sub * 512, sub * 512 + 512)
                            gb_ = geb.next()
                            MM(P, psf(gb_), selc[:, e * 128:(e + 1) * 128], gT[:, loc], True, True,
                               ["selc", ("gT", sub)], [PSR(gb_)])
                            for j in range(2):
                                bg, bu = gub.next(), gub.next()
                                for c in range(8):
                                    MM(P, psf(bg), W[:, c, j * 128:(j + 1) * 128], hT[:, c, ts_], c == 0, c == 7,
                                       [wres], [PSR(bg)], inc=(c == 7))
                                for c in range(8):
                                    MM(P, psf(bu), W[:, c, 256 + j * 128:256 + (j + 1) * 128], hT[:, c, ts_], c == 0, c == 7,
                                       [wres], [PSR(bu)], inc=(c == 7))
                                sg_, tm_ = sgt[si % 2], ttm[si % 2]
                                ACT(P, sg_[:], psf(bg), AF.Silu, [PSR(bg)], [("sgt", si % 2)])
                                TT(P, "dve", tm_[:], sg_[:], psf(bu), ALU.mult, [("sgt", si % 2), PSR(bu)], [("ttm", si % 2)])
                                TT(P, "dve", hid[:, el * 2 + j, loc], tm_[:], psf(gb_), ALU.mult,
                                   [("ttm", si % 2), PSR(gb_)], [("hid", el * 2 + j, sub)])
                                si += 1
                    hres_all = [("hid", k, sub) for k in range(16) for sub in range(2)] + [("wdn", el) for el in range(8)]
                    for lt in range(8):
                        t = hf * 8 + lt
                        tok = slice(t * 128, (t + 1) * 128)
                        i = li % 2
                        tag = ("ln2", i)
                        if eh == 1 and lt == 0:
                            for l2 in range(2):
                                t2 = hf * 8 + l2
                                DMA(P, "sp", hres[(li + l2) % 2][:], hscr_d[seq, t2 * 128:(t2 + 1) * 128, :], [],
                                    [("hres", (li + l2) % 2)])
                        for dh in range(2):
                            b = dnb.next()
                            hs = slice(dh * 512, dh * 512 + 512)
                            for k in range(16):
                                MM(P, psf(b), hid[:, k, lt * 128:(lt + 1) * 128], wdn[:, k, hs], k == 0, k == 15,
                                   hres_all, [PSR(b)], inc=(k == 15))
                            if eh == 0:
                                CP(P, "act", yacc[:, lt, hs], psf(b), [PSR(b)], [("yacc", lt)])
                            else:
                                TT(P, "dve", r2[i][:, hs], yacc[:, lt, hs], psf(b), ALU.add, [("yacc", lt), PSR(b)], [(tag, "r0")])
                                STT(P, "dve", r2[i][:, hs], hres[i][:, hs], ALPHA, r2[i][:, hs], ALU.mult, ALU.add,
                                    [("hres", i), (tag, "r0")], [(tag, "r")])
                        if eh == 1:
                            if lt + 2 < 8:
                                t2 = hf * 8 + lt + 2
                                DMA(P, "sp", hres[i][:], hscr_d[seq, t2 * 128:(t2 + 1) * 128, :], [], [("hres", i)])
                            layer_norm(r2[i], stt_[i], lnj[i], lng, lnb, on[i], tag)
                            DMA(P, "sp", out_d[seq, tok, :], on[i][:], [(tag, "hn")], [])
                            li += 1
            P.barrier()
            A.free(*p5_tmp)
            A.free(hT, gates)
    P.emit()
    return nc, dumps


def _in_map(inp, core, consts, wfm, wtm):
    m = dict(consts)
    m["x"] = np.ascontiguousarray(inp["x"][SEQ_PER_CORE * core:SEQ_PER_CORE * (core + 1)], dtype=np.float32)
    m["wfm"] = wfm
    m["wtm"] = wtm
    cw = inp["conv_w"][0]
    m["convw"] = np.ascontiguousarray(cw.reshape(4, 6, 128).transpose(2, 1, 0).reshape(128, 24))
    m["convb"] = np.ascontiguousarray(inp["conv_b"][0].reshape(6, 128).T)
    m["dtb"] = _rep(inp["dt_bias"][0])
    m["alog"] = _rep(inp["a_log"][0])
    m["dskip"] = _rep(np.repeat(inp["d_skip"][0], 64))
    m["nw"] = _rep(inp["ssd_norm_w"][0])
    m["nwc"] = np.ascontiguousarray(inp["ssd_norm_w"][0].reshape(4, 128).T)
    m["wout"] = np.ascontiguousarray(inp["w_out"][0])
    m["ln1g"] = _rep(inp["ln1_g"][0])
    m["ln1b"] = _rep(inp["ln1_b"][0])
    m["ln2g"] = _rep(inp["ln2_g"][0])
    m["ln2b"] = _rep(inp["ln2_b"][0])
    m["wr"] = np.ascontiguousarray(np.concatenate([inp["w_route_group"][0], inp["w_route_expert"][0]], 1))
    m["br"] = _rep(np.concatenate([inp["b_route_group"][0], inp["b_route_expert"][0]]))
    m["wg"] = np.ascontiguousarray(inp["w_gate"][0])
    m["wu"] = np.ascontiguousarray(inp["w_up"][0])
    m["wd"] = np.ascontiguousarray(inp["w_down"][0])
    return m


def kernel(**inputs):
    inp = {k: np.asarray(v, dtype=np.float32) for k, v in inputs.items()}
    wfm, wtm = _layout_w_in(inp["w_in"])
    consts = _consts()
    nc, _ = build()
    in_maps = [_in_map(inp, c, consts, wfm, wtm) for c in range(NCORES)]
    res = run_bass_kernel_spmd(nc, in_maps, core_ids=list(range(NCORES)))
    out = np.concatenate([np.asarray(res.results[c]["out"], dtype=np.float32) for c in range(NCORES)], axis=0)
    return out
```
